# Optimizing a Trainium2 kernel written in Bass

```python
import jax, jax.numpy as jnp
from jax import lax
import numpy as np

D_MODEL = 1024
BATCH = 8
SEQ = 2048
DEPTH = 2

CHUNK = 64
GROUP_W = 512
N_GROUPS = 3
D_MIX = N_GROUPS * GROUP_W
H_A = 4
DH_A = GROUP_W // H_A
CONV_K = 4
POOL_WINDOWS = (2, 4, 8, 16)
N_POOL = len(POOL_WINDOWS)
DC_B = GROUP_W // N_POOL
H_C = 4
DH_C = GROUP_W // H_C
N_MEM = 256
EPS = 1e-6

_SIZES = (2 * GROUP_W, GROUP_W, GROUP_W, GROUP_W, 2 * H_A, GROUP_W, GROUP_W, GROUP_W, GROUP_W)
N_IN = sum(_SIZES)
_OFFS = tuple(int(v) for v in np.cumsum(_SIZES)[:-1])

kernel_name = "hybrid_mlstm_pool_memattn_block"


def rmsnorm(x, g):
    xf = x.astype(jnp.float32)
    y = xf * lax.rsqrt(jnp.mean(xf * xf, axis=-1, keepdims=True) + EPS)
    return (y * g.astype(jnp.float32)).astype(x.dtype)


def causal_dwconv(x, w, b):
    S = x.shape[1]
    xp = jnp.pad(x, ((0, 0), (CONV_K - 1, 0), (0, 0)))
    return b + sum(xp[:, j:j + S] * w[j] for j in range(CONV_K))


def mlstm_chunkwise(q, k, v, ig, logf):
    B, S, H, D = q.shape
    NC, L = S // CHUNK, CHUNK
    to_c = lambda a: a.reshape(B, NC, L, H, D).transpose(0, 3, 1, 2, 4)
    q, k, v = to_c(q), to_c(k), to_c(v)
    ig = ig.reshape(B, NC, L, H).transpose(0, 3, 1, 2)
    logf = logf.reshape(B, NC, L, H).transpose(0, 3, 1, 2)
    b = jnp.cumsum(logf, axis=-1)
    bL = b[..., -1]
    g = bL[..., None] - b + ig
    gmax = jnp.max(g, axis=-1)
    w = jnp.exp(g - gmax[..., None])
    kv_loc = jnp.einsum('bhcld,bhcle->bhcde', k * w[..., None], v)
    n_loc = jnp.einsum('bhcld,bhcl->bhcd', k, w)

    def step(carry, inp):
        C, n, m = carry
        kv_c, n_c, bL_c, gmax_c = inp
        m_new = jnp.maximum(bL_c + m, gmax_c)
        a = jnp.exp(bL_c + m - m_new)
        bc = jnp.exp(gmax_c - m_new)
        C_new = a[..., None, None] * C + bc[..., None, None] * kv_c
        n_new = a[..., None] * n + bc[..., None] * n_c
        return (C_new, n_new, m_new), (C, n, m)

    init = (jnp.zeros((B, H, D, D), jnp.float32), jnp.zeros((B, H, D), jnp.float32),
            jnp.zeros((B, H), jnp.float32))
    mv = lambda a: jnp.moveaxis(a, 2, 0)
    _, (C_prev, n_prev, m_prev) = lax.scan(step, init, (mv(kv_loc), mv(n_loc), mv(bL), mv(gmax)))
    C_prev = jnp.moveaxis(C_prev, 0, 2)
    n_prev = jnp.moveaxis(n_prev, 0, 2)
    m_prev = jnp.moveaxis(m_prev, 0, 2)

    causal = jnp.tril(jnp.ones((L, L), dtype=bool))
    dmat = jnp.where(causal, b[..., :, None] - b[..., None, :] + ig[..., None, :], -jnp.inf)
    inter = b + m_prev[..., None]
    m_t = jnp.maximum(inter, jnp.max(dmat, axis=-1))
    s = jnp.einsum('bhcld,bhcsd->bhcls', q, k) * jnp.exp(dmat - m_t[..., None])
    a_t = jnp.exp(inter - m_t)
    num = a_t[..., None] * jnp.einsum('bhcld,bhcde->bhcle', q, C_prev) + jnp.einsum('bhcls,bhcse->bhcle', s, v)
    den = a_t * jnp.einsum('bhcld,bhcd->bhcl', q, n_prev) + jnp.sum(s, axis=-1)
    h = num / jnp.maximum(jnp.abs(den), jnp.exp(-m_t))[..., None]
    return h.transpose(0, 2, 3, 1, 4).reshape(B, S, H, D)


def mlstm_branch(qk_raw, v, o, ig, fg, conv_w, conv_b, norm_g):
    B, S, _ = v.shape
    f32 = jnp.float32
    qk = jax.nn.silu(causal_dwconv(qk_raw, conv_w, conv_b))
    q, k = jnp.split(qk, 2, axis=-1)
    q = q.astype(f32).reshape(B, S, H_A, DH_A)
    k = k.astype(f32).reshape(B, S, H_A, DH_A) * (DH_A ** -0.5)
    vh = v.astype(f32).reshape(B, S, H_A, DH_A)
    h = mlstm_chunkwise(q, k, vh, ig.astype(f32), jax.nn.log_sigmoid(fg.astype(f32)))
    h = jax.nn.sigmoid(o.astype(f32)).reshape(B, S, H_A, DH_A) * h
    h = h * lax.rsqrt(jnp.mean(h * h, axis=-1, keepdims=True) + EPS)
    h = h * norm_g.astype(f32).reshape(H_A, DH_A)
    return h.reshape(B, S, GROUP_W).astype(v.dtype)


def pool_branch(u, w, scale):
    B, S, _ = u.shape
    uf = u.astype(jnp.float32).reshape(B, S, N_POOL, DC_B)
    cs = jnp.cumsum(uf, axis=1)
    t1 = jnp.arange(1, S + 1, dtype=jnp.float32)
    pooled = []
    for gi, win in enumerate(POOL_WINDOWS):
        c = cs[:, :, gi]
        lo = jnp.pad(c, ((0, 0), (win, 0), (0, 0)))[:, :S]
        cnt = jnp.minimum(t1, float(win))[None, :, None]
        pooled.append((c - lo) / cnt)
    p = (jnp.stack(pooled, axis=2) - uf).astype(u.dtype)
    y = jnp.einsum('bsgc,gcd->bsgd', p, w).reshape(B, S, GROUP_W)
    return y * scale


def mem_attn_branch(qc, mem, mem_norm_g, w_kv):
    B, S, _ = qc.shape
    mn = rmsnorm(mem, mem_norm_g)
    kv = mn @ w_kv
    km, vm = jnp.split(kv, 2, axis=-1)
    km = km.reshape(B, N_MEM, H_C, DH_C)
    vm = vm.reshape(B, N_MEM, H_C, DH_C)
    q = qc.reshape(B, S, H_C, DH_C)
    s = jnp.einsum('bshd,bmhd->bhsm', q, km).astype(jnp.float32) * (DH_C ** -0.5)
    p = jax.nn.softmax(s, axis=-1).astype(vm.dtype)
    return jnp.einsum('bhsm,bmhd->bshd', p, vm).reshape(B, S, GROUP_W)


def setup_inputs(seed: int = 0) -> dict:
    key = jax.random.key(seed)
    ks = jax.random.split(key, 16)
    nrm = jax.random.normal
    f32 = jnp.float32
    x = nrm(ks[0], (BATCH, SEQ, D_MODEL), f32)
    mem = nrm(ks[1], (BATCH, N_MEM, D_MODEL), f32)
    norm_g = 1.0 + 0.1 * nrm(ks[2], (DEPTH, D_MODEL), f32)
    w_in = nrm(ks[3], (DEPTH, D_MODEL, N_IN), f32) * D_MODEL ** -0.5
    b_i = 0.1 * nrm(ks[4], (DEPTH, H_A), f32)
    b_f = jnp.linspace(3.0, 6.0, H_A, dtype=f32)[None, :] + 0.1 * nrm(ks[5], (DEPTH, H_A), f32)
    b_gates = jnp.concatenate([b_i, b_f], axis=-1)
    conv_w = nrm(ks[6], (DEPTH, CONV_K, 2 * GROUP_W), f32) * CONV_K ** -0.5
    conv_b = 0.02 * nrm(ks[7], (DEPTH, 2 * GROUP_W), f32)
    mlstm_norm_g = 1.0 + 0.1 * nrm(ks[8], (DEPTH, GROUP_W), f32)
    pool_w = nrm(ks[9], (DEPTH, N_POOL, DC_B, DC_B), f32) * DC_B ** -0.5
    pool_scale = 1.0 + 0.1 * nrm(ks[10], (DEPTH, GROUP_W), f32)
    mem_norm_g = 1.0 + 0.1 * nrm(ks[11], (DEPTH, D_MODEL), f32)
    w_mem_kv = nrm(ks[12], (DEPTH, D_MODEL, 2 * GROUP_W), f32) * D_MODEL ** -0.5
    w_out = nrm(ks[13], (DEPTH, D_MIX, D_MODEL), f32) * D_MIX ** -0.5
    final_norm_g = 1.0 + 0.1 * nrm(ks[14], (D_MODEL,), f32)
    return {"x": x, "mem": mem, "norm_g": norm_g, "w_in": w_in, "b_gates": b_gates,
            "conv_w": conv_w, "conv_b": conv_b, "mlstm_norm_g": mlstm_norm_g,
            "pool_w": pool_w, "pool_scale": pool_scale, "mem_norm_g": mem_norm_g,
            "w_mem_kv": w_mem_kv, "w_out": w_out, "final_norm_g": final_norm_g}


def reference(x, mem, norm_g, w_in, b_gates, conv_w, conv_b, mlstm_norm_g, pool_w, pool_scale,
              mem_norm_g, w_mem_kv, w_out, final_norm_g):
    for l in range(DEPTH):
        h = rmsnorm(x, norm_g[l])
        proj = h @ w_in[l]
        qk_raw, v, o, z_a, gates, u, z_b, qc, z_c = jnp.split(proj, _OFFS, axis=-1)
        gates = gates + b_gates[l]
        ig, fg = gates[..., :H_A], gates[..., H_A:]
        y_a = mlstm_branch(qk_raw, v, o, ig, fg, conv_w[l], conv_b[l], mlstm_norm_g[l]) * jax.nn.silu(z_a)
        y_b = pool_branch(u, pool_w[l], pool_scale[l]) * jax.nn.silu(z_b)
        y_c = mem_attn_branch(qc, mem, mem_norm_g[l], w_mem_kv[l]) * jax.nn.silu(z_c)
        x = x + jnp.concatenate([y_a, y_b, y_c], axis=-1) @ w_out[l]
    return rmsnorm(x, final_norm_g)
```

```python
import contextlib
import math
import numpy as np
import concourse.bass as bass
import concourse.mybir as mybir
from concourse.bass_utils import run_bass_kernel_spmd

F32 = mybir.dt.float32
BF16 = mybir.dt.bfloat16
AF = mybir.ActivationFunctionType
ALU = mybir.AluOpType

S = 2048
D = 1024
NIN = 4616
NMEM = 256
EPS = 1e-6
O_QK, O_V, O_O, O_ZA, O_G, O_U, O_ZB, O_QC, O_ZC = 0, 1024, 1536, 2048, 2560, 2568, 3080, 3592, 4104
LNC = math.log(128 ** -0.5)
RING = 16
import os
PSUM_RES = {"MM0", "MM1", "PT", "BBP", "DCA", "DCB", "NUM", "MISC"}
WS_LAG = int(os.environ.get("WS_LAG", "0"))
PUMP_N = int(os.environ.get("PUMP_N", "2"))
BADD = os.environ.get("BADD", "dve")
BADD2 = os.environ.get("BADD2", "dve")
KW_ACT = bool(int(os.environ.get("KW_ACT", "1")))
SEQ_EMIT = bool(int(os.environ.get("SEQ_EMIT", "0")))


class Tracker:
    def __init__(self, nc, es):
        self.nc = nc
        self.es = es
        self.eng = {"pe": nc.tensor, "act": nc.scalar, "dve": nc.vector, "pool": nc.gpsimd, "sp": nc.sync}
        self.sem = {}
        self.cnt = {}
        self.known = {k: {} for k in self.eng}
        for k in self.eng:
            self.sem[k] = es.enter_context(nc.semaphore("s_" + k))
            self.cnt[k] = 0
        self.lastw = {}
        self.readers = {}
        self.nwait = 0

    def _key(self, key):
        if key not in self.sem:
            self.sem[key] = self.es.enter_context(self.nc.semaphore("s_" + key.replace(":", "_")))
            self.cnt[key] = 0
        return self.sem[key]

    def _deps(self, ename, R, W):
        deps = {}

        def add(k, v, war=False):
            if war and k == ename and ename == "pe":
                return
            if v > deps.get(k, 0):
                deps[k] = v

        for r in R:
            if r in self.lastw:
                add(*self.lastw[r])
        for w in W:
            if w in self.lastw:
                add(*self.lastw[w])
            for k, v in self.readers.get(w, {}).items():
                add(k, v, war=True)
        return deps

    def _wait(self, ename, deps):
        e = self.eng[ename]
        for k, v in deps.items():
            if k == ename and ename == "pe":
                continue
            if self.known[ename].get(k, 0) >= v:
                continue
            e.wait_ge(self.sem[k], v)
            self.nwait += 1
            self.known[ename][k] = v

    def _commit(self, key, val, R, W):
        for r in R:
            self.readers.setdefault(r, {})[key] = val
        for w in W:
            self.lastw[w] = (key, val)
            self.readers[w] = {}

    def begin(self):
        self.rec = []

    def end(self):
        r, self.rec = self.rec, None
        return r

    DEF_COST = {"pe": 0.35, "act": 0.42, "dve": 0.33, "pool": 0.5, "sp": 0.1}
    SYNC = 0.15

    def _m_norm(self, R, W):
        W2 = tuple(W) + tuple(r for r in R if r in PSUM_RES)
        R2 = tuple(r for r in R if r not in PSUM_RES)
        return R2, W2

    def _m_start(self, ename, R, W):
        if not hasattr(self, "m_free"):
            self.m_free, self.m_w, self.m_r, self.m_we = {}, {}, {}, {}
        t = self.m_free.get(ename, 0.0)
        for r in R:
            if r in self.m_w:
                t = max(t, self.m_w[r] + (self.SYNC if self.m_we.get(r) != ename else 0.05))
        for w in W:
            if w in self.m_w:
                t = max(t, self.m_w[w] + (self.SYNC if self.m_we.get(w) != ename else 0.0))
            if w in self.m_r:
                t = max(t, self.m_r[w] + self.SYNC)
        return t

    def _m_commit(self, ename, R, W, cost):
        R, W = self._m_norm(R, W)
        st = self._m_start(ename, R, W)
        en = st + cost
        self.m_free[ename] = en
        for r in R:
            self.m_r[r] = max(self.m_r.get(r, 0.0), en)
        for w in W:
            self.m_w[w] = en
            self.m_r[w] = 0.0
            self.m_we[w] = ename

    def emit(self, lists):
        lists = [l_ for l_ in lists if l_]
        if SEQ_EMIT:
            lists = [[x for l_ in lists for x in l_]]
        idx = [0] * len(lists)
        while True:
            best, bk = None, None
            for k, l_ in enumerate(lists):
                if idx[k] < len(l_):
                    kind, a = l_[idx[k]]
                    if kind == "op":
                        R, W = self._m_norm(a[2], a[3])
                        st = self._m_start(a[0], R, W)
                    elif kind == "call":
                        st = -1.0
                    else:
                        st = self._m_start(a[0], (), ())
                    key = (st, (idx[k] + 0.5) / len(l_))
                    if best is None or key < best:
                        best, bk = key, k
            if bk is None:
                break
            kind, a = lists[bk][idx[bk]]
            idx[bk] += 1
            if kind == "op":
                self.op(*a)
            elif kind == "call":
                a()
            else:
                self.dma(*a)

    def call(self, fn):
        if getattr(self, "rec", None) is not None:
            self.rec.append(("call", fn))
        else:
            fn()

    def op(self, ename, fn, R=(), W=(), cost=None):
        if cost is None:
            cost = self.DEF_COST[ename]
        if getattr(self, "rec", None) is not None:
            self.rec.append(("op", (ename, fn, tuple(R), tuple(W), cost)))
            return
        self._m_commit(ename, R, W, cost)
        W = tuple(W) + tuple(r for r in R if r in PSUM_RES)
        R = tuple(r for r in R if r not in PSUM_RES)
        self._wait(ename, self._deps(ename, R, W))
        ins = fn(self.eng[ename])
        self.cnt[ename] += 1
        ins.then_inc(self.sem[ename], 1)
        self._commit(ename, self.cnt[ename], R, W)

    def dma(self, qname, pairs, R=(), W=(), key=None):
        if getattr(self, "rec", None) is not None:
            self.rec.append(("dma", (qname, pairs, tuple(R), tuple(W), key)))
            return
        key = "dma:" + (key or (W[0] if W else R[0]))
        self._m_commit(key, R, W, 2.5)
        sem = self._key(key)
        self._wait(qname, self._deps(qname, R, W))
        for (o, i) in pairs:
            self.eng[qname].dma_start(out=o, in_=i).then_inc(sem, 16)
            self.cnt[key] += 16
        self._commit(key, self.cnt[key], R, W)

    def finish(self, keys):
        for k in keys:
            k = "dma:" + k
            if k in self.cnt and self.cnt[k] > 0:
                self.eng["sp"].wait_ge(self.sem[k], self.cnt[k])


class WStream:
    def __init__(self, T, slots):
        self.T = T
        self.slots = slots
        self.blocks = []
        self.index = {}
        self.issued = 0

    def plan(self, key, fn):
        self.index[key] = len(self.blocks)
        self.blocks.append(fn)

    def _issue_upto(self, i):
        while self.issued <= i and self.issued < len(self.blocks):
            j = self.issued
            s = j % len(self.slots)
            pairs = self.blocks[j](self.slots[s])
            self.T.dma("pool", pairs, W=["W%d" % s], key="W%d" % s)
            self.issued += 1

    def start(self):
        self._issue_upto(len(self.slots) - 1)

    def get(self, key):
        i = self.index[key]
        self._issue_upto(i)
        s = i % len(self.slots)
        return self.slots[s], "W%d" % s

    def done(self, key):
        i = self.index[key]
        if not hasattr(self, "done_set"):
            self.done_set, self.first_undone = set(), 0
        self.done_set.add(i)
        while self.first_undone in self.done_set:
            self.first_undone += 1
        self.limit = max(getattr(self, "limit", -1), self.first_undone + len(self.slots) - 1 - WS_LAG)
        if not getattr(self, "throttle", False):
            self._issue_upto(self.limit)

    def pump(self, n):
        lim = min(getattr(self, "limit", -1), self.issued + n - 1)
        self._issue_upto(lim)


def build_program(n_layers=2, final_norm=True, dbg=None):
    dbg = dbg or {}
    nc = bass.Bass("TRN2", target_bir_lowering=False)
    L = 2
    x_d = nc.dram_tensor("x", [S, D], F32, kind="ExternalInput").ap()
    mem_d = nc.dram_tensor("mem", [NMEM, D], F32, kind="ExternalInput").ap()
    w_in_d = nc.dram_tensor("w_in", [L, D, NIN], F32, kind="ExternalInput").ap()
    w_kv_d = nc.dram_tensor("w_mem_kv", [L, D, 1024], F32, kind="ExternalInput").ap()
    w_out_d = nc.dram_tensor("w_out", [L, 1536, D], F32, kind="ExternalInput").ap()
    pool_w_d = nc.dram_tensor("pool_w", [L, 4, 128, 128], F32, kind="ExternalInput").ap()
    smallp_d = nc.dram_tensor("smallp", [128, L, 56], F32, kind="ExternalInput").ap()
    gb_d = nc.dram_tensor("gb", [7, 128, D], F32, kind="ExternalInput").ap()
    cst_d = nc.dram_tensor("cst", [128, 536], F32, kind="ExternalInput").ap()
    out_d = nc.dram_tensor("out", [S, D], F32, kind="ExternalOutput").ap()
    dbg_d = {}
    for name, shape in dbg.items():
        dbg_d[name] = nc.dram_tensor("dbg_" + name, list(shape), F32, kind="ExternalOutput").ap()

    with contextlib.ExitStack() as es:
        T = Tracker(nc, es)

        def sb(name, shape, dt):
            return es.enter_context(nc.sbuf_tensor(name, list(shape), dt))

        def ps(name, shape, dt):
            return es.enter_context(nc.psum_tensor(name, list(shape), dt))

        X = sb("X", [128, 16, D], F32)
        XNT = sb("XNT", [128, 8, 1024], BF16)
        YT = sb("YT", [128, 12, 1024], BF16)
        slots = [sb("W%d" % i, [128, 1024], BF16) for i in range(RING)]
        FBIG = sb("FBIG", [128, 8 * 528], F32)
        FT = [FBIG[:, i * 528:(i + 1) * 528] for i in range(8)]
        G0 = sb("G0", [128, 2048], BF16)
        G1 = sb("G1", [128, 2048], BF16)
        G2 = sb("G2", [128, 2048], BF16)
        XNK = [sb("XNK%d" % i, [128, 1024], BF16) for i in range(2)]
        MEMB = FBIG[:, 0:1024]
        CST = sb("CST", [128, 536], F32)
        IDB = sb("IDB", [128, 128], BF16)
        ONESF = sb("ONESF", [128, 128], F32)
        NEGH = sb("NEGH", [128, 32], F32)
        GX = sb("GX", [128, D], F32)
        GM = GX
        DG = sb("DG", [128, 4, 4, 128], BF16)
        CBROW = sb("CBROW", [1, 1024], BF16)
        ONESROW = sb("ONESROW", [1, 512], BF16)
        HALOB = sb("HALOB", [128, 8, 4], BF16)
        SPALL = sb("SPALL", [128, L, 56], F32)
        SPX2 = sb("SPX", [128, 2, 24], F32)
        PW = sb("PW", [128, 4, 128], BF16)
        WG = sb("WG", [128, 8, 8], BF16)
        KMT = sb("KMT", [128, 4, 256], BF16)
        VM = sb("VM", [128, 2, 4, 129], BF16)
        SS = sb("SS", [128, 16], F32)
        RSTD = sb("RSTD", [128, 16], F32)
        TMPS = sb("TMPS", [128, 16], F32)
        MSS = sb("MSS", [128, 8], F32)
        HALO_U = sb("HALO_U", [128, 4, 16], F32)
        GA = sb("GA", [128, 8, 8], F32)
        E1 = sb("E1", [128, 8, 4], F32)
        L1 = sb("L1", [128, 32], F32)
        TB = sb("TB", [128, 64], F32)
        EXPB = sb("EXPB", [128, 32], F32)
        WK = sb("WK", [128, 32], F32)
        VB = [sb("VB%d" % i, [128, 2, 129], BF16) for i in range(2)]
        THO = [sb("THO%d" % i, [128, 256], F32) for i in range(2)]
        EBT = sb("EBT", [128, 32], F32)
        IEB = sb("IEB", [128, 32], F32)
        DEC = sb("DEC", [128, 32], F32)
        STB = [sb("STB%d" % i, [128, 256], BF16) for i in range(2)]
        KW = sb("KW", [128, 2, 128], BF16)
        C32 = sb("C32", [128, 4, 129], F32)
        CBA = [sb("CBA%d" % i, [128, 2, 129], BF16) for i in range(2)]
        TTt = sb("TTt", [128, 256], F32)
        HGt = sb("HGt", [128, 256], F32)
        G3 = sb("G3", [128, 1536], BF16)
        SM = sb("SM", [128, 16], F32)
        YTK = [sb("YTK%d" % i, [128, 256], BF16) for i in range(2)]
        RDC = sb("RDC", [128, 4], F32)
        MM = [ps("MM0", [128, 512], F32), ps("MM1", [128, 512], F32)]
        PT = ps("PT", [128, 1024], BF16)
        BBP = ps("BBP", [128, 512], F32)
        DCA = ps("DCA", [128, 512], F32)
        DCB = ps("DCB", [128, 512], F32)
        NUM = ps("NUM", [128, 512], F32)
        MISC = ps("MISC", [128, 512], F32)
        mmi = [0]

        def next_mm():
            mmi[0] ^= 1
            return MM[mmi[0]], "MM%d" % mmi[0]

        U_ = CST[:, 128:256]
        LX_ = CST[:, 256:384]
        INVC = CST[:, 384:400]
        LNC8 = CST[:, 400:408]
        SEL_ = CST[:, 408:536]

        ws = WStream(T, slots)

        def wv(wd, l):
            return wd[l].rearrange("(kc p) n -> p kc n", p=128)

        def feat_block(wd, l, c0):
            def f(slot):
                return [(slot[:, :].rearrange("p (k c) -> p k c", c=128), wv(wd, l)[:, :, c0:c0 + 128])]
            return f

        def mov_block(wd, l, j, c0):
            def f(slot):
                return [(slot[:, :].rearrange("p (k c) -> p k c", c=512), wv(wd, l)[:, 2 * j:2 * j + 2, c0:c0 + 512])]
            return f

        def vo_block(l, j, hp):
            def f(slot):
                v = slot[:, :].rearrange("p (k c) -> p k c", c=512)
                w = wv(w_in_d, l)
                return [(v[:, :, 0:256], w[:, 2 * j:2 * j + 2, O_V + hp * 256:O_V + hp * 256 + 256]),
                        (v[:, :, 256:512], w[:, 2 * j:2 * j + 2, O_O + hp * 256:O_O + hp * 256 + 256])]
            return f

        def wo_block(l, kc):
            def f(slot):
                return [(slot[:, :], w_out_d[l, kc * 128:(kc + 1) * 128, :])]
            return f

        for l in range(n_layers):
            for h in range(4):
                ws.plan(("kvk", l, h), feat_block(w_kv_d, l, h * 128))
            for j in range(4):
                ws.plan(("kvv", l, j), mov_block(w_kv_d, l, j, 512))
            for hf in range(2):
                for j in range(4):
                    ws.plan(("zc", l, hf, j), mov_block(w_in_d, l, j, O_ZC))
                for h in range(4):
                    ws.plan(("qc", l, hf, h), feat_block(w_in_d, l, O_QC + h * 128))
                for g in range(4):
                    ws.plan(("u", l, hf, g), feat_block(w_in_d, l, O_U + g * 128))
                    ws.plan(("zb", l, hf, g), feat_block(w_in_d, l, O_ZB + g * 128))
                for hp in range(2):
                    for hh in range(2):
                        h = 2 * hp + hh
                        ws.plan(("q", l, hf, h), feat_block(w_in_d, l, O_QK + h * 128))
                        ws.plan(("k", l, hf, h), feat_block(w_in_d, l, O_QK + 512 + h * 128))
                    for hh in range(2):
                        ws.plan(("za", l, hf, 2 * hp + hh), feat_block(w_in_d, l, O_ZA + (2 * hp + hh) * 128))
                    for j in range(4):
                        ws.plan(("vo", l, hf, hp, j), vo_block(l, j, hp))
                for ch in range(2):
                    for j in range(6):
                        ws.plan(("wo", l, hf, ch, j), mov_block(w_out_d, l, j, ch * 512))

        T.dma("sp", [(CST[:, :], cst_d[:, :])], W=["CST"])
        T.dma("sp", [(SPALL[:, :, :], smallp_d[:, :, :])], W=["SPALL"])
        T.dma("pool", [(IDB[:, :], cst_d[:, 0:128])], W=["IDB"])
        T.op("dve", lambda e: e.memset(ONESF[:, :], 1.0), W=["ONESF"])
        T.op("dve", lambda e: e.memset(NEGH[:, :], -0.5), W=["NEGH"])
        T.op("dve", lambda e: e.memset(VM[:, :, :, 128:129], 2.0), W=["VM"])
        for p_ in range(2):
            T.op("dve", lambda e, p_=p_: e.memset(VB[p_][:, :, 128:129], 1.0), W=["VB%d" % p_])
        T.op("dve", lambda e: e.memset(ONESROW[:, :], 1.0), W=["ONESROW"])
        ws._issue_upto(7)
        T.dma("sp", [(GX[:, :], gb_d[2])], W=["GX"])
        T.dma("sp", [(MEMB, mem_d[0:128, :])], W=["F0", "F1"], key="MEMB")
        for t in range(8):
            T.dma("sp", [(X[:, t, :], x_d[t * 128:(t + 1) * 128, :])], W=["X%d" % t])
        pending_done = []

        def wdone(key):
            if getattr(T, "rec", None) is not None:
                pending_done.append(key)
            else:
                ws.done(key)

        def flush_done():
            for k_ in pending_done:
                ws.done(k_)
            del pending_done[:]
        mbi = [0]

        def next_mbank():
            mbi[0] ^= 1
            return (BBP, "BBP") if mbi[0] else (NUM, "NUM")

        def dump(name, ap_sb, res):
            if name in dbg_d:
                T.dma("pool", [(dbg_d[name], ap_sb)], R=res, key="dbg_" + name)

        def rstd_cols(ss_ap, out_ap, n, res_in, res_out, scale, eps):
            T.op("dve", lambda e: e.tensor_scalar(out=TMPS[:, 0:n], in0=ss_ap, scalar1=scale, scalar2=eps,
                                                   op0=ALU.mult, op1=ALU.add), R=res_in, W=["TMPS"])
            T.op("pool", lambda e: e.tensor_tensor(out=out_ap, in0=TMPS[:, 0:n], in1=NEGH[:, 0:n], op=ALU.pow),
                 R=["TMPS", "NEGH"], W=res_out)

        def norm_transpose(src_ap, src_res, rstd_ap, rstd_res, gtile, gres, par, dst_ap, dst_res):
            xn = XNK[par]
            T.op("dve", lambda e: e.scalar_tensor_tensor(out=xn[:, :], in0=src_ap, scalar=rstd_ap, in1=gtile[:, :],
                                                          op0=ALU.mult, op1=ALU.mult),
                 R=(src_res if isinstance(src_res, list) else [src_res]) + [rstd_res, gres], W=["XNK%d" % par], cost=1.25)

            def tr(e):
                ins = None
                for kc in range(8):
                    ins = e.transpose(PT[:, kc * 128:(kc + 1) * 128], xn[:, kc * 128:(kc + 1) * 128], IDB[:, :])
                return ins
            T.op("pe", tr, R=["XNK%d" % par, "IDB"], W=["PT"], cost=1.2)
            T.op("act", lambda e: e.copy(out=dst_ap, in_=PT[:, :].rearrange("p (k c) -> p k c", c=128)),
                 R=["PT"], W=(["G1a", "G1b"] if dst_res == "G1a" else [dst_res]), cost=1.0)

        def mm_feat(blk, XN_cols, out_ap):
            def f(e):
                ins = None
                bv = blk[:, :].rearrange("p (k c) -> p k c", c=128)
                for kc in range(8):
                    ins = e.matmul(out_ap, bv[:, kc, :], XNT[:, kc, XN_cols[0]:XN_cols[1]], start=(kc == 0), stop=(kc == 7))
                return ins
            return f

        def silu2_from_psum(pm, pm_res, n, th_tile, th_res, out_ap, out_res, c_act=0.65, c_dve=0.72):
            T.op("act", lambda e: e.activation(out=th_tile[:, 0:n], in_=pm[:, 0:n], func=AF.Tanh, scale=0.5),
                 R=[pm_res], W=[th_res], cost=c_act)
            T.op("dve", lambda e: e.scalar_tensor_tensor(out=out_ap, in0=th_tile[:, 0:n], scalar=1.0, in1=pm[:, 0:n],
                                                          op0=ALU.add, op1=ALU.mult),
                 R=[th_res, pm_res], W=(out_res if isinstance(out_res, list) else [out_res]), cost=c_dve)

        CWvs = [SPALL[:, l_, 0:32].rearrange("p (b j) -> p b j", j=4) for l_ in range(L)]

        def layer_setup(l):
            SPXl, sxr = SPX2[:, l % 2, :], "SPX%d" % (l % 2)
            T.dma("pool", [(CBROW[0:1, :], gb_d[5 + l][0:1, :])], W=["CBROW"])
            T.dma("pool", [(PW[:, :, :], pool_w_d[l].rearrange("g c d -> c g d"))], W=["PW"])
            T.dma("pool", [(WG[:, :, :], wv(w_in_d, l)[:, :, O_G:O_G + 8])], W=["WG"])
            SP = SPALL[:, l, :]
            T.op("dve", lambda e: e.tensor_tensor(out=SPXl[:, 0:8], in0=SP[:, 48:56], in1=LNC8, op=ALU.add),
                 R=["SPALL", "CST"], W=[sxr])
            T.op("dve", lambda e: e.tensor_scalar(out=SPXl[:, 8:12], in0=SP[:, 40:44], scalar1=1.0, scalar2=None, op0=ALU.mult),
                 R=["SPALL"], W=[sxr])
            T.op("dve", lambda e: e.tensor_scalar(out=SPXl[:, 12:16], in0=SP[:, 44:48], scalar1=0.5, scalar2=None, op0=ALU.mult),
                 R=["SPALL"], W=[sxr])
            T.op("dve", lambda e: e.memset(C32[:, :, :], 0.0), W=["C32"])
            T.op("dve", lambda e: e.memset(HALO_U[:, :, :], 0.0), W=["HALO_U"])
            T.op("dve", lambda e: e.memset(HALOB[:, :, :], 0.0), W=["HALOB"])

        def phase_M(l, preloaded=False):
            MNT = G1[:, :].rearrange("p (k c) -> p k c", c=256)
            if not preloaded:
                T.dma("sp", [(GX[:, :], gb_d[2 + l])], W=["GX"])
            for mc in range(2):
                if not (preloaded and mc == 0):
                    T.dma("sp", [(MEMB, mem_d[mc * 128:(mc + 1) * 128, :])], W=["F0", "F1"], key="MEMB")
                T.op("act", lambda e, mc=mc: e.activation(out=G2[:, 0:1024], in_=MEMB, func=AF.Square, accum_out=MSS[:, mc:mc + 1]),
                     R=["F0", "F1"], W=["MSS", "G2a"])
                rstd_cols(MSS[:, mc:mc + 1], MSS[:, 4 + mc:5 + mc], 1, ["MSS"], ["MSS"], 1.0 / D, EPS)
                norm_transpose(MEMB, ["F0", "F1"], MSS[:, 4 + mc:5 + mc], "MSS", GX, "GX", mc,
                               MNT[:, :, mc * 128:(mc + 1) * 128], "G1a")
            for h in range(4):
                blk, bres = ws.get(("kvk", l, h))
                pm, pres = next_mbank()

                def f(e, blk=blk, pm=pm):
                    ins = None
                    bv = blk[:, :].rearrange("p (k c) -> p k c", c=128)
                    for kc in range(8):
                        ins = e.matmul(pm[:, 0:256], bv[:, kc, :], MNT[:, kc, :], start=(kc == 0), stop=(kc == 7))
                    return ins
                T.op("pe", f, R=[bres, "G1a", "G1b"], W=[pres])
                wdone(("kvk", l, h))
                T.op("act", lambda e, pm=pm, h=h: e.activation(out=KMT[:, h, :], in_=pm[:, 0:256], func=AF.Identity, scale=128 ** -0.5),
                     R=[pres], W=["KMT"])
            vblk = [ws.get(("kvv", l, j)) for j in range(4)]
            for mc in range(2):
                pm, pres = next_mbank()

                def f(e, pm=pm, mc=mc):
                    ins = None
                    for kc in range(8):
                        bv = vblk[kc // 2][0][:, :].rearrange("p (k c) -> p k c", c=512)
                        ins = e.matmul(pm[:, :], MNT[:, kc, mc * 128:(mc + 1) * 128], bv[:, kc % 2, :], start=(kc == 0), stop=(kc == 7))
                    return ins
                T.op("pe", f, R=["G1a", "G1b"] + [b_[1] for b_ in vblk], W=[pres])
                T.op("act", lambda e, pm=pm, mc=mc: e.copy(out=VM[:, mc, :, 0:128], in_=pm[:, :].rearrange("p (h e) -> p h e", e=128)),
                     R=[pres], W=["VM"])
            for j in range(4):
                wdone(("kvv", l, j))

        def phase_N(l, hf):
            t0 = hf * 8
            T.dma("sp", [(GX[:, :], gb_d[l])], W=["GX"])
            for tl in range(8):
                t = t0 + tl
                T.op("act", lambda e, t=t: e.activation(out=G2[:, (t % 2) * 1024:(t % 2) * 1024 + 1024], in_=X[:, t, :], func=AF.Square, accum_out=SS[:, t:t + 1]),
                     R=["X%d" % t], W=["SS"] + (["G2a"] if t % 2 == 0 else ["G2b", "G2c"]))
            rstd_cols(SS[:, t0:t0 + 8], RSTD[:, t0:t0 + 8], 8, ["SS"], ["RSTD"], 1.0 / D, EPS)
            for tl in range(8):
                t = t0 + tl
                norm_transpose(X[:, t, :], "X%d" % t, RSTD[:, t:t + 1], "RSTD", GX, "GX", tl % 2,
                               XNT[:, :, tl * 128:(tl + 1) * 128], "XNT%d" % (tl // 4))

        def phase_O(l, hf, fin=False):
            t0 = hf * 8
            SPXl, sxr = SPX2[:, l % 2, :], "SPX%d" % (l % 2)
            for ch in range(2):
                wob = [ws.get(("wo", l, hf, ch, j)) for j in range(6)]
                wv_ = [b_[0][:, :].rearrange("p (k c) -> p k c", c=512) for b_ in wob]
                for kc in range(4):
                    T.op("dve", lambda e, kc=kc, wv_=wv_: e.tensor_scalar(out=wv_[kc // 2][:, kc % 2, :], in0=wv_[kc // 2][:, kc % 2, :],
                                                                scalar1=SPXl[:, 8 + kc:9 + kc], scalar2=None, op0=ALU.mult),
                         R=[wob[kc // 2][1], sxr], W=[wob[kc // 2][1]], cost=0.25)
                for tl in range(8):
                    t = t0 + tl
                    c0 = tl * 128
                    pm, pres = next_mm()

                    def f(e, pm=pm, c0=c0, wv_=wv_):
                        ins = None
                        for kc in range(12):
                            ins = e.matmul(pm[:, :], YT[:, kc, c0:c0 + 128], wv_[kc // 2][:, kc % 2, :],
                                           start=(kc == 0), stop=(kc == 11))
                        return ins
                    yres = ["YT%s%d" % (b_, tl // 4) for b_ in "abc"]
                    T.op("pe", f, R=yres + [b_[1] for b_ in wob], W=[pres], cost=2.8)
                    T.op("dve", lambda e, pm=pm, t=t, ch=ch: e.tensor_tensor(out=X[:, t, ch * 512:(ch + 1) * 512], in0=pm[:, :],
                                                                            in1=X[:, t, ch * 512:(ch + 1) * 512], op=ALU.add),
                         R=[pres, "X%d" % t], W=["X%d" % t], cost=0.7)
                    if fin and ch == 1:
                        final_tile(t, 8 + tl)
                for j in range(6):
                    wdone(("wo", l, hf, ch, j))

        okeys = []

        def final_tile(t, col):
            if final_norm:
                T.op("act", lambda e: e.activation(out=G2[:, (t % 2) * 1024:(t % 2) * 1024 + 1024], in_=X[:, t, :], func=AF.Square, accum_out=SS[:, t:t + 1]),
                     R=["X%d" % t], W=["SS"] + (["G2a"] if t % 2 == 0 else ["G2b", "G2c"]), cost=1.1)
                T.op("dve", lambda e: e.tensor_scalar(out=TMPS[:, col:col + 1], in0=SS[:, t:t + 1], scalar1=1.0 / D, scalar2=EPS,
                                                      op0=ALU.mult, op1=ALU.add), R=["SS"], W=["TMPS"], cost=0.1)
                T.op("pool", lambda e: e.tensor_tensor(out=RSTD[:, t:t + 1], in0=TMPS[:, col:col + 1], in1=NEGH[:, 0:1], op=ALU.pow),
                     R=["TMPS", "NEGH"], W=["RSTD"], cost=0.7)
                T.op("dve", lambda e: e.scalar_tensor_tensor(out=X[:, t, :], in0=X[:, t, :], scalar=RSTD[:, t:t + 1], in1=GX[:, :],
                                                             op0=ALU.mult, op1=ALU.mult), R=["X%d" % t, "RSTD", "GX"], W=["X%d" % t], cost=1.25)
            T.dma("sp", [(out_d[t * 128:(t + 1) * 128, :], X[:, t, :])], R=["X%d" % t], key="out%d" % t)
            okeys.append("out%d" % t)

        def phase_final(ts, load_g=True):
            n_ = len(ts)
            if final_norm:
                if load_g:
                    T.dma("sp", [(GX[:, :], gb_d[4])], W=["GX"])
                for t in ts:
                    T.op("act", lambda e, t=t: e.activation(out=G2[:, (t % 2) * 1024:(t % 2) * 1024 + 1024], in_=X[:, t, :], func=AF.Square, accum_out=SS[:, t:t + 1]),
                         R=["X%d" % t], W=["SS"] + (["G2a"] if t % 2 == 0 else ["G2b", "G2c"]))
                rstd_cols(SS[:, ts[0]:ts[0] + n_], RSTD[:, ts[0]:ts[0] + n_], n_, ["SS"], ["RSTD"], 1.0 / D, EPS)
                for t in ts:
                    T.op("dve", lambda e, t=t: e.scalar_tensor_tensor(out=X[:, t, :], in0=X[:, t, :], scalar=RSTD[:, t:t + 1], in1=GX[:, :],
                                                                     op0=ALU.mult, op1=ALU.mult), R=["X%d" % t, "RSTD", "GX"], W=["X%d" % t])
            for t in ts:
                T.dma("sp", [(out_d[t * 128:(t + 1) * 128, :], X[:, t, :])], R=["X%d" % t], key="out%d" % t)
                okeys.append("out%d" % t)

        layer_setup(0)
        phase_M(0, preloaded=True)
        ws._issue_upto(11)
        phase_N(0, 0)
        ws._issue_upto(RING - 1)
        for t in range(8, 16):
            T.dma("sp", [(X[:, t, :], x_d[t * 128:(t + 1) * 128, :])], W=["X%d" % t])
        for l in range(n_layers):
            SPXl, sxr = SPX2[:, l % 2, :], "SPX%d" % (l % 2)
            CWv = CWvs[l]
            CBv = SPALL[:, l, 32:40]
            for hf in range(2):
                t0 = hf * 8
                first = (hf == 0)
                if l == 0 and hf == 0:
                    dump("xnt", XNT[:, :, :], ["XNT0", "XNT1"])

                zcb = [ws.get(("zc", l, hf, j)) for j in range(4)]
                qcb = [ws.get(("qc", l, hf, h)) for h in range(4)]
                YC = G0[:, :].rearrange("p (t c) -> p t c", c=512)
                SZC = G1[:, :].rearrange("p (t c) -> p t c", c=512)
                cb_ = [(MM[0], "MM0"), (NUM, "NUM")]
                ci_ = [0]

                def next_c():
                    ci_[0] = (ci_[0] + 1) % 2
                    return cb_[ci_[0]]
                listC = []
                for mt in range(2):
                    xres = "XNT%d" % mt
                    T.begin()
                    for tt in range(4):
                        c0 = mt * 512 + tt * 128
                        pm, pres = next_c()

                        def f(e, pm=pm, c0=c0):
                            ins = None
                            for kc in range(8):
                                bv = zcb[kc // 2][0][:, :].rearrange("p (k c) -> p k c", c=512)
                                ins = e.matmul(pm[:, :], XNT[:, kc, c0:c0 + 128], bv[:, kc % 2, :], start=(kc == 0), stop=(kc == 7))
                            return ins
                        T.op("pe", f, R=[xres] + [b[1] for b in zcb], W=[pres], cost=1.9)
                        th = FT[6 + tt % 2]
                        silu2_from_psum(pm, pres, 512, th, "F%d" % (6 + tt % 2), SZC[:, tt, :], "G1a" if tt < 2 else "G1b")
                    if mt == 1:
                        for j in range(4):
                            T.call(lambda j=j: ws.done(("zc", l, hf, j)))
                    Zl = T.end()
                    Pl, Vl = [], []
                    for h in range(4):
                        T.begin()
                        if h % 2 == 0:
                            PTB, QCB, ptr_, qcr_ = G2[:, 0:1024].rearrange("p (m c) -> p m c", c=512), G2[:, 1024:1536], "G2a", "G2b"
                        else:
                            PTB, QCB, ptr_, qcr_ = G3[:, 0:1024].rearrange("p (m c) -> p m c", c=512), G3[:, 1024:1536], "G3a", "G3b"
                        pm, pres = next_c()
                        T.op("pe", mm_feat(qcb[h][0], (mt * 512, mt * 512 + 512), pm[:, :]), R=[xres, qcb[h][1]], W=[pres], cost=1.9)
                        T.op("act", lambda e, pm=pm, QCB=QCB: e.copy(out=QCB, in_=pm[:, :]), R=[pres], W=[qcr_], cost=0.65)
                        for mc in range(2):
                            pm2, pres2 = next_c()
                            T.op("pe", lambda e, pm2=pm2, mc=mc, h=h, QCB=QCB: e.matmul(pm2[:, :], KMT[:, h, mc * 128:(mc + 1) * 128], QCB, start=True, stop=True),
                                 R=["KMT", qcr_], W=[pres2])
                            T.op("act", lambda e, pm2=pm2, mc=mc, PTB=PTB: e.activation(out=PTB[:, mc, :], in_=pm2[:, :], func=AF.Exp),
                                 R=[pres2], W=[ptr_], cost=0.65)
                        if mt == 1:
                            T.call(lambda h=h: ws.done(("qc", l, hf, h)))
                        Pl.append(T.end())
                        T.begin()
                        for tp in range(2):
                            bank, bres_ = (DCA, "DCA") if tp == 0 else (DCB, "DCB")

                            def f(e, bank=bank, tp=tp, h=h, PTB=PTB):
                                ins = None
                                for i in range(2):
                                    tt = tp * 2 + i
                                    for mc in range(2):
                                        ins = e.matmul(bank[:, i * 129:(i + 1) * 129], PTB[:, mc, tt * 128:(tt + 1) * 128],
                                                       VM[:, mc, h, :], start=(mc == 0), stop=(mc == 1))
                                return ins
                            T.op("pe", f, R=[ptr_, "VM"], W=[bres_], cost=0.5)
                            bv = bank[:, 0:258].rearrange("p (i e) -> p i e", e=129)
                            T.op("dve", lambda e, bv=bv, tp=tp: e.reciprocal(out=RDC[:, 2 * tp:2 * tp + 2], in_=bv[:, :, 128]), R=[bres_], W=["RDC%d" % tp], cost=0.2)
                            for i in range(2):
                                tt = tp * 2 + i
                                T.op("dve", lambda e, bv=bv, i=i, tt=tt, h=h, tp=tp: e.scalar_tensor_tensor(
                                    out=YC[:, tt, h * 128:(h + 1) * 128], in0=bv[:, i, 0:128], scalar=RDC[:, 2 * tp + i:2 * tp + i + 1],
                                    in1=SZC[:, tt, h * 128:(h + 1) * 128], op0=ALU.mult, op1=ALU.mult),
                                    R=[bres_, "RDC%d" % tp, "G1a", "G1b"], W=["G0a" if tt < 2 else "G0b"], cost=0.3)
                        Vl.append(T.end())
                    T.begin()
                    for tt in range(4):
                        c0 = mt * 512 + tt * 128

                        def tr(e, tt=tt):
                            ins = None
                            for h in range(4):
                                ins = e.transpose(PT[:, h * 128:(h + 1) * 128], YC[:, tt, h * 128:(h + 1) * 128], IDB[:, :])
                            return ins
                        T.op("pe", tr, R=["G0a", "G0b", "IDB"], W=["PT"])
                        T.op("act", lambda e, c0=c0: e.copy(out=YT[:, 8:12, c0:c0 + 128], in_=PT[:, 0:512].rearrange("p (h c) -> p h c", c=128)),
                             R=["PT"], W=["YTc%d" % mt])
                    TRl = T.end()
                    listC += Zl + Pl[0] + Pl[1] + Vl[0] + Pl[2] + Vl[1] + Pl[3] + Vl[2] + Vl[3] + TRl

                ub = [ws.get(("u", l, hf, g)) for g in range(4)]
                zbb = [ws.get(("zb", l, hf, g)) for g in range(4)]
                UW, S1, S2, THB, SZB, TMPF = FT[0], FT[1], FT[2], FT[3], FT[4], FT[5]
                Xl, Yl = [], []
                for mt in range(2):
                    xres = "XNT%d" % mt
                    for g in range(4):
                        T.begin()
                        win = 2 << g
                        PB, pbr = XNK[len(Xl) % 2][:, 0:512], "XNK%d" % (len(Xl) % 2)
                        pm, pres = MM[1], "MM1"
                        T.op("pe", mm_feat(ub[g][0], (mt * 512, mt * 512 + 512), pm[:, :]), R=[xres, ub[g][1]], W=[pres], cost=1.9)
                        T.op("dve", lambda e, g=g: e.tensor_copy(out=UW[:, 0:16], in_=HALO_U[:, g, :]), R=["HALO_U"], W=["F0"])
                        T.op("act", lambda e, pm=pm: e.copy(out=UW[:, 16:528], in_=pm[:, :]), R=[pres], W=["F0"], cost=0.65)
                        T.op(BADD, lambda e: e.tensor_tensor(out=S1[:, 1:528], in0=UW[:, 1:528], in1=UW[:, 0:527], op=ALU.add),
                             R=["F0"], W=["F1"], cost=0.72 if BADD == "dve" else 1.1)
                        cur, cres = S1, "F1"
                        oth, ores = S2, "F2"
                        lo, sh = 1, 2
                        for step in range(g):
                            nlo = lo + sh
                            T.op(BADD if step % 2 == 0 else BADD2, lambda e, cur=cur, oth=oth, nlo=nlo, sh=sh: e.tensor_tensor(
                                out=oth[:, nlo:528], in0=cur[:, nlo:528], in1=cur[:, nlo - sh:528 - sh], op=ALU.add),
                                R=[cres], W=[ores], cost=0.72)
                            cur, cres, oth, ores = oth, ores, cur, cres
                            lo, sh = nlo, sh * 2
                        T.op("dve", lambda e, cur=cur, win=win, PB=PB: e.scalar_tensor_tensor(
                            out=PB, in0=cur[:, 16:528], scalar=1.0 / win, in1=UW[:, 16:528], op0=ALU.mult, op1=ALU.subtract),
                            R=[cres, "F0"], W=[pbr], cost=0.7)
                        if first and mt == 0:
                            w1 = win - 1
                            T.op("dve", lambda e, cur=cur, w1=w1: e.tensor_tensor(out=TMPF[:, 0:w1], in0=cur[:, 16:16 + w1], in1=INVC[:, 0:w1], op=ALU.mult),
                                 R=[cres, "CST"], W=["F5"])
                            T.op("dve", lambda e, w1=w1, PB=PB: e.tensor_tensor(out=PB[:, 0:w1], in0=TMPF[:, 0:w1], in1=UW[:, 16:16 + w1], op=ALU.subtract),
                                 R=["F5", "F0"], W=[pbr])
                        T.op("dve", lambda e, g=g: e.tensor_copy(out=HALO_U[:, g, :], in_=UW[:, 512:528]), R=["F0"], W=["HALO_U"])
                        Xl.append(T.end())
                        T.begin()
                        pm2, pres2 = BBP, "BBP"
                        T.op("pe", lambda e, pm2=pm2, g=g, PB=PB: e.matmul(pm2[:, :], PW[:, g, :], PB, start=True, stop=True), R=["PW", pbr], W=[pres2])
                        T.op("pe", mm_feat(zbb[g][0], (mt * 512, mt * 512 + 512), MM[1][:, :]), R=[xres, zbb[g][1]], W=["MM1"], cost=1.9)
                        silu2_from_psum(MM[1], "MM1", 512, THB, "F3", SZB[:, 0:512], "F4")
                        T.op("dve", lambda e, pm2=pm2, g=g, mt=mt: e.scalar_tensor_tensor(
                            out=YT[:, 4 + g, mt * 512:(mt + 1) * 512], in0=pm2[:, :], scalar=SPXl[:, 12 + g:13 + g], in1=SZB[:, 0:512],
                            op0=ALU.mult, op1=ALU.mult), R=[pres2, sxr, "F4"], W=["YTb%d" % mt], cost=0.7)
                        if mt == 1:
                            T.call(lambda g=g: (ws.done(("u", l, hf, g)), ws.done(("zb", l, hf, g))))
                        Yl.append(T.end())
                listB = list(Xl[0])
                for k_ in range(8):
                    if k_ + 1 < 8:
                        listB += Xl[k_ + 1]
                    listB += Yl[k_]
                T.begin()
                def f(e):
                    ins = None
                    for tl in range(8):
                        for kc in range(8):
                            ins = e.matmul(MISC[:, tl * 8:(tl + 1) * 8], XNT[:, kc, tl * 128:(tl + 1) * 128], WG[:, kc, :],
                                           start=(kc == 0), stop=(kc == 7))
                    return ins
                T.op("pe", f, R=["XNT0", "XNT1", "WG"], W=["MISC"])
                T.op("dve", lambda e: e.tensor_tensor(out=GA[:, :, :], in0=MISC[:, 0:64].rearrange("p (t g) -> p t g", g=8),
                                                      in1=SPXl[:, 0:8].unsqueeze(1).to_broadcast([128, 8, 8]), op=ALU.add),
                     R=["MISC", sxr], W=["GA"])
                T.op("act", lambda e: e.activation(out=E1[:, :, :], in_=GA[:, :, 4:8], func=AF.Exp, scale=-1.0), R=["GA"], W=["E1"])
                T.op("act", lambda e: e.activation(out=L1[:, :].rearrange("p (t h) -> p t h", h=4), in_=E1[:, :, :], func=AF.Ln, bias=1.0),
                     R=["E1"], W=["L1"])
                T.op("pe", lambda e: e.matmul(MISC[:, 64:96], U_, L1[:, :], start=True, stop=True), R=["CST", "L1"], W=["MISC"])
                T.op("pe", lambda e: e.matmul(MISC[:, 96:128], LX_, L1[:, :], start=True, stop=True), R=["CST", "L1"], W=["MISC"])
                IGC = GA[:, :, 0:4]
                T.op("dve", lambda e: e.tensor_tensor(out=TB[:, 0:32].rearrange("p (t h) -> p t h", h=4), in0=IGC,
                                                      in1=MISC[:, 64:96].rearrange("p (t h) -> p t h", h=4), op=ALU.add),
                     R=["GA", "MISC"], W=["TB"])
                T.op("dve", lambda e: e.tensor_tensor(out=TB[:, 32:64].rearrange("p (t h) -> p t h", h=4), in0=IGC,
                                                      in1=MISC[:, 96:128].rearrange("p (t h) -> p t h", h=4), op=ALU.subtract),
                     R=["GA", "MISC"], W=["TB"])
                T.op("act", lambda e: e.activation(out=EXPB[:, :], in_=TB[:, 0:32], func=AF.Exp), R=["TB"], W=["EXPB"])
                T.op("act", lambda e: e.activation(out=WK[:, :], in_=TB[:, 32:64], func=AF.Exp), R=["TB"], W=["WK"])
                T.op("act", lambda e: e.activation(out=EBT[:, :], in_=MISC[:, 64:96], func=AF.Exp, scale=-1.0), R=["MISC"], W=["EBT"])
                T.op("act", lambda e: e.activation(out=IEB[:, :], in_=MISC[:, 64:96], func=AF.Exp), R=["MISC"], W=["IEB"])
                T.op("pe", lambda e: e.matmul(MISC[:, 128:160], SEL_, EBT[:, :], start=True, stop=True), R=["CST", "EBT"], W=["MISC"])
                T.op("act", lambda e: e.copy(out=DEC[:, :], in_=MISC[:, 128:160]), R=["MISC"], W=["DEC"])

                listG = T.end()
                T.emit([listC, listB, listG])
                if l == 0 and hf == 0:
                    dump("ytc", YT[:, 8:12, :], ["YTc0", "YTc1"])
                if l == 0 and hf == 0:
                    dump("ytb", YT[:, 4:8, :], ["YTb0", "YTb1"])

                QTm = [G0[:, 0:1024].rearrange("p (h c) -> p h c", c=512), G2[:, 0:1024].rearrange("p (h c) -> p h c", c=512)]
                KTm = [G0[:, 1024:2048].rearrange("p (h c) -> p h c", c=512), G2[:, 1024:2048].rearrange("p (h c) -> p h c", c=512)]
                QTr = ["G0a", "G2a"]
                KTr = [["G0b"], ["G2b", "G2c"]]
                SZAm = [G1[:, 1024:2048].rearrange("p (h c) -> p h c", c=512), G1[:, 0:1024].rearrange("p (h c) -> p h c", c=512)]
                SZAr = ["G1b", "G1a"]
                wts = {}

                def fetch_weights(hp, with_vo=True):
                    wts[hp] = ([ws.get(("q", l, hf, 2 * hp + hh)) for hh in range(2)],
                               [ws.get(("k", l, hf, 2 * hp + hh)) for hh in range(2)],
                               [ws.get(("za", l, hf, 2 * hp + hh)) for hh in range(2)],
                               [ws.get(("vo", l, hf, hp, j)) for j in range(4)] if with_vo else None)

                def build_dg(hp, bps=(0, 1, 2, 3)):
                    for bp in bps:
                        blk_i = 4 * (bp % 2) + 2 * hp + bp // 2
                        T.op("dve", lambda e, bp=bp, blk_i=blk_i: e.tensor_tensor(
                            out=DG[:, bp, :, :], in0=IDB[:, :].unsqueeze(1).to_broadcast([128, 4, 128]),
                            in1=CWv[:, blk_i, :].unsqueeze(2).to_broadcast([128, 4, 128]), op=ALU.mult),
                            R=["IDB", "SPALL"], W=["DG%d" % bp], cost=0.7)

                def prepass_block(hp, mt, bidx):
                    qb, kb, zab, vob = wts[hp]
                    xres = "XNT%d" % mt
                    mp = mt % 2
                    if bidx < 4:
                        hh, isk = bidx // 2, bidx % 2
                        h = 2 * hp + hh
                        blk_i = 4 * isk + h
                        wb = (kb if isk else qb)[hh]
                        RAWB, rres = XNK[bidx % 2], "XNK%d" % (bidx % 2)
                        TH, tres = FT[4 + bidx % 2], "F%d" % (4 + bidx % 2)
                        pm, pres = MM[1], "MM1"
                        T.op("pe", mm_feat(wb[0], (mt * 512, mt * 512 + 512), pm[:, :]), R=[xres, wb[1]], W=[pres], cost=1.9)
                        T.op("dve", lambda e: e.tensor_copy(out=RAWB[:, 0:3], in_=HALOB[:, blk_i, 0:3]), R=["HALOB"], W=[rres], cost=0.1)
                        T.op("act", lambda e: e.copy(out=RAWB[:, 3:515], in_=pm[:, :]), R=[pres], W=[rres], cost=0.65)
                        T.op("act", lambda e: e.copy(out=HALOB[:, blk_i, 0:3], in_=RAWB[:, 512:515]), R=[rres], W=["HALOB"], cost=0.2)
                        pc, pcres = DCB, "DCB"

                        def f(e):
                            ins = None
                            for j in range(4):
                                ins = e.matmul(pc[:, :], DG[:, bidx, j, :], RAWB[:, j:j + 512], start=(j == 0), stop=(j == 3))
                            return ins
                        T.op("pe", f, R=["DG%d" % bidx, rres], W=[pcres], cost=0.95)
                        dstv = (KTm if isk else QTm)[mp][:, hh, :]
                        dstres = KTr[mp] if isk else [QTr[mp]]
                        cbv = CBv
                        T.op("act", lambda e: e.activation(out=dstv, in_=pc[:, 0:512], func=AF.Silu, bias=cbv[:, blk_i:blk_i + 1]),
                             R=[pcres, "SPALL"], W=dstres, cost=0.65)
                    else:
                        hh = bidx - 4
                        pm, pres = MM[1], "MM1"
                        T.op("pe", mm_feat(zab[hh][0], (mt * 512, mt * 512 + 512), pm[:, :]), R=[xres, zab[hh][1]], W=[pres], cost=1.9)
                        T.op("act", lambda e: e.activation(out=SZAm[mp][:, hh, :], in_=pm[:, 0:512], func=AF.Silu),
                             R=[pres], W=[SZAr[mp]], cost=0.65)

                def front(g):
                    hp, i = g // 8, g % 8
                    qb, kb, zab, vob = wts[hp]
                    mt, tt = i // 4, i % 4
                    mp = mt % 2
                    p = g % 2
                    xres = "XNT%d" % mt
                    QT, KT = QTm[mp], KTm[mp]
                    qres, kres = QTr[mp], KTr[mp]
                    c0 = i * 128
                    cm = tt * 128
                    vb, vbr = VB[p], "VB%d" % p
                    T.op("act", lambda e: e.copy(out=CBA[p][:, :, :], in_=C32[:, 2 * hp:2 * hp + 2, :]), R=["C32"], W=["CBA%d" % p])
                    pm, pres = MM[0], "MM0"

                    def f(e):
                        ins = None
                        for kc in range(8):
                            bv = vob[kc // 2][0][:, :].rearrange("p (k c) -> p k c", c=512)
                            ins = e.matmul(pm[:, :], XNT[:, kc, c0:c0 + 128], bv[:, kc % 2, :], start=(kc == 0), stop=(kc == 7))
                        return ins
                    T.op("pe", f, R=[xres] + [b_[1] for b_ in vob], W=[pres], cost=1.9)
                    gi = [i * 4 + 2 * hp + hh for hh in range(2)]

                    def tr(e):
                        ins = None
                        for hh in range(2):
                            ins = e.transpose(PT[:, hh * 128:(hh + 1) * 128], KT[:, hh, cm:cm + 128], IDB[:, :])
                        return ins
                    T.op("pe", tr, R=kres + ["IDB"], W=["PT"])
                    T.op("act", lambda e: e.copy(out=vb[:, :, 0:128], in_=pm[:, 0:256].rearrange("p (h e) -> p h e", e=128)),
                         R=[pres], W=[vbr])
                    T.op("act", lambda e: e.activation(out=THO[p][:, :], in_=pm[:, 256:512], func=AF.Tanh, scale=0.5),
                         R=[pres], W=["THO%d" % p])
                    if KW_ACT:
                        for hh in range(2):
                            T.op("act", lambda e, hh=hh: e.activation(out=KW[:, hh, :], in_=PT[:, hh * 128:(hh + 1) * 128], func=AF.Copy,
                                                                     scale=WK[:, gi[hh]:gi[hh] + 1]),
                                 R=["PT", "WK"], W=["KW%d" % hh], cost=0.3)
                    else:
                        T.op("dve", lambda e: e.tensor_tensor(
                            out=KW[:, :, :], in0=PT[:, 0:256].rearrange("p (h c) -> p h c", c=128),
                            in1=WK[:, gi[0]:gi[0] + 2].unsqueeze(2).to_broadcast([128, 2, 128]), op=ALU.mult),
                            R=["PT", "WK"], W=["KW0", "KW1"], cost=0.42)

                    def f(e):
                        ins = None
                        for hh in range(2):
                            ins = e.matmul(DCA[:, hh * 129:(hh + 1) * 129], KW[:, hh, :], vb[:, hh, :], start=True, stop=True)
                        return ins
                    T.op("pe", f, R=["KW0", "KW1", vbr], W=["DCA"], cost=0.35)

                    def f(e):
                        ins = None
                        for hh in range(2):
                            ins = e.matmul(BBP[:, hh * 128:(hh + 1) * 128], KT[:, hh, cm:cm + 128], QT[:, hh, cm:cm + 128], start=True, stop=True)
                        return ins
                    T.op("pe", f, R=kres + [qres], W=["BBP"], cost=0.3)
                    for hh in range(2):
                        h = 2 * hp + hh
                        T.op("dve", lambda e, hh=hh, h=h: e.scalar_tensor_tensor(
                            out=C32[:, h, :], in0=C32[:, h, :], scalar=DEC[:, gi[hh]:gi[hh] + 1],
                            in1=DCA[:, hh * 129:(hh + 1) * 129], op0=ALU.mult, op1=ALU.add), R=["C32", "DEC", "DCA"], W=["C32"], cost=0.36)
                    for hh in range(2):
                        T.op("dve", lambda e, hh=hh, g_=gi[hh]: e.scalar_tensor_tensor(
                            out=STB[p][:, hh * 128:(hh + 1) * 128], in0=BBP[:, hh * 128:(hh + 1) * 128], scalar=EXPB[:, g_:g_ + 1],
                            in1=U_, op0=ALU.mult, op1=ALU.mult), R=["BBP", "EXPB", "CST"], W=["STB%d" % p], cost=0.27)

                def back_a(g):
                    hp, i = g // 8, g % 8
                    p = g % 2
                    vb, vbr = VB[p], "VB%d" % p
                    mp = (i // 4) % 2
                    QT, qres = QTm[mp], QTr[mp]
                    cm = (i % 4) * 128
                    gi0 = i * 4 + 2 * hp

                    def f(e):
                        ins = None
                        for hh in range(2):
                            o_ = NUM[:, hh * 129:(hh + 1) * 129]
                            e.matmul(o_, STB[p][:, hh * 128:(hh + 1) * 128], vb[:, hh, :], start=True, stop=False)
                            ins = e.matmul(o_, QT[:, hh, cm:cm + 128], CBA[p][:, hh, :], start=False, stop=True)
                        return ins
                    T.op("pe", f, R=["STB%d" % p, vbr, qres, "CBA%d" % p], W=["NUM"], cost=0.5)
                    NV = NUM[:, 0:258].rearrange("p (h e) -> p h e", e=129)
                    T.op("act", lambda e: e.activation(out=SM[:, 0:2], in_=NV[:, :, 128], func=AF.Abs), R=["NUM"], W=["SM"])
                    T.op("dve", lambda e: e.scalar_tensor_tensor(
                        out=TTt[:, :].rearrange("p (h e) -> p h e", e=128), in0=THO[p][:, :].rearrange("p (h e) -> p h e", e=128),
                        scalar=1.0, in1=NV[:, :, 0:128], op0=ALU.add, op1=ALU.mult), R=["THO%d" % p, "NUM"], W=["TTt0", "TTt1"])
                    T.op("dve", lambda e: e.tensor_tensor(out=SM[:, 2:4], in0=SM[:, 0:2], in1=IEB[:, gi0:gi0 + 2], op=ALU.max),
                         R=["SM", "IEB"], W=["SMb"], cost=0.1)
                    T.op("dve", lambda e: e.tensor_tensor(out=SM[:, 4:6], in0=SM[:, 2:4], in1=SM[:, 2:4], op=ALU.mult), R=["SMb"], W=["SMc"], cost=0.1)
                    for hh in range(2):
                        T.op("act", lambda e, hh=hh: e.activation(out=HGt[:, hh * 128:(hh + 1) * 128], in_=TTt[:, hh * 128:(hh + 1) * 128], func=AF.Square,
                                                                 scale=128 ** -0.5, accum_out=SM[:, 6 + hh:7 + hh]),
                             R=["TTt%d" % hh], W=["SMd%d" % hh, "HGt%d" % hh])
                    T.op("dve", lambda e: e.scalar_tensor_tensor(out=SM[:, 8:10], in0=SM[:, 4:6], scalar=4 * EPS, in1=SM[:, 6:8],
                                                                 op0=ALU.mult, op1=ALU.add), R=["SMc", "SMd0", "SMd1"], W=["SMe"], cost=0.1)
                    T.op("pool", lambda e: e.tensor_tensor(out=SM[:, 10:12], in0=SM[:, 8:10], in1=NEGH[:, 0:2], op=ALU.pow),
                         R=["SMe", "NEGH"], W=["SMf"], cost=0.7)
                    T.op("dve", lambda e: e.tensor_tensor(
                        out=YTK[p][:, :].rearrange("p (h e) -> p h e", e=128), in0=TTt[:, :].rearrange("p (h e) -> p h e", e=128),
                        in1=SM[:, 10:12].unsqueeze(2).to_broadcast([128, 2, 128]), op=ALU.mult),
                        R=["TTt0", "TTt1", "SMf"], W=["YTK%d" % p], cost=0.42)

                def back_b(g):
                    hp, i = g // 8, g % 8
                    mt, tt = i // 4, i % 4
                    mp = mt % 2
                    p = g % 2
                    c0 = i * 128
                    cm = tt * 128

                    def tr(e):
                        ins = None
                        for hh in range(2):
                            ins = e.transpose(PT[:, 256 + hh * 128:384 + hh * 128], YTK[p][:, hh * 128:(hh + 1) * 128], IDB[:, :])
                        return ins
                    T.op("pe", tr, R=["YTK%d" % p, "IDB"], W=["PT"])
                    T.op("dve", lambda e: e.tensor_tensor(
                        out=YT[:, 2 * hp:2 * hp + 2, c0:c0 + 128], in0=PT[:, 256:512].rearrange("p (h c) -> p h c", c=128),
                        in1=SZAm[mp][:, :, cm:cm + 128], op=ALU.mult), R=["PT", SZAr[mp]], W=["YTa%d" % mt])

                def rec(fn, *a):
                    T.begin()
                    fn(*a)
                    return T.end()

                fetch_weights(0)
                ws.throttle = True
                build_dg(0)
                for bidx in range(6):
                    prepass_block(0, 0, bidx)
                front(0)
                for g in range(16):
                    hp, i = g // 8, g % 8
                    lists = []
                    if g == 7:
                        fetch_weights(1)
                    if g + 1 < 16:
                        lists.append(rec(front, g + 1))
                    lists.append(rec(back_a, g))
                    if g >= 1:
                        lists.append(rec(back_b, g - 1))
                    if i < 3:
                        lists.append(rec(lambda: (prepass_block(hp, 1, 2 * i), prepass_block(hp, 1, 2 * i + 1),
                                                  (build_dg(1, (0, 1)) if (hp == 0 and i == 2) else None))))
                    elif hp == 0 and i == 3:
                        lists.append(rec(lambda: build_dg(1, (2, 3))))
                    elif hp == 0 and 4 <= i < 7:
                        if i == 4:
                            fetch_weights(1, with_vo=False)
                        k_ = i - 4
                        lists.append(rec(lambda: (prepass_block(1, 0, 2 * k_), prepass_block(1, 0, 2 * k_ + 1))))

                    T.emit(lists)
                    if i == 3:
                        for hh in range(2):
                            ws.done(("q", l, hf, 2 * hp + hh))
                            ws.done(("k", l, hf, 2 * hp + hh))
                        for hh in range(2):
                            ws.done(("za", l, hf, 2 * hp + hh))
                    if i == 7:
                        for j in range(4):
                            ws.done(("vo", l, hf, hp, j))
                    ws.pump(PUMP_N)
                T.emit([rec(back_b, 15)])
                ws.throttle = False
                ws._issue_upto(ws.limit)
                if l == 0 and hf == 0:
                    dump("yta", YT[:, 0:4, :], ["YTa0", "YTa1"])

                last = (hf == 1 and l + 1 == n_layers)
                if last and final_norm:
                    T.dma("sp", [(GX[:, :], gb_d[4])], W=["GX"])
                T.begin()
                phase_O(l, hf, fin=last)
                LO = T.end()
                T.begin()
                if hf == 0:
                    phase_N(l, 1)
                elif l + 1 < n_layers:
                    layer_setup(l + 1)
                    phase_N(l + 1, 0)
                else:
                    phase_final(list(range(0, 8)), load_g=False)
                LN = T.end()
                T.emit([LO, LN])
                flush_done()
                if hf == 1 and l + 1 < n_layers:
                    phase_M(l + 1)
        T.finish(okeys + ["dbg_" + n for n in dbg_d])
    return nc


def host_constants():
    idx = np.arange(128)
    same = (idx[:, None] // 64) == (idx[None, :] // 64)
    ident = np.eye(128, dtype=np.float32)
    U = (idx[:, None] <= idx[None, :]).astype(np.float32)
    LX = (idx[:, None] > idx[None, :]).astype(np.float32)
    invc = np.broadcast_to(1.0 / np.arange(1, 17, dtype=np.float32), (128, 16))
    lnc8 = np.broadcast_to(np.array([LNC] * 4 + [0.0] * 4, dtype=np.float32), (128, 8))
    sel = np.zeros((128, 128), np.float32)
    sel[127, :] = 1.0
    return np.ascontiguousarray(np.concatenate([ident, U, LX, invc, lnc8, sel], axis=1).astype(np.float32))


def host_layout(norm_g, b_gates, conv_w, conv_b, mlstm_norm_g, pool_scale, mem_norm_g, final_norm_g):
    Ln = norm_g.shape[0]
    sp = np.zeros((128, Ln, 56), np.float32)
    for l in range(Ln):
        cw = conv_w[l].reshape(4, 8, 128)
        sp[:, l, 0:32] = cw.transpose(2, 1, 0).reshape(128, 32)
        sp[:, l, 32:40] = conv_b[l].reshape(8, 128).T
        sp[:, l, 40:44] = mlstm_norm_g[l].reshape(4, 128).T
        sp[:, l, 44:48] = pool_scale[l].reshape(4, 128).T
        sp[:, l, 48:56] = np.broadcast_to(b_gates[l], (128, 8))
    gb = np.stack([np.broadcast_to(v, (128, D)) for v in (norm_g[0], norm_g[1], mem_norm_g[0], mem_norm_g[1], final_norm_g,
                                                            conv_b[0], conv_b[1])])
    return sp, np.ascontiguousarray(gb.astype(np.float32))


_CACHE = {}


def kernel(x, mem, norm_g, w_in, b_gates, conv_w, conv_b, mlstm_norm_g, pool_w, pool_scale,
           mem_norm_g, w_mem_kv, w_out, final_norm_g):
    f = lambda a: np.ascontiguousarray(np.asarray(a, dtype=np.float32))
    x, mem, w_in, w_mem_kv, w_out, pool_w = f(x), f(mem), f(w_in), f(w_mem_kv), f(w_out), f(pool_w)
    sp, gb = host_layout(f(norm_g), f(b_gates), f(conv_w), f(conv_b), f(mlstm_norm_g), f(pool_scale), f(mem_norm_g), f(final_norm_g))
    cst = host_constants()
    if "nc" not in _CACHE:
        _CACHE["nc"] = build_program()
    nc = _CACHE["nc"]
    n = x.shape[0]
    in_maps = [{"x": x[b], "mem": mem[b], "w_in": w_in, "w_mem_kv": w_mem_kv, "w_out": w_out, "pool_w": pool_w,
                "smallp": sp, "gb": gb, "cst": cst} for b in range(n)]
    res = run_bass_kernel_spmd(nc, in_maps, core_ids=list(range(n)))
    return np.stack([np.asarray(r["out"], dtype=np.float32) for r in res.results], axis=0)
```

```python
import contextlib
import math
import numpy as np
import concourse.bass as bass
import concourse.mybir as mybir
from concourse.bass_utils import run_bass_kernel_spmd

F32 = mybir.dt.float32
BF16 = mybir.dt.bfloat16
AF = mybir.ActivationFunctionType
ALU = mybir.AluOpType

S = 2048
D = 1024
NIN = 4616
NMEM = 256
EPS = 1e-6
O_QK, O_V, O_O, O_ZA, O_G, O_U, O_ZB, O_QC, O_ZC = 0, 1024, 1536, 2048, 2560, 2568, 3080, 3592, 4104
LNC = math.log(128 ** -0.5)
RING = 16
import os
PSUM_RES = {"MM0", "MM1", "PT", "BBP", "DCA", "DCB", "NUM", "MISC"}
WS_LAG = int(os.environ.get("WS_LAG", "0"))
PUMP_N = int(os.environ.get("PUMP_N", "2"))
BADD = os.environ.get("BADD", "dve")
BADD2 = os.environ.get("BADD2", "dve")
KW_ACT = bool(int(os.environ.get("KW_ACT", "0")))
SEQ_EMIT = bool(int(os.environ.get("SEQ_EMIT", "0")))


class Tracker:
    def __init__(self, nc, es):
        self.nc = nc
        self.es = es
        self.eng = {"pe": nc.tensor, "act": nc.scalar, "dve": nc.vector, "pool": nc.gpsimd, "sp": nc.sync}
        self.sem = {}
        self.cnt = {}
        self.known = {k: {} for k in self.eng}
        for k in self.eng:
            self.sem[k] = es.enter_context(nc.semaphore("s_" + k))
            self.cnt[k] = 0
        self.lastw = {}
        self.readers = {}
        self.nwait = 0

    def _key(self, key):
        if key not in self.sem:
            self.sem[key] = self.es.enter_context(self.nc.semaphore("s_" + key.replace(":", "_")))
            self.cnt[key] = 0
        return self.sem[key]

    def _deps(self, ename, R, W):
        deps = {}

        def add(k, v, war=False):
            if war and k == ename and ename == "pe":
                return
            if v > deps.get(k, 0):
                deps[k] = v

        for r in R:
            if r in self.lastw:
                add(*self.lastw[r])
        for w in W:
            if w in self.lastw:
                add(*self.lastw[w])
            for k, v in self.readers.get(w, {}).items():
                add(k, v, war=True)
        return deps

    def _wait(self, ename, deps):
        e = self.eng[ename]
        for k, v in deps.items():
            if k == ename and ename == "pe":
                continue
            if self.known[ename].get(k, 0) >= v:
                continue
            e.wait_ge(self.sem[k], v)
            self.nwait += 1
            self.known[ename][k] = v

    def _commit(self, key, val, R, W):
        for r in R:
            self.readers.setdefault(r, {})[key] = val
        for w in W:
            self.lastw[w] = (key, val)
            self.readers[w] = {}

    def begin(self):
        self.rec = []

    def end(self):
        r, self.rec = self.rec, None
        return r

    DEF_COST = {"pe": 0.35, "act": 0.42, "dve": 0.33, "pool": 0.5, "sp": 0.1}
    SYNC = float(os.environ.get("M_SYNC", "0.15"))

    def _m_norm(self, R, W):
        W2 = tuple(W) + tuple(r for r in R if r in PSUM_RES)
        R2 = tuple(r for r in R if r not in PSUM_RES)
        return R2, W2

    def _m_start(self, ename, R, W):
        if not hasattr(self, "m_free"):
            self.m_free, self.m_w, self.m_r, self.m_we = {}, {}, {}, {}
        t = self.m_free.get(ename, 0.0)
        for r in R:
            if r in self.m_w:
                t = max(t, self.m_w[r] + (self.SYNC if self.m_we.get(r) != ename else 0.05))
        for w in W:
            if w in self.m_w:
                t = max(t, self.m_w[w] + (self.SYNC if self.m_we.get(w) != ename else 0.0))
            if w in self.m_r:
                t = max(t, self.m_r[w] + self.SYNC)
        return t

    def _m_commit(self, ename, R, W, cost):
        R, W = self._m_norm(R, W)
        st = self._m_start(ename, R, W)
        en = st + cost
        self.m_free[ename] = en
        for r in R:
            self.m_r[r] = max(self.m_r.get(r, 0.0), en)
        for w in W:
            self.m_w[w] = en
            self.m_r[w] = 0.0
            self.m_we[w] = ename

    def emit(self, lists):
        lists = [l_ for l_ in lists if l_]
        if SEQ_EMIT:
            lists = [[x for l_ in lists for x in l_]]
        idx = [0] * len(lists)
        while True:
            best, bk = None, None
            for k, l_ in enumerate(lists):
                if idx[k] < len(l_):
                    kind, a = l_[idx[k]]
                    if kind == "op":
                        R, W = self._m_norm(a[2], a[3])
                        st = self._m_start(a[0], R, W)
                    elif kind == "call":
                        st = -1.0
                    elif kind == "wait":
                        if not a():
                            continue
                        st = -2.0
                    else:
                        st = self._m_start(a[0], (), ())
                    key = (st, (idx[k] + 0.5) / len(l_))
                    if best is None or key < best:
                        best, bk = key, k
            if bk is None:
                assert all(idx[k] >= len(l_) for k, l_ in enumerate(lists)), "emit: all remaining lists blocked"
                break
            kind, a = lists[bk][idx[bk]]
            idx[bk] += 1
            if kind == "op":
                self.op(*a)
            elif kind == "call":
                a()
            elif kind == "wait":
                pass
            else:
                self.dma(*a)

    def waitfor(self, pred):
        if getattr(self, "rec", None) is not None:
            self.rec.append(("wait", pred))
        else:
            assert pred()

    def call(self, fn):
        if getattr(self, "rec", None) is not None:
            self.rec.append(("call", fn))
        else:
            fn()

    def op(self, ename, fn, R=(), W=(), cost=None):
        if cost is None:
            cost = self.DEF_COST[ename]
        if getattr(self, "rec", None) is not None:
            self.rec.append(("op", (ename, fn, tuple(R), tuple(W), cost)))
            return
        self._m_commit(ename, R, W, cost)
        W = tuple(W) + tuple(r for r in R if r in PSUM_RES)
        R = tuple(r for r in R if r not in PSUM_RES)
        self._wait(ename, self._deps(ename, R, W))
        ins = fn(self.eng[ename])
        self.cnt[ename] += 1
        ins.then_inc(self.sem[ename], 1)
        self._commit(ename, self.cnt[ename], R, W)

    def dma(self, qname, pairs, R=(), W=(), key=None):
        if getattr(self, "rec", None) is not None:
            self.rec.append(("dma", (qname, pairs, tuple(R), tuple(W), key)))
            return
        key = "dma:" + (key or (W[0] if W else R[0]))
        self._m_commit(key, R, W, 2.5)
        sem = self._key(key)
        self._wait(qname, self._deps(qname, R, W))
        for (o, i) in pairs:
            self.eng[qname].dma_start(out=o, in_=i).then_inc(sem, 16)
            self.cnt[key] += 16
        self._commit(key, self.cnt[key], R, W)

    def finish(self, keys):
        for k in keys:
            k = "dma:" + k
            if k in self.cnt and self.cnt[k] > 0:
                self.eng["sp"].wait_ge(self.sem[k], self.cnt[k])


class WStream:
    def __init__(self, T, slots):
        self.T = T
        self.slots = slots
        self.blocks = []
        self.index = {}
        self.issued = 0

    def plan(self, key, fn):
        self.index[key] = len(self.blocks)
        self.blocks.append(fn)

    def _issue_upto(self, i):
        while self.issued <= i and self.issued < len(self.blocks):
            j = self.issued
            s = j % len(self.slots)
            pairs = self.blocks[j](self.slots[s])
            self.T.dma("pool", pairs, W=["W%d" % s], key="W%d" % s)
            self.issued += 1

    def start(self):
        self._issue_upto(len(self.slots) - 1)

    def get(self, key):
        i = self.index[key]
        self._issue_upto(i)
        s = i % len(self.slots)
        return self.slots[s], "W%d" % s

    def peek(self, key):
        i = self.index[key]
        s_ = i % len(self.slots)
        return self.slots[s_], "W%d" % s_

    def can_issue(self, key):
        i = self.index[key]
        return i < self.issued or i < len(self.slots) or i <= getattr(self, "limit", -1)

    def get_safe(self, key):
        T = self.T
        if getattr(T, "rec", None) is None:
            return self.get(key)
        i = self.index[key]
        T.waitfor(lambda: self.can_issue(key))
        T.call(lambda: self._issue_upto(i))
        return self.peek(key)

    def done(self, key):
        i = self.index[key]
        if not hasattr(self, "done_set"):
            self.done_set, self.first_undone = set(), 0
        self.done_set.add(i)
        while self.first_undone in self.done_set:
            self.first_undone += 1
        self.limit = max(getattr(self, "limit", -1), self.first_undone + len(self.slots) - 1 - WS_LAG)
        if not getattr(self, "throttle", False):
            self._issue_upto(self.limit)

    def pump(self, n):
        lim = min(getattr(self, "limit", -1), self.issued + n - 1)
        self._issue_upto(lim)


def build_program(n_layers=2, final_norm=True, dbg=None):
    dbg = dbg or {}
    nc = bass.Bass("TRN2", target_bir_lowering=False)
    L = 2
    x_d = nc.dram_tensor("x", [S, D], F32, kind="ExternalInput").ap()
    mem_d = nc.dram_tensor("mem", [NMEM, D], F32, kind="ExternalInput").ap()
    w_in_d = nc.dram_tensor("w_in", [L, D, NIN], F32, kind="ExternalInput").ap()
    w_kv_d = nc.dram_tensor("w_mem_kv", [L, D, 1024], F32, kind="ExternalInput").ap()
    w_out_d = nc.dram_tensor("w_out", [L, 1536, D], F32, kind="ExternalInput").ap()
    pool_w_d = nc.dram_tensor("pool_w", [L, 4, 128, 128], F32, kind="ExternalInput").ap()
    smallp_d = nc.dram_tensor("smallp", [128, L, 56], F32, kind="ExternalInput").ap()
    gb_d = nc.dram_tensor("gb", [7, 128, D], F32, kind="ExternalInput").ap()
    cst_d = nc.dram_tensor("cst", [128, 536], F32, kind="ExternalInput").ap()
    out_d = nc.dram_tensor("out", [S, D], F32, kind="ExternalOutput").ap()
    dbg_d = {}
    for name, shape in dbg.items():
        dbg_d[name] = nc.dram_tensor("dbg_" + name, list(shape), F32, kind="ExternalOutput").ap()

    with contextlib.ExitStack() as es:
        T = Tracker(nc, es)

        def sb(name, shape, dt):
            return es.enter_context(nc.sbuf_tensor(name, list(shape), dt))

        def ps(name, shape, dt):
            return es.enter_context(nc.psum_tensor(name, list(shape), dt))

        X = sb("X", [128, 16, D], F32)
        XNT = sb("XNT", [128, 8, 1024], BF16)
        YT = sb("YT", [128, 12, 1024], BF16)
        slots = [sb("W%d" % i, [128, 1024], BF16) for i in range(RING)]
        FBIG = sb("FBIG", [128, 8 * 528], F32)
        FT = [FBIG[:, i * 528:(i + 1) * 528] for i in range(8)]
        G0 = sb("G0", [128, 2048], BF16)
        G1 = sb("G1", [128, 2048], BF16)
        G2 = sb("G2", [128, 2048], BF16)
        XNK = [sb("XNK%d" % i, [128, 1024], BF16) for i in range(2)]
        MEMB = FBIG[:, 0:1024]
        CST = sb("CST", [128, 536], F32)
        IDB = sb("IDB", [128, 128], BF16)
        ONESF = sb("ONESF", [128, 128], F32)
        NEGH = sb("NEGH", [128, 32], F32)
        GX = sb("GX", [128, D], F32)
        GM = GX
        DG = sb("DG", [128, 4, 4, 128], BF16)
        CBROW = sb("CBROW", [1, 1024], BF16)
        ONESROW = sb("ONESROW", [1, 512], BF16)
        HALOB = sb("HALOB", [128, 8, 4], BF16)
        SPALL = sb("SPALL", [128, L, 56], F32)
        SPX2 = sb("SPX", [128, 2, 24], F32)
        PW = sb("PW", [128, 4, 128], BF16)
        WG = sb("WG", [128, 8, 8], BF16)
        KMT = sb("KMT", [128, 4, 256], BF16)
        VM = sb("VM", [128, 2, 4, 129], BF16)
        SS = sb("SS", [128, 16], F32)
        RSTD = sb("RSTD", [128, 16], F32)
        TMPS = sb("TMPS", [128, 16], F32)
        MSS = sb("MSS", [128, 8], F32)
        HALO_U = sb("HALO_U", [128, 4, 16], F32)
        GA = sb("GA", [128, 8, 8], F32)
        E1 = sb("E1", [128, 8, 4], F32)
        L1 = sb("L1", [128, 32], F32)
        TB = sb("TB", [128, 64], F32)
        EXPB = sb("EXPB", [128, 32], F32)
        WK = sb("WK", [128, 32], F32)
        VB = [sb("VB%d" % i, [128, 2, 129], BF16) for i in range(2)]
        THO = [sb("THO%d" % i, [128, 256], F32) for i in range(2)]
        EBT = sb("EBT", [128, 32], F32)
        IEB = sb("IEB", [128, 32], F32)
        DEC = sb("DEC", [128, 32], F32)
        STB = [sb("STB%d" % i, [128, 256], BF16) for i in range(2)]
        KW = sb("KW", [128, 2, 128], BF16)
        C32 = sb("C32", [128, 4, 129], F32)
        CBA = [sb("CBA%d" % i, [128, 2, 129], BF16) for i in range(2)]
        TTt = sb("TTt", [128, 256], F32)
        HGt = sb("HGt", [128, 256], F32)
        G3 = sb("G3", [128, 1536], BF16)
        SM = sb("SM", [128, 16], F32)
        YTK = [sb("YTK%d" % i, [128, 256], BF16) for i in range(2)]
        RDC = sb("RDC", [128, 4], F32)
        MM = [ps("MM0", [128, 512], F32), ps("MM1", [128, 512], F32)]
        PT = ps("PT", [128, 1024], BF16)
        BBP = ps("BBP", [128, 512], F32)
        DCA = ps("DCA", [128, 512], F32)
        DCB = ps("DCB", [128, 512], F32)
        NUM = ps("NUM", [128, 512], F32)
        MISC = ps("MISC", [128, 512], F32)
        mmi = [0]

        def next_mm():
            mmi[0] ^= 1
            return MM[mmi[0]], "MM%d" % mmi[0]

        U_ = CST[:, 128:256]
        LX_ = CST[:, 256:384]
        INVC = CST[:, 384:400]
        LNC8 = CST[:, 400:408]
        SEL_ = CST[:, 408:536]

        ws = WStream(T, slots)

        def wv(wd, l):
            return wd[l].rearrange("(kc p) n -> p kc n", p=128)

        def feat_block(wd, l, c0):
            def f(slot):
                return [(slot[:, :].rearrange("p (k c) -> p k c", c=128), wv(wd, l)[:, :, c0:c0 + 128])]
            return f

        def mov_block(wd, l, j, c0):
            def f(slot):
                return [(slot[:, :].rearrange("p (k c) -> p k c", c=512), wv(wd, l)[:, 2 * j:2 * j + 2, c0:c0 + 512])]
            return f

        def vo_block(l, j, hp):
            def f(slot):
                v = slot[:, :].rearrange("p (k c) -> p k c", c=512)
                w = wv(w_in_d, l)
                return [(v[:, :, 0:256], w[:, 2 * j:2 * j + 2, O_V + hp * 256:O_V + hp * 256 + 256]),
                        (v[:, :, 256:512], w[:, 2 * j:2 * j + 2, O_O + hp * 256:O_O + hp * 256 + 256])]
            return f

        def wo_block(l, kc):
            def f(slot):
                return [(slot[:, :], w_out_d[l, kc * 128:(kc + 1) * 128, :])]
            return f

        for l in range(n_layers):
            for h in range(4):
                ws.plan(("kvk", l, h), feat_block(w_kv_d, l, h * 128))
            for j in range(4):
                ws.plan(("kvv", l, j), mov_block(w_kv_d, l, j, 512))
            for hf in range(2):
                for j in range(4):
                    ws.plan(("zc", l, hf, j), mov_block(w_in_d, l, j, O_ZC))
                for h in range(4):
                    ws.plan(("qc", l, hf, h), feat_block(w_in_d, l, O_QC + h * 128))
                for g in range(4):
                    ws.plan(("u", l, hf, g), feat_block(w_in_d, l, O_U + g * 128))
                    ws.plan(("zb", l, hf, g), feat_block(w_in_d, l, O_ZB + g * 128))
                for hp in range(2):
                    for hh in range(2):
                        h = 2 * hp + hh
                        ws.plan(("q", l, hf, h), feat_block(w_in_d, l, O_QK + h * 128))
                        ws.plan(("k", l, hf, h), feat_block(w_in_d, l, O_QK + 512 + h * 128))
                    for hh in range(2):
                        ws.plan(("za", l, hf, 2 * hp + hh), feat_block(w_in_d, l, O_ZA + (2 * hp + hh) * 128))
                    for j in range(4):
                        ws.plan(("vo", l, hf, hp, j), vo_block(l, j, hp))
                for ch in range(2):
                    for j in range(6):
                        ws.plan(("wo", l, hf, ch, j), mov_block(w_out_d, l, j, ch * 512))

        T.dma("sp", [(CST[:, :], cst_d[:, :])], W=["CST"])
        T.dma("sp", [(SPALL[:, :, :], smallp_d[:, :, :])], W=["SPALL"])
        T.dma("pool", [(IDB[:, :], cst_d[:, 0:128])], W=["IDB"])
        T.op("dve", lambda e: e.memset(ONESF[:, :], 1.0), W=["ONESF"])
        T.op("dve", lambda e: e.memset(NEGH[:, :], -0.5), W=["NEGH"])
        T.op("dve", lambda e: e.memset(VM[:, :, :, 128:129], 2.0), W=["VM"])
        for p_ in range(2):
            T.op("dve", lambda e, p_=p_: e.memset(VB[p_][:, :, 128:129], 1.0), W=["VB%d" % p_])
        T.op("dve", lambda e: e.memset(ONESROW[:, :], 1.0), W=["ONESROW"])
        ws._issue_upto(7)
        T.dma("sp", [(FBIG[:, 2 * 528:2 * 528 + 1024], gb_d[2])], W=["F2", "F3"], key="GXM")
        T.dma("sp", [(MEMB, mem_d[0:128, :])], W=["F0", "F1"], key="MEMB")
        for t in range(8):
            T.dma("sp", [(X[:, t, :], x_d[t * 128:(t + 1) * 128, :])], W=["X%d" % t])
        pending_done = []

        def wdone(key):
            T.call(lambda: ws.done(key))

        def flush_done():
            for k_ in pending_done:
                ws.done(k_)
            del pending_done[:]
        mbi = [0]

        def next_mbank():
            mbi[0] ^= 1
            return (BBP, "BBP") if mbi[0] else (NUM, "NUM")

        def dump(name, ap_sb, res):
            if name in dbg_d:
                T.dma("pool", [(dbg_d[name], ap_sb)], R=res, key="dbg_" + name)

        def rstd_cols(ss_ap, out_ap, n, res_in, res_out, scale, eps):
            T.op("dve", lambda e: e.tensor_scalar(out=TMPS[:, 0:n], in0=ss_ap, scalar1=scale, scalar2=eps,
                                                   op0=ALU.mult, op1=ALU.add), R=res_in, W=["TMPS"])
            T.op("pool", lambda e: e.tensor_tensor(out=out_ap, in0=TMPS[:, 0:n], in1=NEGH[:, 0:n], op=ALU.pow),
                 R=["TMPS", "NEGH"], W=res_out)

        def norm_transpose(src_ap, src_res, rstd_ap, rstd_res, gtile, gres, par, dst_ap, dst_res):
            xn = XNK[par]
            T.op("dve", lambda e: e.scalar_tensor_tensor(out=xn[:, :], in0=src_ap, scalar=rstd_ap, in1=gtile[:, :],
                                                          op0=ALU.mult, op1=ALU.mult),
                 R=(src_res if isinstance(src_res, list) else [src_res]) + [rstd_res, gres], W=["XNK%d" % par], cost=1.25)

            def tr(e):
                ins = None
                for kc in range(8):
                    ins = e.transpose(PT[:, kc * 128:(kc + 1) * 128], xn[:, kc * 128:(kc + 1) * 128], IDB[:, :])
                return ins
            T.op("pe", tr, R=["XNK%d" % par, "IDB"], W=["PT"], cost=1.2)
            T.op("act", lambda e: e.copy(out=dst_ap, in_=PT[:, :].rearrange("p (k c) -> p k c", c=128)),
                 R=["PT"], W=(["G1a", "G1b"] if dst_res == "G1a" else [dst_res]), cost=1.0)

        def mm_feat(blk, XN_cols, out_ap):
            def f(e):
                ins = None
                bv = blk[:, :].rearrange("p (k c) -> p k c", c=128)
                for kc in range(8):
                    ins = e.matmul(out_ap, bv[:, kc, :], XNT[:, kc, XN_cols[0]:XN_cols[1]], start=(kc == 0), stop=(kc == 7))
                return ins
            return f

        def silu2_from_psum(pm, pm_res, n, th_tile, th_res, out_ap, out_res, c_act=0.65, c_dve=0.72):
            T.op("act", lambda e: e.activation(out=th_tile[:, 0:n], in_=pm[:, 0:n], func=AF.Tanh, scale=0.5),
                 R=[pm_res], W=[th_res], cost=c_act)
            T.op("dve", lambda e: e.scalar_tensor_tensor(out=out_ap, in0=th_tile[:, 0:n], scalar=1.0, in1=pm[:, 0:n],
                                                          op0=ALU.add, op1=ALU.mult),
                 R=[th_res, pm_res], W=(out_res if isinstance(out_res, list) else [out_res]), cost=c_dve)

        CWvs = [SPALL[:, l_, 0:32].rearrange("p (b j) -> p b j", j=4) for l_ in range(L)]

        def layer_setup(l):
            SPXl, sxr = SPX2[:, l % 2, :], "SPX%d" % (l % 2)
            T.dma("pool", [(CBROW[0:1, :], gb_d[5 + l][0:1, :])], W=["CBROW"])
            T.dma("pool", [(PW[:, :, :], pool_w_d[l].rearrange("g c d -> c g d"))], W=["PW"])
            T.dma("pool", [(WG[:, :, :], wv(w_in_d, l)[:, :, O_G:O_G + 8])], W=["WG"])
            SP = SPALL[:, l, :]
            T.op("dve", lambda e: e.tensor_tensor(out=SPXl[:, 0:8], in0=SP[:, 48:56], in1=LNC8, op=ALU.add),
                 R=["SPALL", "CST"], W=[sxr])
            T.op("dve", lambda e: e.tensor_scalar(out=SPXl[:, 8:12], in0=SP[:, 40:44], scalar1=1.0, scalar2=None, op0=ALU.mult),
                 R=["SPALL"], W=[sxr])
            T.op("dve", lambda e: e.tensor_scalar(out=SPXl[:, 12:16], in0=SP[:, 44:48], scalar1=0.5, scalar2=None, op0=ALU.mult),
                 R=["SPALL"], W=[sxr])
            T.op("dve", lambda e: e.memset(C32[:, :, :], 0.0), W=["C32"])
            T.op("dve", lambda e: e.memset(HALO_U[:, :, :], 0.0), W=["HALO_U"])
            T.op("dve", lambda e: e.memset(HALOB[:, :, :], 0.0), W=["HALOB"])

        def phase_M(l, preloaded=False):
            MNT = G1[:, :].rearrange("p (k c) -> p k c", c=256)
            GXM = FBIG[:, 2 * 528:2 * 528 + 1024]
            MNF = FBIG[:, 4 * 528:4 * 528 + 1024]
            if not preloaded:
                T.dma("sp", [(GXM, gb_d[2 + l])], W=["F2", "F3"], key="GXM")
            for mc in range(2):
                if not (preloaded and mc == 0):
                    T.dma("sp", [(MEMB, mem_d[mc * 128:(mc + 1) * 128, :])], W=["F0", "F1"], key="MEMB")
                T.op("act", lambda e, mc=mc: e.activation(out=MNF, in_=MEMB, func=AF.Square, accum_out=MSS[:, mc:mc + 1]),
                     R=["F0", "F1"], W=["MSS", "F4", "F5"], cost=1.2)
                T.op("dve", lambda e, mc=mc: e.tensor_scalar(out=TMPS[:, 15:16], in0=MSS[:, mc:mc + 1], scalar1=1.0 / D, scalar2=EPS,
                                                             op0=ALU.mult, op1=ALU.add), R=["MSS"], W=["TMPS"], cost=0.1)
                T.op("pool", lambda e, mc=mc: e.tensor_tensor(out=MSS[:, 4 + mc:5 + mc], in0=TMPS[:, 15:16], in1=NEGH[:, 0:1], op=ALU.pow),
                     R=["TMPS", "NEGH"], W=["MSS"], cost=0.7)
                T.op("dve", lambda e, mc=mc: e.scalar_tensor_tensor(out=MNF, in0=MEMB, scalar=MSS[:, 4 + mc:5 + mc], in1=GXM,
                                                                    op0=ALU.mult, op1=ALU.mult),
                     R=["F0", "F1", "MSS", "F2", "F3"], W=["F4", "F5"], cost=1.25)
                for half_, (bk, bkr) in enumerate(((BBP, "BBP"), (NUM, "NUM"))):
                    def tr(e, half_=half_, bk=bk):
                        ins = None
                        for k in range(4):
                            kc = half_ * 4 + k
                            ins = e.transpose(bk[:, k * 128:(k + 1) * 128], MNF[:, kc * 128:(kc + 1) * 128], CST[:, 0:128])
                        return ins
                    T.op("pe", tr, R=["F4", "F5", "CST"], W=[bkr], cost=1.0)
                    T.op("act", lambda e, half_=half_, bk=bk, mc=mc: e.copy(out=MNT[:, half_ * 4:half_ * 4 + 4, mc * 128:(mc + 1) * 128],
                                                                          in_=bk[:, :].rearrange("p (k c) -> p k c", c=128)),
                         R=[bkr], W=["G1a", "G1b"], cost=0.7)
            for h in range(4):
                blk, bres = ws.get_safe(("kvk", l, h))
                pm, pres = next_mbank()

                def f(e, blk=blk, pm=pm):
                    ins = None
                    bv = blk[:, :].rearrange("p (k c) -> p k c", c=128)
                    for kc in range(8):
                        ins = e.matmul(pm[:, 0:256], bv[:, kc, :], MNT[:, kc, :], start=(kc == 0), stop=(kc == 7))
                    return ins
                T.op("pe", f, R=[bres, "G1a", "G1b"], W=[pres])
                wdone(("kvk", l, h))
                T.op("act", lambda e, pm=pm, h=h: e.activation(out=KMT[:, h, :], in_=pm[:, 0:256], func=AF.Identity, scale=128 ** -0.5),
                     R=[pres], W=["KMT"])
            vblk = [ws.get_safe(("kvv", l, j)) for j in range(4)]
            for mc in range(2):
                pm, pres = next_mbank()

                def f(e, pm=pm, mc=mc):
                    ins = None
                    for kc in range(8):
                        bv = vblk[kc // 2][0][:, :].rearrange("p (k c) -> p k c", c=512)
                        ins = e.matmul(pm[:, :], MNT[:, kc, mc * 128:(mc + 1) * 128], bv[:, kc % 2, :], start=(kc == 0), stop=(kc == 7))
                    return ins
                T.op("pe", f, R=["G1a", "G1b"] + [b_[1] for b_ in vblk], W=[pres])
                T.op("act", lambda e, pm=pm, mc=mc: e.copy(out=VM[:, mc, :, 0:128], in_=pm[:, :].rearrange("p (h e) -> p h e", e=128)),
                     R=[pres], W=["VM"])
            for j in range(4):
                wdone(("kvv", l, j))

        def phase_N(l, hf):
            t0 = hf * 8
            T.dma("sp", [(GX[:, :], gb_d[l])], W=["GX"])
            for tl in range(8):
                t = t0 + tl
                T.op("act", lambda e, t=t: e.activation(out=G2[:, (t % 2) * 1024:(t % 2) * 1024 + 1024], in_=X[:, t, :], func=AF.Square, accum_out=SS[:, t:t + 1]),
                     R=["X%d" % t], W=["SS"] + (["G2a"] if t % 2 == 0 else ["G2b", "G2c"]))
            rstd_cols(SS[:, t0:t0 + 8], RSTD[:, t0:t0 + 8], 8, ["SS"], ["RSTD"], 1.0 / D, EPS)
            for tl in range(8):
                t = t0 + tl
                norm_transpose(X[:, t, :], "X%d" % t, RSTD[:, t:t + 1], "RSTD", GX, "GX", tl % 2,
                               XNT[:, :, tl * 128:(tl + 1) * 128], "XNT%d" % (tl // 4))

        def phase_O(l, hf, fin=False):
            t0 = hf * 8
            SPXl, sxr = SPX2[:, l % 2, :], "SPX%d" % (l % 2)
            for ch in range(2):
                wob = [ws.get_safe(("wo", l, hf, ch, j)) for j in range(6)]
                wv_ = [b_[0][:, :].rearrange("p (k c) -> p k c", c=512) for b_ in wob]
                for kc in range(4):
                    T.op("dve", lambda e, kc=kc, wv_=wv_: e.tensor_scalar(out=wv_[kc // 2][:, kc % 2, :], in0=wv_[kc // 2][:, kc % 2, :],
                                                                scalar1=SPXl[:, 8 + kc:9 + kc], scalar2=None, op0=ALU.mult),
                         R=[wob[kc // 2][1], sxr], W=[wob[kc // 2][1]], cost=0.25)
                for tl in range(8):
                    t = t0 + tl
                    c0 = tl * 128
                    pm, pres = next_mm()

                    def f(e, pm=pm, c0=c0, wv_=wv_):
                        ins = None
                        for kc in range(12):
                            ins = e.matmul(pm[:, :], YT[:, kc, c0:c0 + 128], wv_[kc // 2][:, kc % 2, :],
                                           start=(kc == 0), stop=(kc == 11))
                        return ins
                    yres = ["YT%s%d" % (b_, tl // 4) for b_ in "abc"]
                    T.op("pe", f, R=yres + [b_[1] for b_ in wob], W=[pres], cost=2.8)
                    T.op("dve", lambda e, pm=pm, t=t, ch=ch: e.tensor_tensor(out=X[:, t, ch * 512:(ch + 1) * 512], in0=pm[:, :],
                                                                            in1=X[:, t, ch * 512:(ch + 1) * 512], op=ALU.add),
                         R=[pres, "X%d" % t], W=["X%d" % t], cost=0.7)
                    if fin and ch == 1:
                        final_tile(t, 8 + tl)
                for j in range(6):
                    wdone(("wo", l, hf, ch, j))

        okeys = []

        def final_tile(t, col):
            if final_norm:
                T.op("act", lambda e: e.activation(out=G2[:, (t % 2) * 1024:(t % 2) * 1024 + 1024], in_=X[:, t, :], func=AF.Square, accum_out=SS[:, t:t + 1]),
                     R=["X%d" % t], W=["SS"] + (["G2a"] if t % 2 == 0 else ["G2b", "G2c"]), cost=1.1)
                T.op("dve", lambda e: e.tensor_scalar(out=TMPS[:, col:col + 1], in0=SS[:, t:t + 1], scalar1=1.0 / D, scalar2=EPS,
                                                      op0=ALU.mult, op1=ALU.add), R=["SS"], W=["TMPS"], cost=0.1)
                T.op("pool", lambda e: e.tensor_tensor(out=RSTD[:, t:t + 1], in0=TMPS[:, col:col + 1], in1=NEGH[:, 0:1], op=ALU.pow),
                     R=["TMPS", "NEGH"], W=["RSTD"], cost=0.7)
                T.op("dve", lambda e: e.scalar_tensor_tensor(out=X[:, t, :], in0=X[:, t, :], scalar=RSTD[:, t:t + 1], in1=GX[:, :],
                                                             op0=ALU.mult, op1=ALU.mult), R=["X%d" % t, "RSTD", "GX"], W=["X%d" % t], cost=1.25)
            T.dma("sp", [(out_d[t * 128:(t + 1) * 128, :], X[:, t, :])], R=["X%d" % t], key="out%d" % t)
            okeys.append("out%d" % t)

        def phase_final(ts, load_g=True):
            n_ = len(ts)
            if final_norm:
                if load_g:
                    T.dma("sp", [(GX[:, :], gb_d[4])], W=["GX"])
                for t in ts:
                    T.op("act", lambda e, t=t: e.activation(out=G2[:, (t % 2) * 1024:(t % 2) * 1024 + 1024], in_=X[:, t, :], func=AF.Square, accum_out=SS[:, t:t + 1]),
                         R=["X%d" % t], W=["SS"] + (["G2a"] if t % 2 == 0 else ["G2b", "G2c"]))
                rstd_cols(SS[:, ts[0]:ts[0] + n_], RSTD[:, ts[0]:ts[0] + n_], n_, ["SS"], ["RSTD"], 1.0 / D, EPS)
                for t in ts:
                    T.op("dve", lambda e, t=t: e.scalar_tensor_tensor(out=X[:, t, :], in0=X[:, t, :], scalar=RSTD[:, t:t + 1], in1=GX[:, :],
                                                                     op0=ALU.mult, op1=ALU.mult), R=["X%d" % t, "RSTD", "GX"], W=["X%d" % t])
            for t in ts:
                T.dma("sp", [(out_d[t * 128:(t + 1) * 128, :], X[:, t, :])], R=["X%d" % t], key="out%d" % t)
                okeys.append("out%d" % t)

        layer_setup(0)
        T.begin()
        phase_M(0, preloaded=True)
        LM0 = T.end()
        T.begin()
        phase_N(0, 0)
        LN0 = T.end()
        T.emit([LM0, LN0])
        flush_done()
        ws._issue_upto(RING - 1)
        for t in range(8, 16):
            T.dma("sp", [(X[:, t, :], x_d[t * 128:(t + 1) * 128, :])], W=["X%d" % t])
        for l in range(n_layers):
            SPXl, sxr = SPX2[:, l % 2, :], "SPX%d" % (l % 2)
            CWv = CWvs[l]
            CBv = SPALL[:, l, 32:40]
            for hf in range(2):
                t0 = hf * 8
                first = (hf == 0)
                if l == 0 and hf == 0:
                    dump("xnt", XNT[:, :, :], ["XNT0", "XNT1"])

                zcb = [ws.get(("zc", l, hf, j)) for j in range(4)]
                qcb = [ws.get(("qc", l, hf, h)) for h in range(4)]
                YC = G0[:, :].rearrange("p (t c) -> p t c", c=512)
                SZC = G1[:, :].rearrange("p (t c) -> p t c", c=512)
                cb_ = [(MM[0], "MM0"), (NUM, "NUM")]
                ci_ = [0]

                def next_c():
                    ci_[0] = (ci_[0] + 1) % 2
                    return cb_[ci_[0]]
                listC = []
                for mt in range(2):
                    xres = "XNT%d" % mt
                    T.begin()
                    for tt in range(4):
                        c0 = mt * 512 + tt * 128
                        pm, pres = next_c()

                        def f(e, pm=pm, c0=c0):
                            ins = None
                            for kc in range(8):
                                bv = zcb[kc // 2][0][:, :].rearrange("p (k c) -> p k c", c=512)
                                ins = e.matmul(pm[:, :], XNT[:, kc, c0:c0 + 128], bv[:, kc % 2, :], start=(kc == 0), stop=(kc == 7))
                            return ins
                        T.op("pe", f, R=[xres] + [b[1] for b in zcb], W=[pres], cost=1.9)
                        th = FT[6 + tt % 2]
                        silu2_from_psum(pm, pres, 512, th, "F%d" % (6 + tt % 2), SZC[:, tt, :], "G1a" if tt < 2 else "G1b")
                    if mt == 1:
                        for j in range(4):
                            T.call(lambda j=j: ws.done(("zc", l, hf, j)))
                    Zl = T.end()
                    Pl, Vl = [], []
                    for h in range(4):
                        T.begin()
                        if h % 2 == 0:
                            PTB, QCB, ptr_, qcr_ = G2[:, 0:1024].rearrange("p (m c) -> p m c", c=512), G2[:, 1024:1536], "G2a", "G2b"
                        else:
                            PTB, QCB, ptr_, qcr_ = G3[:, 0:1024].rearrange("p (m c) -> p m c", c=512), G3[:, 1024:1536], "G3a", "G3b"
                        pm, pres = next_c()
                        T.op("pe", mm_feat(qcb[h][0], (mt * 512, mt * 512 + 512), pm[:, :]), R=[xres, qcb[h][1]], W=[pres], cost=1.9)
                        T.op("act", lambda e, pm=pm, QCB=QCB: e.copy(out=QCB, in_=pm[:, :]), R=[pres], W=[qcr_], cost=0.65)
                        for mc in range(2):
                            pm2, pres2 = next_c()
                            T.op("pe", lambda e, pm2=pm2, mc=mc, h=h, QCB=QCB: e.matmul(pm2[:, :], KMT[:, h, mc * 128:(mc + 1) * 128], QCB, start=True, stop=True),
                                 R=["KMT", qcr_], W=[pres2])
                            T.op("act", lambda e, pm2=pm2, mc=mc, PTB=PTB: e.activation(out=PTB[:, mc, :], in_=pm2[:, :], func=AF.Exp),
                                 R=[pres2], W=[ptr_], cost=0.65)
                        if mt == 1:
                            T.call(lambda h=h: ws.done(("qc", l, hf, h)))
                        Pl.append(T.end())
                        T.begin()
                        for tp in range(2):
                            bank, bres_ = (DCA, "DCA") if tp == 0 else (DCB, "DCB")

                            def f(e, bank=bank, tp=tp, h=h, PTB=PTB):
                                ins = None
                                for i in range(2):
                                    tt = tp * 2 + i
                                    for mc in range(2):
                                        ins = e.matmul(bank[:, i * 129:(i + 1) * 129], PTB[:, mc, tt * 128:(tt + 1) * 128],
                                                       VM[:, mc, h, :], start=(mc == 0), stop=(mc == 1))
                                return ins
                            T.op("pe", f, R=[ptr_, "VM"], W=[bres_], cost=0.5)
                            bv = bank[:, 0:258].rearrange("p (i e) -> p i e", e=129)
                            T.op("dve", lambda e, bv=bv, tp=tp: e.reciprocal(out=RDC[:, 2 * tp:2 * tp + 2], in_=bv[:, :, 128]), R=[bres_], W=["RDC%d" % tp], cost=0.2)
                            for i in range(2):
                                tt = tp * 2 + i
                                T.op("dve", lambda e, bv=bv, i=i, tt=tt, h=h, tp=tp: e.scalar_tensor_tensor(
                                    out=YC[:, tt, h * 128:(h + 1) * 128], in0=bv[:, i, 0:128], scalar=RDC[:, 2 * tp + i:2 * tp + i + 1],
                                    in1=SZC[:, tt, h * 128:(h + 1) * 128], op0=ALU.mult, op1=ALU.mult),
                                    R=[bres_, "RDC%d" % tp, "G1a", "G1b"], W=["G0a" if tt < 2 else "G0b"], cost=0.3)
                        Vl.append(T.end())
                    T.begin()
                    for tt in range(4):
                        c0 = mt * 512 + tt * 128

                        def tr(e, tt=tt):
                            ins = None
                            for h in range(4):
                                ins = e.transpose(PT[:, h * 128:(h + 1) * 128], YC[:, tt, h * 128:(h + 1) * 128], IDB[:, :])
                            return ins
                        T.op("pe", tr, R=["G0a", "G0b", "IDB"], W=["PT"])
                        T.op("act", lambda e, c0=c0: e.copy(out=YT[:, 8:12, c0:c0 + 128], in_=PT[:, 0:512].rearrange("p (h c) -> p h c", c=128)),
                             R=["PT"], W=["YTc%d" % mt])
                    TRl = T.end()
                    listC += Zl + Pl[0] + Pl[1] + Vl[0] + Pl[2] + Vl[1] + Pl[3] + Vl[2] + Vl[3] + TRl

                ub = [ws.get(("u", l, hf, g)) for g in range(4)]
                zbb = [ws.get(("zb", l, hf, g)) for g in range(4)]
                UW, S1, S2, THB, SZB, TMPF = FT[0], FT[1], FT[2], FT[3], FT[4], FT[5]
                Xl, Yl = [], []
                for mt in range(2):
                    xres = "XNT%d" % mt
                    for g in range(4):
                        T.begin()
                        win = 2 << g
                        PB, pbr = XNK[len(Xl) % 2][:, 0:512], "XNK%d" % (len(Xl) % 2)
                        pm, pres = MM[1], "MM1"
                        T.op("pe", mm_feat(ub[g][0], (mt * 512, mt * 512 + 512), pm[:, :]), R=[xres, ub[g][1]], W=[pres], cost=1.9)
                        T.op("dve", lambda e, g=g: e.tensor_copy(out=UW[:, 0:16], in_=HALO_U[:, g, :]), R=["HALO_U"], W=["F0"])
                        T.op("act", lambda e, pm=pm: e.copy(out=UW[:, 16:528], in_=pm[:, :]), R=[pres], W=["F0"], cost=0.65)
                        T.op(BADD, lambda e: e.tensor_tensor(out=S1[:, 1:528], in0=UW[:, 1:528], in1=UW[:, 0:527], op=ALU.add),
                             R=["F0"], W=["F1"], cost=0.72 if BADD == "dve" else 1.1)
                        cur, cres = S1, "F1"
                        oth, ores = S2, "F2"
                        lo, sh = 1, 2
                        for step in range(g):
                            nlo = lo + sh
                            T.op(BADD if step % 2 == 0 else BADD2, lambda e, cur=cur, oth=oth, nlo=nlo, sh=sh: e.tensor_tensor(
                                out=oth[:, nlo:528], in0=cur[:, nlo:528], in1=cur[:, nlo - sh:528 - sh], op=ALU.add),
                                R=[cres], W=[ores], cost=0.72)
                            cur, cres, oth, ores = oth, ores, cur, cres
                            lo, sh = nlo, sh * 2
                        T.op("dve", lambda e, cur=cur, win=win, PB=PB: e.scalar_tensor_tensor(
                            out=PB, in0=cur[:, 16:528], scalar=1.0 / win, in1=UW[:, 16:528], op0=ALU.mult, op1=ALU.subtract),
                            R=[cres, "F0"], W=[pbr], cost=0.7)
                        if first and mt == 0:
                            w1 = win - 1
                            T.op("dve", lambda e, cur=cur, w1=w1: e.tensor_tensor(out=TMPF[:, 0:w1], in0=cur[:, 16:16 + w1], in1=INVC[:, 0:w1], op=ALU.mult),
                                 R=[cres, "CST"], W=["F5"])
                            T.op("dve", lambda e, w1=w1, PB=PB: e.tensor_tensor(out=PB[:, 0:w1], in0=TMPF[:, 0:w1], in1=UW[:, 16:16 + w1], op=ALU.subtract),
                                 R=["F5", "F0"], W=[pbr])
                        T.op("dve", lambda e, g=g: e.tensor_copy(out=HALO_U[:, g, :], in_=UW[:, 512:528]), R=["F0"], W=["HALO_U"])
                        Xl.append(T.end())
                        T.begin()
                        pm2, pres2 = BBP, "BBP"
                        T.op("pe", lambda e, pm2=pm2, g=g, PB=PB: e.matmul(pm2[:, :], PW[:, g, :], PB, start=True, stop=True), R=["PW", pbr], W=[pres2])
                        T.op("pe", mm_feat(zbb[g][0], (mt * 512, mt * 512 + 512), MM[1][:, :]), R=[xres, zbb[g][1]], W=["MM1"], cost=1.9)
                        silu2_from_psum(MM[1], "MM1", 512, THB, "F3", SZB[:, 0:512], "F4")
                        T.op("dve", lambda e, pm2=pm2, g=g, mt=mt: e.scalar_tensor_tensor(
                            out=YT[:, 4 + g, mt * 512:(mt + 1) * 512], in0=pm2[:, :], scalar=SPXl[:, 12 + g:13 + g], in1=SZB[:, 0:512],
                            op0=ALU.mult, op1=ALU.mult), R=[pres2, sxr, "F4"], W=["YTb%d" % mt], cost=0.7)
                        if mt == 1:
                            T.call(lambda g=g: (ws.done(("u", l, hf, g)), ws.done(("zb", l, hf, g))))
                        Yl.append(T.end())
                listB = list(Xl[0])
                for k_ in range(8):
                    if k_ + 1 < 8:
                        listB += Xl[k_ + 1]
                    listB += Yl[k_]
                T.begin()
                def f(e):
                    ins = None
                    for tl in range(8):
                        for kc in range(8):
                            ins = e.matmul(MISC[:, tl * 8:(tl + 1) * 8], XNT[:, kc, tl * 128:(tl + 1) * 128], WG[:, kc, :],
                                           start=(kc == 0), stop=(kc == 7))
                    return ins
                T.op("pe", f, R=["XNT0", "XNT1", "WG"], W=["MISC"])
                T.op("dve", lambda e: e.tensor_tensor(out=GA[:, :, :], in0=MISC[:, 0:64].rearrange("p (t g) -> p t g", g=8),
                                                      in1=SPXl[:, 0:8].unsqueeze(1).to_broadcast([128, 8, 8]), op=ALU.add),
                     R=["MISC", sxr], W=["GA"])
                T.op("act", lambda e: e.activation(out=E1[:, :, :], in_=GA[:, :, 4:8], func=AF.Exp, scale=-1.0), R=["GA"], W=["E1"])
                T.op("act", lambda e: e.activation(out=L1[:, :].rearrange("p (t h) -> p t h", h=4), in_=E1[:, :, :], func=AF.Ln, bias=1.0),
                     R=["E1"], W=["L1"])
                T.op("pe", lambda e: e.matmul(MISC[:, 64:96], U_, L1[:, :], start=True, stop=True), R=["CST", "L1"], W=["MISC"])
                T.op("pe", lambda e: e.matmul(MISC[:, 96:128], LX_, L1[:, :], start=True, stop=True), R=["CST", "L1"], W=["MISC"])
                IGC = GA[:, :, 0:4]
                T.op("dve", lambda e: e.tensor_tensor(out=TB[:, 0:32].rearrange("p (t h) -> p t h", h=4), in0=IGC,
                                                      in1=MISC[:, 64:96].rearrange("p (t h) -> p t h", h=4), op=ALU.add),
                     R=["GA", "MISC"], W=["TB"])
                T.op("dve", lambda e: e.tensor_tensor(out=TB[:, 32:64].rearrange("p (t h) -> p t h", h=4), in0=IGC,
                                                      in1=MISC[:, 96:128].rearrange("p (t h) -> p t h", h=4), op=ALU.subtract),
                     R=["GA", "MISC"], W=["TB"])
                T.op("act", lambda e: e.activation(out=EXPB[:, :], in_=TB[:, 0:32], func=AF.Exp), R=["TB"], W=["EXPB"])
                T.op("act", lambda e: e.activation(out=WK[:, :], in_=TB[:, 32:64], func=AF.Exp), R=["TB"], W=["WK"])
                T.op("act", lambda e: e.activation(out=EBT[:, :], in_=MISC[:, 64:96], func=AF.Exp, scale=-1.0), R=["MISC"], W=["EBT"])
                T.op("act", lambda e: e.activation(out=IEB[:, :], in_=MISC[:, 64:96], func=AF.Exp), R=["MISC"], W=["IEB"])
                T.op("pe", lambda e: e.matmul(MISC[:, 128:160], SEL_, EBT[:, :], start=True, stop=True), R=["CST", "EBT"], W=["MISC"])
                T.op("act", lambda e: e.copy(out=DEC[:, :], in_=MISC[:, 128:160]), R=["MISC"], W=["DEC"])

                listG = T.end()
                T.emit([listC, listB, listG])
                if l == 0 and hf == 0:
                    dump("ytc", YT[:, 8:12, :], ["YTc0", "YTc1"])
                if l == 0 and hf == 0:
                    dump("ytb", YT[:, 4:8, :], ["YTb0", "YTb1"])

                QTm = [G0[:, 0:1024].rearrange("p (h c) -> p h c", c=512), G2[:, 0:1024].rearrange("p (h c) -> p h c", c=512)]
                KTm = [G0[:, 1024:2048].rearrange("p (h c) -> p h c", c=512), G2[:, 1024:2048].rearrange("p (h c) -> p h c", c=512)]
                QTr = ["G0a", "G2a"]
                KTr = [["G0b"], ["G2b", "G2c"]]
                SZAm = [G1[:, 1024:2048].rearrange("p (h c) -> p h c", c=512), G1[:, 0:1024].rearrange("p (h c) -> p h c", c=512)]
                SZAr = ["G1b", "G1a"]
                wts = {}

                def fetch_weights(hp, with_vo=True):
                    wts[hp] = ([ws.get(("q", l, hf, 2 * hp + hh)) for hh in range(2)],
                               [ws.get(("k", l, hf, 2 * hp + hh)) for hh in range(2)],
                               [ws.get(("za", l, hf, 2 * hp + hh)) for hh in range(2)],
                               [ws.get(("vo", l, hf, hp, j)) for j in range(4)] if with_vo else None)

                def build_dg(hp, bps=(0, 1, 2, 3)):
                    for bp in bps:
                        blk_i = 4 * (bp % 2) + 2 * hp + bp // 2
                        T.op("dve", lambda e, bp=bp, blk_i=blk_i: e.tensor_tensor(
                            out=DG[:, bp, :, :], in0=IDB[:, :].unsqueeze(1).to_broadcast([128, 4, 128]),
                            in1=CWv[:, blk_i, :].unsqueeze(2).to_broadcast([128, 4, 128]), op=ALU.mult),
                            R=["IDB", "SPALL"], W=["DG%d" % bp], cost=0.7)

                def prepass_block(hp, mt, bidx):
                    qb, kb, zab, vob = wts[hp]
                    xres = "XNT%d" % mt
                    mp = mt % 2
                    if bidx < 4:
                        hh, isk = bidx // 2, bidx % 2
                        h = 2 * hp + hh
                        blk_i = 4 * isk + h
                        wb = (kb if isk else qb)[hh]
                        RAWB, rres = XNK[bidx % 2], "XNK%d" % (bidx % 2)
                        TH, tres = FT[4 + bidx % 2], "F%d" % (4 + bidx % 2)
                        pm, pres = MM[1], "MM1"
                        T.op("pe", mm_feat(wb[0], (mt * 512, mt * 512 + 512), pm[:, :]), R=[xres, wb[1]], W=[pres], cost=1.9)
                        T.op("dve", lambda e: e.tensor_copy(out=RAWB[:, 0:3], in_=HALOB[:, blk_i, 0:3]), R=["HALOB"], W=[rres], cost=0.1)
                        T.op("act", lambda e: e.copy(out=RAWB[:, 3:515], in_=pm[:, :]), R=[pres], W=[rres], cost=0.65)
                        T.op("act", lambda e: e.copy(out=HALOB[:, blk_i, 0:3], in_=RAWB[:, 512:515]), R=[rres], W=["HALOB"], cost=0.2)
                        pc, pcres = DCB, "DCB"

                        def f(e):
                            ins = None
                            for j in range(4):
                                ins = e.matmul(pc[:, :], DG[:, bidx, j, :], RAWB[:, j:j + 512], start=(j == 0), stop=(j == 3))
                            return ins
                        T.op("pe", f, R=["DG%d" % bidx, rres], W=[pcres], cost=0.95)
                        dstv = (KTm if isk else QTm)[mp][:, hh, :]
                        dstres = KTr[mp] if isk else [QTr[mp]]
                        cbv = CBv
                        T.op("act", lambda e: e.activation(out=dstv, in_=pc[:, 0:512], func=AF.Silu, bias=cbv[:, blk_i:blk_i + 1]),
                             R=[pcres, "SPALL"], W=dstres, cost=0.65)
                    else:
                        hh = bidx - 4
                        pm, pres = (MM[1], "MM1") if hh == 0 else (DCB, "DCB")
                        T.op("pe", mm_feat(zab[hh][0], (mt * 512, mt * 512 + 512), pm[:, :]), R=[xres, zab[hh][1]], W=[pres], cost=1.9)
                        T.op("act", lambda e: e.activation(out=SZAm[mp][:, hh, :], in_=pm[:, 0:512], func=AF.Silu),
                             R=[pres], W=[SZAr[mp]], cost=0.65)

                def front(g):
                    hp, i = g // 8, g % 8
                    qb, kb, zab, vob = wts[hp]
                    mt, tt = i // 4, i % 4
                    mp = mt % 2
                    p = g % 2
                    xres = "XNT%d" % mt
                    QT, KT = QTm[mp], KTm[mp]
                    qres, kres = QTr[mp], KTr[mp]
                    c0 = i * 128
                    cm = tt * 128
                    vb, vbr = VB[p], "VB%d" % p
                    T.op("act", lambda e: e.copy(out=CBA[p][:, :, :], in_=C32[:, 2 * hp:2 * hp + 2, :]), R=["C32"], W=["CBA%d" % p])
                    pm, pres = MM[0], "MM0"

                    def f(e):
                        ins = None
                        for kc in range(8):
                            bv = vob[kc // 2][0][:, :].rearrange("p (k c) -> p k c", c=512)
                            ins = e.matmul(pm[:, :], XNT[:, kc, c0:c0 + 128], bv[:, kc % 2, :], start=(kc == 0), stop=(kc == 7))
                        return ins
                    T.op("pe", f, R=[xres] + [b_[1] for b_ in vob], W=[pres], cost=1.9)
                    gi = [i * 4 + 2 * hp + hh for hh in range(2)]

                    def tr(e):
                        ins = None
                        for hh in range(2):
                            ins = e.transpose(PT[:, hh * 128:(hh + 1) * 128], KT[:, hh, cm:cm + 128], IDB[:, :])
                        return ins
                    T.op("pe", tr, R=kres + ["IDB"], W=["PT"])
                    T.op("act", lambda e: e.copy(out=vb[:, :, 0:128], in_=pm[:, 0:256].rearrange("p (h e) -> p h e", e=128)),
                         R=[pres], W=[vbr])
                    T.op("act", lambda e: e.activation(out=THO[p][:, :], in_=pm[:, 256:512], func=AF.Tanh, scale=0.5),
                         R=[pres], W=["THO%d" % p])
                    if KW_ACT:
                        for hh in range(2):
                            T.op("act", lambda e, hh=hh: e.activation(out=KW[:, hh, :], in_=PT[:, hh * 128:(hh + 1) * 128], func=AF.Copy,
                                                                     scale=WK[:, gi[hh]:gi[hh] + 1]),
                                 R=["PT", "WK"], W=["KW%d" % hh], cost=0.3)
                    else:
                        T.op("dve", lambda e: e.tensor_tensor(
                            out=KW[:, :, :], in0=PT[:, 0:256].rearrange("p (h c) -> p h c", c=128),
                            in1=WK[:, gi[0]:gi[0] + 2].unsqueeze(2).to_broadcast([128, 2, 128]), op=ALU.mult),
                            R=["PT", "WK"], W=["KW0", "KW1"], cost=0.42)

                    def f(e):
                        ins = None
                        for hh in range(2):
                            ins = e.matmul(DCA[:, hh * 129:(hh + 1) * 129], KW[:, hh, :], vb[:, hh, :], start=True, stop=True)
                        return ins
                    T.op("pe", f, R=["KW0", "KW1", vbr], W=["DCA"], cost=0.35)

                    def f(e):
                        ins = None
                        for hh in range(2):
                            ins = e.matmul(BBP[:, hh * 128:(hh + 1) * 128], KT[:, hh, cm:cm + 128], QT[:, hh, cm:cm + 128], start=True, stop=True)
                        return ins
                    T.op("pe", f, R=kres + [qres], W=["BBP"], cost=0.3)
                    for hh in range(2):
                        h = 2 * hp + hh
                        T.op("dve", lambda e, hh=hh, h=h: e.scalar_tensor_tensor(
                            out=C32[:, h, :], in0=C32[:, h, :], scalar=DEC[:, gi[hh]:gi[hh] + 1],
                            in1=DCA[:, hh * 129:(hh + 1) * 129], op0=ALU.mult, op1=ALU.add), R=["C32", "DEC", "DCA"], W=["C32"], cost=0.36)
                    for hh in range(2):
                        T.op("dve", lambda e, hh=hh, g_=gi[hh]: e.scalar_tensor_tensor(
                            out=STB[p][:, hh * 128:(hh + 1) * 128], in0=BBP[:, hh * 128:(hh + 1) * 128], scalar=EXPB[:, g_:g_ + 1],
                            in1=U_, op0=ALU.mult, op1=ALU.mult), R=["BBP", "EXPB", "CST"], W=["STB%d" % p], cost=0.27)

                def back_a(g):
                    hp, i = g // 8, g % 8
                    p = g % 2
                    vb, vbr = VB[p], "VB%d" % p
                    mp = (i // 4) % 2
                    QT, qres = QTm[mp], QTr[mp]
                    cm = (i % 4) * 128
                    gi0 = i * 4 + 2 * hp

                    def f(e):
                        ins = None
                        for hh in range(2):
                            o_ = NUM[:, hh * 129:(hh + 1) * 129]
                            e.matmul(o_, STB[p][:, hh * 128:(hh + 1) * 128], vb[:, hh, :], start=True, stop=False)
                            ins = e.matmul(o_, QT[:, hh, cm:cm + 128], CBA[p][:, hh, :], start=False, stop=True)
                        return ins
                    T.op("pe", f, R=["STB%d" % p, vbr, qres, "CBA%d" % p], W=["NUM"], cost=0.5)
                    NV = NUM[:, 0:258].rearrange("p (h e) -> p h e", e=129)
                    T.op("act", lambda e: e.activation(out=SM[:, 0:2], in_=NV[:, :, 128], func=AF.Abs), R=["NUM"], W=["SM"])
                    T.op("dve", lambda e: e.scalar_tensor_tensor(
                        out=TTt[:, :].rearrange("p (h e) -> p h e", e=128), in0=THO[p][:, :].rearrange("p (h e) -> p h e", e=128),
                        scalar=1.0, in1=NV[:, :, 0:128], op0=ALU.add, op1=ALU.mult), R=["THO%d" % p, "NUM"], W=["TTt0", "TTt1"])
                    T.op("dve", lambda e: e.tensor_tensor(out=SM[:, 2:4], in0=SM[:, 0:2], in1=IEB[:, gi0:gi0 + 2], op=ALU.max),
                         R=["SM", "IEB"], W=["SMb"], cost=0.1)
                    T.op("dve", lambda e: e.tensor_tensor(out=SM[:, 4:6], in0=SM[:, 2:4], in1=SM[:, 2:4], op=ALU.mult), R=["SMb"], W=["SMc"], cost=0.1)
                    for hh in range(2):
                        T.op("act", lambda e, hh=hh: e.activation(out=HGt[:, hh * 128:(hh + 1) * 128], in_=TTt[:, hh * 128:(hh + 1) * 128], func=AF.Square,
                                                                 scale=128 ** -0.5, accum_out=SM[:, 6 + hh:7 + hh]),
                             R=["TTt%d" % hh], W=["SMd%d" % hh, "HGt%d" % hh])
                    T.op("dve", lambda e: e.scalar_tensor_tensor(out=SM[:, 8:10], in0=SM[:, 4:6], scalar=4 * EPS, in1=SM[:, 6:8],
                                                                 op0=ALU.mult, op1=ALU.add), R=["SMc", "SMd0", "SMd1"], W=["SMe"], cost=0.1)
                    T.op("pool", lambda e: e.tensor_tensor(out=SM[:, 10:12], in0=SM[:, 8:10], in1=NEGH[:, 0:2], op=ALU.pow),
                         R=["SMe", "NEGH"], W=["SMf"], cost=0.7)
                    T.op("dve", lambda e: e.tensor_tensor(
                        out=YTK[p][:, :].rearrange("p (h e) -> p h e", e=128), in0=TTt[:, :].rearrange("p (h e) -> p h e", e=128),
                        in1=SM[:, 10:12].unsqueeze(2).to_broadcast([128, 2, 128]), op=ALU.mult),
                        R=["TTt0", "TTt1", "SMf"], W=["YTK%d" % p], cost=0.42)

                def back_b(g):
                    hp, i = g // 8, g % 8
                    mt, tt = i // 4, i % 4
                    mp = mt % 2
                    p = g % 2
                    c0 = i * 128
                    cm = tt * 128

                    def tr(e):
                        ins = None
                        for hh in range(2):
                            ins = e.transpose(PT[:, 256 + hh * 128:384 + hh * 128], YTK[p][:, hh * 128:(hh + 1) * 128], IDB[:, :])
                        return ins
                    T.op("pe", tr, R=["YTK%d" % p, "IDB"], W=["PT"])
                    T.op("dve", lambda e: e.tensor_tensor(
                        out=YT[:, 2 * hp:2 * hp + 2, c0:c0 + 128], in0=PT[:, 256:512].rearrange("p (h c) -> p h c", c=128),
                        in1=SZAm[mp][:, :, cm:cm + 128], op=ALU.mult), R=["PT", SZAr[mp]], W=["YTa%d" % mt])

                def rec(fn, *a):
                    T.begin()
                    fn(*a)
                    return T.end()

                fetch_weights(0)
                ws.throttle = True
                build_dg(0)
                for bidx in range(6):
                    prepass_block(0, 0, bidx)
                front(0)
                for g in range(16):
                    hp, i = g // 8, g % 8
                    lists = []
                    if g == 7:
                        fetch_weights(1)
                    if g + 1 < 16:
                        lists.append(rec(front, g + 1))
                    lists.append(rec(back_a, g))
                    if g >= 1:
                        lists.append(rec(back_b, g - 1))
                    if i < 3:
                        lists.append(rec(lambda: (prepass_block(hp, 1, 2 * i), prepass_block(hp, 1, 2 * i + 1),
                                                  (build_dg(1, (0, 1)) if (hp == 0 and i == 2) else None))))
                    elif hp == 0 and i == 3:
                        lists.append(rec(lambda: build_dg(1, (2, 3))))
                    elif hp == 0 and 4 <= i < 7:
                        if i == 4:
                            fetch_weights(1, with_vo=False)
                        k_ = i - 4
                        lists.append(rec(lambda: (prepass_block(1, 0, 2 * k_), prepass_block(1, 0, 2 * k_ + 1))))

                    T.emit(lists)
                    if i == 3:
                        for hh in range(2):
                            ws.done(("q", l, hf, 2 * hp + hh))
                            ws.done(("k", l, hf, 2 * hp + hh))
                        for hh in range(2):
                            ws.done(("za", l, hf, 2 * hp + hh))
                    if i == 7:
                        for j in range(4):
                            ws.done(("vo", l, hf, hp, j))
                    ws.pump(PUMP_N)
                T.emit([rec(back_b, 15)])
                ws.throttle = False
                ws._issue_upto(ws.limit)
                if l == 0 and hf == 0:
                    dump("yta", YT[:, 0:4, :], ["YTa0", "YTa1"])

                last = (hf == 1 and l + 1 == n_layers)
                if last and final_norm:
                    T.dma("sp", [(GX[:, :], gb_d[4])], W=["GX"])
                T.begin()
                phase_O(l, hf, fin=last)
                LO = T.end()
                T.begin()
                if hf == 0:
                    phase_N(l, 1)
                elif l + 1 < n_layers:
                    layer_setup(l + 1)
                    phase_N(l + 1, 0)
                else:
                    phase_final(list(range(0, 8)), load_g=False)
                LN = T.end()
                lists_ = [LO, LN]
                if hf == 1 and l + 1 < n_layers:
                    T.begin()
                    phase_M(l + 1)
                    lists_.append(T.end())
                T.emit(lists_)
                flush_done()
        T.finish(okeys + ["dbg_" + n for n in dbg_d])
    return nc


def host_constants():
    idx = np.arange(128)
    same = (idx[:, None] // 64) == (idx[None, :] // 64)
    ident = np.eye(128, dtype=np.float32)
    U = (idx[:, None] <= idx[None, :]).astype(np.float32)
    LX = (idx[:, None] > idx[None, :]).astype(np.float32)
    invc = np.broadcast_to(1.0 / np.arange(1, 17, dtype=np.float32), (128, 16))
    lnc8 = np.broadcast_to(np.array([LNC] * 4 + [0.0] * 4, dtype=np.float32), (128, 8))
    sel = np.zeros((128, 128), np.float32)
    sel[127, :] = 1.0
    return np.ascontiguousarray(np.concatenate([ident, U, LX, invc, lnc8, sel], axis=1).astype(np.float32))


def host_layout(norm_g, b_gates, conv_w, conv_b, mlstm_norm_g, pool_scale, mem_norm_g, final_norm_g):
    Ln = norm_g.shape[0]
    sp = np.zeros((128, Ln, 56), np.float32)
    for l in range(Ln):
        cw = conv_w[l].reshape(4, 8, 128)
        sp[:, l, 0:32] = cw.transpose(2, 1, 0).reshape(128, 32)
        sp[:, l, 32:40] = conv_b[l].reshape(8, 128).T
        sp[:, l, 40:44] = mlstm_norm_g[l].reshape(4, 128).T
        sp[:, l, 44:48] = pool_scale[l].reshape(4, 128).T
        sp[:, l, 48:56] = np.broadcast_to(b_gates[l], (128, 8))
    gb = np.stack([np.broadcast_to(v, (128, D)) for v in (norm_g[0], norm_g[1], mem_norm_g[0], mem_norm_g[1], final_norm_g,
                                                            conv_b[0], conv_b[1])])
    return sp, np.ascontiguousarray(gb.astype(np.float32))


_CACHE = {}


def kernel(x, mem, norm_g, w_in, b_gates, conv_w, conv_b, mlstm_norm_g, pool_w, pool_scale,
           mem_norm_g, w_mem_kv, w_out, final_norm_g):
    f = lambda a: np.ascontiguousarray(np.asarray(a, dtype=np.float32))
    x, mem, w_in, w_mem_kv, w_out, pool_w = f(x), f(mem), f(w_in), f(w_mem_kv), f(w_out), f(pool_w)
    sp, gb = host_layout(f(norm_g), f(b_gates), f(conv_w), f(conv_b), f(mlstm_norm_g), f(pool_scale), f(mem_norm_g), f(final_norm_g))
    cst = host_constants()
    if "nc" not in _CACHE:
        _CACHE["nc"] = build_program()
    nc = _CACHE["nc"]
    n = x.shape[0]
    in_maps = [{"x": x[b], "mem": mem[b], "w_in": w_in, "w_mem_kv": w_mem_kv, "w_out": w_out, "pool_w": pool_w,
                "smallp": sp, "gb": gb, "cst": cst} for b in range(n)]
    res = run_bass_kernel_spmd(nc, in_maps, core_ids=list(range(n)))
    return np.stack([np.asarray(r["out"], dtype=np.float32) for r in res.results], axis=0)
```

```python
import contextlib
import math
import numpy as np
import concourse.bass as bass
import concourse.mybir as mybir
from concourse.bass_utils import run_bass_kernel_spmd

F32 = mybir.dt.float32
BF16 = mybir.dt.bfloat16
AF = mybir.ActivationFunctionType
ALU = mybir.AluOpType

S = 2048
D = 1024
NIN = 4616
NMEM = 256
EPS = 1e-6
O_QK, O_V, O_O, O_ZA, O_G, O_U, O_ZB, O_QC, O_ZC = 0, 1024, 1536, 2048, 2560, 2568, 3080, 3592, 4104
LNC = math.log(128 ** -0.5)
RING = 16
import os
PSUM_RES = {"MM0", "MM1", "PT", "BBP", "DCA", "DCB", "NUM", "MISC"}
WS_LAG = int(os.environ.get("WS_LAG", "0"))
PUMP_N = int(os.environ.get("PUMP_N", "2"))
BADD = os.environ.get("BADD", "dve")
BADD2 = os.environ.get("BADD2", "dve")
KW_ACT = bool(int(os.environ.get("KW_ACT", "0")))
SEQ_EMIT = bool(int(os.environ.get("SEQ_EMIT", "0")))


class Tracker:
    def __init__(self, nc, es):
        self.nc = nc
        self.es = es
        self.eng = {"pe": nc.tensor, "act": nc.scalar, "dve": nc.vector, "pool": nc.gpsimd, "sp": nc.sync}
        self.sem = {}
        self.cnt = {}
        self.known = {k: {} for k in self.eng}
        for k in self.eng:
            self.sem[k] = es.enter_context(nc.semaphore("s_" + k))
            self.cnt[k] = 0
        self.lastw = {}
        self.readers = {}
        self.nwait = 0

    def _key(self, key):
        if key not in self.sem:
            self.sem[key] = self.es.enter_context(self.nc.semaphore("s_" + key.replace(":", "_")))
            self.cnt[key] = 0
        return self.sem[key]

    def _deps(self, ename, R, W):
        deps = {}

        def add(k, v, war=False):
            if war and k == ename and ename == "pe":
                return
            if v > deps.get(k, 0):
                deps[k] = v

        for r in R:
            if r in self.lastw:
                add(*self.lastw[r])
        for w in W:
            if w in self.lastw:
                add(*self.lastw[w])
            for k, v in self.readers.get(w, {}).items():
                add(k, v, war=True)
        return deps

    def _wait(self, ename, deps):
        e = self.eng[ename]
        for k, v in deps.items():
            if k == ename and ename == "pe":
                continue
            if self.known[ename].get(k, 0) >= v:
                continue
            e.wait_ge(self.sem[k], v)
            self.nwait += 1
            self.known[ename][k] = v

    def _commit(self, key, val, R, W):
        for r in R:
            self.readers.setdefault(r, {})[key] = val
        for w in W:
            self.lastw[w] = (key, val)
            self.readers[w] = {}

    def begin(self):
        self.rec = []

    def end(self):
        r, self.rec = self.rec, None
        return r

    DEF_COST = {"pe": 0.35, "act": 0.42, "dve": 0.33, "pool": 0.5, "sp": 0.1}
    SYNC = 0.15

    def _m_norm(self, R, W):
        W2 = tuple(W) + tuple(r for r in R if r in PSUM_RES)
        R2 = tuple(r for r in R if r not in PSUM_RES)
        return R2, W2

    def _m_start(self, ename, R, W):
        if not hasattr(self, "m_free"):
            self.m_free, self.m_w, self.m_r, self.m_we = {}, {}, {}, {}
        t = self.m_free.get(ename, 0.0)
        for r in R:
            if r in self.m_w:
                t = max(t, self.m_w[r] + (self.SYNC if self.m_we.get(r) != ename else 0.05))
        for w in W:
            if w in self.m_w:
                t = max(t, self.m_w[w] + (self.SYNC if self.m_we.get(w) != ename else 0.0))
            if w in self.m_r:
                t = max(t, self.m_r[w] + self.SYNC)
        return t

    def _m_commit(self, ename, R, W, cost):
        R, W = self._m_norm(R, W)
        st = self._m_start(ename, R, W)
        en = st + cost
        self.m_free[ename] = en
        for r in R:
            self.m_r[r] = max(self.m_r.get(r, 0.0), en)
        for w in W:
            self.m_w[w] = en
            self.m_r[w] = 0.0
            self.m_we[w] = ename

    def emit(self, lists):
        lists = [l_ for l_ in lists if l_]
        if SEQ_EMIT:
            lists = [[x for l_ in lists for x in l_]]
        idx = [0] * len(lists)
        while True:
            best, bk = None, None
            for k, l_ in enumerate(lists):
                if idx[k] < len(l_):
                    kind, a = l_[idx[k]]
                    if kind == "op":
                        R, W = self._m_norm(a[2], a[3])
                        st = self._m_start(a[0], R, W)
                    elif kind == "call":
                        st = -1.0
                    elif kind == "wait":
                        if not a():
                            continue
                        st = -2.0
                    else:
                        st = self._m_start(a[0], (), ())
                    key = (st, (idx[k] + 0.5) / len(l_))
                    if best is None or key < best:
                        best, bk = key, k
            if bk is None:
                assert all(idx[k] >= len(l_) for k, l_ in enumerate(lists)), "emit: all remaining lists blocked"
                break
            kind, a = lists[bk][idx[bk]]
            idx[bk] += 1
            if kind == "op":
                self.op(*a)
            elif kind == "call":
                a()
            elif kind == "wait":
                pass
            else:
                self.dma(*a)

    def waitfor(self, pred):
        if getattr(self, "rec", None) is not None:
            self.rec.append(("wait", pred))
        else:
            assert pred()

    def call(self, fn):
        if getattr(self, "rec", None) is not None:
            self.rec.append(("call", fn))
        else:
            fn()

    def op(self, ename, fn, R=(), W=(), cost=None):
        if cost is None:
            cost = self.DEF_COST[ename]
        if getattr(self, "rec", None) is not None:
            self.rec.append(("op", (ename, fn, tuple(R), tuple(W), cost)))
            return
        self._m_commit(ename, R, W, cost)
        W = tuple(W) + tuple(r for r in R if r in PSUM_RES)
        R = tuple(r for r in R if r not in PSUM_RES)
        self._wait(ename, self._deps(ename, R, W))
        ins = fn(self.eng[ename])
        self.cnt[ename] += 1
        ins.then_inc(self.sem[ename], 1)
        self._commit(ename, self.cnt[ename], R, W)

    def dma(self, qname, pairs, R=(), W=(), key=None):
        if getattr(self, "rec", None) is not None:
            self.rec.append(("dma", (qname, pairs, tuple(R), tuple(W), key)))
            return
        key = "dma:" + (key or (W[0] if W else R[0]))
        self._m_commit(key, R, W, 2.5)
        sem = self._key(key)
        self._wait(qname, self._deps(qname, R, W))
        for (o, i) in pairs:
            self.eng[qname].dma_start(out=o, in_=i).then_inc(sem, 16)
            self.cnt[key] += 16
        self._commit(key, self.cnt[key], R, W)

    def finish(self, keys):
        for k in keys:
            k = "dma:" + k
            if k in self.cnt and self.cnt[k] > 0:
                self.eng["sp"].wait_ge(self.sem[k], self.cnt[k])


class WStream:
    def __init__(self, T, slots):
        self.T = T
        self.slots = slots
        self.blocks = []
        self.index = {}
        self.issued = 0

    def plan(self, key, fn):
        self.index[key] = len(self.blocks)
        self.blocks.append(fn)

    def _issue_upto(self, i):
        while self.issued <= i and self.issued < len(self.blocks):
            j = self.issued
            s = j % len(self.slots)
            pairs = self.blocks[j](self.slots[s])
            self.T.dma("pool", pairs, W=["W%d" % s], key="W%d" % s)
            self.issued += 1

    def start(self):
        self._issue_upto(len(self.slots) - 1)

    def get(self, key):
        i = self.index[key]
        self._issue_upto(i)
        s = i % len(self.slots)
        return self.slots[s], "W%d" % s

    def peek(self, key):
        i = self.index[key]
        s_ = i % len(self.slots)
        return self.slots[s_], "W%d" % s_

    def can_issue(self, key):
        i = self.index[key]
        return i < self.issued or i < len(self.slots) or i <= getattr(self, "limit", -1)

    def get_safe(self, key):
        T = self.T
        if getattr(T, "rec", None) is None:
            return self.get(key)
        i = self.index[key]
        T.waitfor(lambda: self.can_issue(key))
        T.call(lambda: self._issue_upto(i))
        return self.peek(key)

    def done(self, key):
        i = self.index[key]
        if not hasattr(self, "done_set"):
            self.done_set, self.first_undone = set(), 0
        self.done_set.add(i)
        while self.first_undone in self.done_set:
            self.first_undone += 1
        self.limit = max(getattr(self, "limit", -1), self.first_undone + len(self.slots) - 1 - WS_LAG)
        if not getattr(self, "throttle", False):
            self._issue_upto(self.limit)

    def pump(self, n):
        lim = min(getattr(self, "limit", -1), self.issued + n - 1)
        self._issue_upto(lim)


def build_program(n_layers=2, final_norm=True, dbg=None):
    dbg = dbg or {}
    nc = bass.Bass("TRN2", target_bir_lowering=False)
    L = 2
    x_d = nc.dram_tensor("x", [S, D], F32, kind="ExternalInput").ap()
    mem_d = nc.dram_tensor("mem", [NMEM, D], F32, kind="ExternalInput").ap()
    w_in_d = nc.dram_tensor("w_in", [L, D, NIN], F32, kind="ExternalInput").ap()
    w_kv_d = nc.dram_tensor("w_mem_kv", [L, D, 1024], F32, kind="ExternalInput").ap()
    w_out_d = nc.dram_tensor("w_out", [L, 1536, D], F32, kind="ExternalInput").ap()
    pool_w_d = nc.dram_tensor("pool_w", [L, 4, 128, 128], F32, kind="ExternalInput").ap()
    smallp_d = nc.dram_tensor("smallp", [128, L, 56], F32, kind="ExternalInput").ap()
    gb_d = nc.dram_tensor("gb", [7, 128, D], F32, kind="ExternalInput").ap()
    cst_d = nc.dram_tensor("cst", [128, 536], F32, kind="ExternalInput").ap()
    out_d = nc.dram_tensor("out", [S, D], F32, kind="ExternalOutput").ap()
    dbg_d = {}
    for name, shape in dbg.items():
        dbg_d[name] = nc.dram_tensor("dbg_" + name, list(shape), F32, kind="ExternalOutput").ap()

    with contextlib.ExitStack() as es:
        T = Tracker(nc, es)

        def sb(name, shape, dt):
            return es.enter_context(nc.sbuf_tensor(name, list(shape), dt))

        def ps(name, shape, dt):
            return es.enter_context(nc.psum_tensor(name, list(shape), dt))

        X = sb("X", [128, 16, D], F32)
        XNT = sb("XNT", [128, 8, 1024], BF16)
        YT = sb("YT", [128, 12, 1024], BF16)
        slots = [sb("W%d" % i, [128, 1024], BF16) for i in range(RING)]
        FBIG = sb("FBIG", [128, 8 * 528], F32)
        FT = [FBIG[:, i * 528:(i + 1) * 528] for i in range(8)]
        G0 = sb("G0", [128, 2048], BF16)
        G1 = sb("G1", [128, 2048], BF16)
        G2 = sb("G2", [128, 2048], BF16)
        XNK = [sb("XNK%d" % i, [128, 1024], BF16) for i in range(2)]
        MEMB = FBIG[:, 0:1024]
        CST = sb("CST", [128, 536], F32)
        IDB = sb("IDB", [128, 128], BF16)
        ONESF = sb("ONESF", [128, 128], F32)
        NEGH = sb("NEGH", [128, 32], F32)
        GX = sb("GX", [128, D], F32)
        GM = GX
        DG = sb("DG", [128, 4, 4, 128], BF16)
        CBROW = sb("CBROW", [1, 1024], BF16)
        ONESROW = sb("ONESROW", [1, 512], BF16)
        HALOB = sb("HALOB", [128, 8, 4], BF16)
        SPALL = sb("SPALL", [128, L, 56], F32)
        SPX2 = sb("SPX", [128, 2, 24], F32)
        PW = sb("PW", [128, 4, 128], BF16)
        WG = sb("WG", [128, 8, 8], BF16)
        KMT = sb("KMT", [128, 4, 256], BF16)
        VM = sb("VM", [128, 2, 4, 129], BF16)
        SS = sb("SS", [128, 16], F32)
        RSTD = sb("RSTD", [128, 16], F32)
        TMPS = sb("TMPS", [128, 16], F32)
        MSS = sb("MSS", [128, 8], F32)
        HALO_U = sb("HALO_U", [128, 4, 16], F32)
        GA = sb("GA", [128, 8, 8], F32)
        E1 = sb("E1", [128, 8, 4], F32)
        L1 = sb("L1", [128, 32], F32)
        TB = sb("TB", [128, 64], F32)
        EXPB = sb("EXPB", [128, 32], F32)
        WK = sb("WK", [128, 32], F32)
        VB = [sb("VB%d" % i, [128, 2, 129], BF16) for i in range(2)]
        THO = [sb("THO%d" % i, [128, 256], F32) for i in range(2)]
        EBT = sb("EBT", [128, 32], F32)
        IEB = sb("IEB", [128, 32], F32)
        DEC = sb("DEC", [128, 32], F32)
        STB = [sb("STB%d" % i, [128, 256], BF16) for i in range(2)]
        KW = sb("KW", [128, 2, 128], BF16)
        C32 = sb("C32", [128, 4, 129], F32)
        CBA = [sb("CBA%d" % i, [128, 2, 129], BF16) for i in range(2)]
        TTt = sb("TTt", [128, 256], F32)
        HGt = sb("HGt", [128, 256], F32)
        G3 = sb("G3", [128, 1536], BF16)
        SM = sb("SM", [128, 16], F32)
        YTK = [sb("YTK%d" % i, [128, 256], BF16) for i in range(2)]
        RDC = sb("RDC", [128, 4], F32)
        MM = [ps("MM0", [128, 512], F32), ps("MM1", [128, 512], F32)]
        PT = ps("PT", [128, 1024], BF16)
        BBP = ps("BBP", [128, 512], F32)
        DCA = ps("DCA", [128, 512], F32)
        DCB = ps("DCB", [128, 512], F32)
        NUM = ps("NUM", [128, 512], F32)
        MISC = ps("MISC", [128, 512], F32)
        mmi = [0]

        def next_mm():
            mmi[0] ^= 1
            return MM[mmi[0]], "MM%d" % mmi[0]

        U_ = CST[:, 128:256]
        LX_ = CST[:, 256:384]
        INVC = CST[:, 384:400]
        LNC8 = CST[:, 400:408]
        SEL_ = CST[:, 408:536]

        ws = WStream(T, slots)

        def wv(wd, l):
            return wd[l].rearrange("(kc p) n -> p kc n", p=128)

        def feat_block(wd, l, c0):
            def f(slot):
                return [(slot[:, :].rearrange("p (k c) -> p k c", c=128), wv(wd, l)[:, :, c0:c0 + 128])]
            return f

        def mov_block(wd, l, j, c0):
            def f(slot):
                return [(slot[:, :].rearrange("p (k c) -> p k c", c=512), wv(wd, l)[:, 2 * j:2 * j + 2, c0:c0 + 512])]
            return f

        def vo_block(l, j, hp):
            def f(slot):
                v = slot[:, :].rearrange("p (k c) -> p k c", c=512)
                w = wv(w_in_d, l)
                return [(v[:, :, 0:256], w[:, 2 * j:2 * j + 2, O_V + hp * 256:O_V + hp * 256 + 256]),
                        (v[:, :, 256:512], w[:, 2 * j:2 * j + 2, O_O + hp * 256:O_O + hp * 256 + 256])]
            return f

        def wo_block(l, kc):
            def f(slot):
                return [(slot[:, :], w_out_d[l, kc * 128:(kc + 1) * 128, :])]
            return f

        for l in range(n_layers):
            for h in range(4):
                ws.plan(("kvk", l, h), feat_block(w_kv_d, l, h * 128))
            for j in range(4):
                ws.plan(("kvv", l, j), mov_block(w_kv_d, l, j, 512))
            for hf in range(2):
                for j in range(4):
                    ws.plan(("zc", l, hf, j), mov_block(w_in_d, l, j, O_ZC))
                for h in range(4):
                    ws.plan(("qc", l, hf, h), feat_block(w_in_d, l, O_QC + h * 128))
                for g in range(4):
                    ws.plan(("u", l, hf, g), feat_block(w_in_d, l, O_U + g * 128))
                    ws.plan(("zb", l, hf, g), feat_block(w_in_d, l, O_ZB + g * 128))
                for hp in range(2):
                    for hh in range(2):
                        h = 2 * hp + hh
                        ws.plan(("q", l, hf, h), feat_block(w_in_d, l, O_QK + h * 128))
                        ws.plan(("k", l, hf, h), feat_block(w_in_d, l, O_QK + 512 + h * 128))
                    for hh in range(2):
                        ws.plan(("za", l, hf, 2 * hp + hh), feat_block(w_in_d, l, O_ZA + (2 * hp + hh) * 128))
                    for j in range(4):
                        ws.plan(("vo", l, hf, hp, j), vo_block(l, j, hp))
                for ch in range(2):
                    for j in range(6):
                        ws.plan(("wo", l, hf, ch, j), mov_block(w_out_d, l, j, ch * 512))

        T.dma("sp", [(CST[:, :], cst_d[:, :])], W=["CST"])
        T.dma("sp", [(SPALL[:, :, :], smallp_d[:, :, :])], W=["SPALL"])
        T.dma("pool", [(IDB[:, :], cst_d[:, 0:128])], W=["IDB"])
        T.op("dve", lambda e: e.memset(ONESF[:, :], 1.0), W=["ONESF"])
        T.op("dve", lambda e: e.memset(NEGH[:, :], -0.5), W=["NEGH"])
        T.op("dve", lambda e: e.memset(VM[:, :, :, 128:129], 2.0), W=["VM"])
        for p_ in range(2):
            T.op("dve", lambda e, p_=p_: e.memset(VB[p_][:, :, 128:129], 1.0), W=["VB%d" % p_])
        T.op("dve", lambda e: e.memset(ONESROW[:, :], 1.0), W=["ONESROW"])
        ws._issue_upto(7)
        T.dma("sp", [(FBIG[:, 2 * 528:2 * 528 + 1024], gb_d[2])], W=["F2", "F3"], key="GXM")
        T.dma("sp", [(MEMB, mem_d[0:128, :])], W=["F0", "F1"], key="MEMB")
        for t in range(8):
            T.dma("sp", [(X[:, t, :], x_d[t * 128:(t + 1) * 128, :])], W=["X%d" % t])
        pending_done = []

        def wdone(key):
            T.call(lambda: ws.done(key))

        def flush_done():
            for k_ in pending_done:
                ws.done(k_)
            del pending_done[:]
        mbi = [0]

        def next_mbank():
            mbi[0] ^= 1
            return (BBP, "BBP") if mbi[0] else (NUM, "NUM")

        def dump(name, ap_sb, res):
            if name in dbg_d:
                T.dma("pool", [(dbg_d[name], ap_sb)], R=res, key="dbg_" + name)

        def rstd_cols(ss_ap, out_ap, n, res_in, res_out, scale, eps):
            T.op("dve", lambda e: e.tensor_scalar(out=TMPS[:, 0:n], in0=ss_ap, scalar1=scale, scalar2=eps,
                                                   op0=ALU.mult, op1=ALU.add), R=res_in, W=["TMPS"])
            T.op("pool", lambda e: e.tensor_tensor(out=out_ap, in0=TMPS[:, 0:n], in1=NEGH[:, 0:n], op=ALU.pow),
                 R=["TMPS", "NEGH"], W=res_out)

        def norm_transpose(src_ap, src_res, rstd_ap, rstd_res, gtile, gres, par, dst_ap, dst_res):
            xn = XNK[par]
            T.op("dve", lambda e: e.scalar_tensor_tensor(out=xn[:, :], in0=src_ap, scalar=rstd_ap, in1=gtile[:, :],
                                                          op0=ALU.mult, op1=ALU.mult),
                 R=(src_res if isinstance(src_res, list) else [src_res]) + [rstd_res, gres], W=["XNK%d" % par], cost=1.25)

            def tr(e):
                ins = None
                for kc in range(8):
                    ins = e.transpose(PT[:, kc * 128:(kc + 1) * 128], xn[:, kc * 128:(kc + 1) * 128], IDB[:, :])
                return ins
            T.op("pe", tr, R=["XNK%d" % par, "IDB"], W=["PT"], cost=1.2)
            T.op("act", lambda e: e.copy(out=dst_ap, in_=PT[:, :].rearrange("p (k c) -> p k c", c=128)),
                 R=["PT"], W=(["G1a", "G1b"] if dst_res == "G1a" else [dst_res]), cost=1.0)

        def mm_feat(blk, XN_cols, out_ap):
            def f(e):
                ins = None
                bv = blk[:, :].rearrange("p (k c) -> p k c", c=128)
                for kc in range(8):
                    ins = e.matmul(out_ap, bv[:, kc, :], XNT[:, kc, XN_cols[0]:XN_cols[1]], start=(kc == 0), stop=(kc == 7))
                return ins
            return f

        def silu2_from_psum(pm, pm_res, n, th_tile, th_res, out_ap, out_res, c_act=0.65, c_dve=0.72):
            T.op("act", lambda e: e.activation(out=th_tile[:, 0:n], in_=pm[:, 0:n], func=AF.Tanh, scale=0.5),
                 R=[pm_res], W=[th_res], cost=c_act)
            T.op("dve", lambda e: e.scalar_tensor_tensor(out=out_ap, in0=th_tile[:, 0:n], scalar=1.0, in1=pm[:, 0:n],
                                                          op0=ALU.add, op1=ALU.mult),
                 R=[th_res, pm_res], W=(out_res if isinstance(out_res, list) else [out_res]), cost=c_dve)

        CWvs = [SPALL[:, l_, 0:32].rearrange("p (b j) -> p b j", j=4) for l_ in range(L)]

        def layer_setup(l):
            SPXl, sxr = SPX2[:, l % 2, :], "SPX%d" % (l % 2)
            T.dma("pool", [(CBROW[0:1, :], gb_d[5 + l][0:1, :])], W=["CBROW"])
            T.dma("pool", [(PW[:, :, :], pool_w_d[l].rearrange("g c d -> c g d"))], W=["PW"])
            T.dma("pool", [(WG[:, :, :], wv(w_in_d, l)[:, :, O_G:O_G + 8])], W=["WG"])
            SP = SPALL[:, l, :]
            T.op("dve", lambda e: e.tensor_tensor(out=SPXl[:, 0:8], in0=SP[:, 48:56], in1=LNC8, op=ALU.add),
                 R=["SPALL", "CST"], W=[sxr])
            T.op("dve", lambda e: e.tensor_scalar(out=SPXl[:, 8:12], in0=SP[:, 40:44], scalar1=1.0, scalar2=None, op0=ALU.mult),
                 R=["SPALL"], W=[sxr])
            T.op("dve", lambda e: e.tensor_scalar(out=SPXl[:, 12:16], in0=SP[:, 44:48], scalar1=0.5, scalar2=None, op0=ALU.mult),
                 R=["SPALL"], W=[sxr])
            T.op("dve", lambda e: e.memset(C32[:, :, :], 0.0), W=["C32"])
            T.op("dve", lambda e: e.memset(HALO_U[:, :, :], 0.0), W=["HALO_U"])
            T.op("dve", lambda e: e.memset(HALOB[:, :, :], 0.0), W=["HALOB"])

        def phase_M(l, preloaded=False):
            MNT = G1[:, :].rearrange("p (k c) -> p k c", c=256)
            GXM = FBIG[:, 2 * 528:2 * 528 + 1024]
            MNF = FBIG[:, 4 * 528:4 * 528 + 1024]
            if not preloaded:
                T.dma("sp", [(GXM, gb_d[2 + l])], W=["F2", "F3"], key="GXM")
            for mc in range(2):
                if not (preloaded and mc == 0):
                    T.dma("sp", [(MEMB, mem_d[mc * 128:(mc + 1) * 128, :])], W=["F0", "F1"], key="MEMB")
                T.op("act", lambda e, mc=mc: e.activation(out=MNF, in_=MEMB, func=AF.Square, accum_out=MSS[:, mc:mc + 1]),
                     R=["F0", "F1"], W=["MSS", "F4", "F5"], cost=1.2)
                T.op("dve", lambda e, mc=mc: e.tensor_scalar(out=TMPS[:, 15:16], in0=MSS[:, mc:mc + 1], scalar1=1.0 / D, scalar2=EPS,
                                                             op0=ALU.mult, op1=ALU.add), R=["MSS"], W=["TMPS"], cost=0.1)
                T.op("pool", lambda e, mc=mc: e.tensor_tensor(out=MSS[:, 4 + mc:5 + mc], in0=TMPS[:, 15:16], in1=NEGH[:, 0:1], op=ALU.pow),
                     R=["TMPS", "NEGH"], W=["MSS"], cost=0.7)
                T.op("dve", lambda e, mc=mc: e.scalar_tensor_tensor(out=MNF, in0=MEMB, scalar=MSS[:, 4 + mc:5 + mc], in1=GXM,
                                                                    op0=ALU.mult, op1=ALU.mult),
                     R=["F0", "F1", "MSS", "F2", "F3"], W=["F4", "F5"], cost=1.25)
                for half_, (bk, bkr) in enumerate(((BBP, "BBP"), (NUM, "NUM"))):
                    def tr(e, half_=half_, bk=bk):
                        ins = None
                        for k in range(4):
                            kc = half_ * 4 + k
                            ins = e.transpose(bk[:, k * 128:(k + 1) * 128], MNF[:, kc * 128:(kc + 1) * 128], CST[:, 0:128])
                        return ins
                    T.op("pe", tr, R=["F4", "F5", "CST"], W=[bkr], cost=1.0)
                    T.op("act", lambda e, half_=half_, bk=bk, mc=mc: e.copy(out=MNT[:, half_ * 4:half_ * 4 + 4, mc * 128:(mc + 1) * 128],
                                                                          in_=bk[:, :].rearrange("p (k c) -> p k c", c=128)),
                         R=[bkr], W=["G1a", "G1b"], cost=0.7)
            if preloaded:
                T.call(lambda: ws._issue_upto(11))
            for h in range(4):
                blk, bres = ws.get_safe(("kvk", l, h))
                pm, pres = next_mbank()

                def f(e, blk=blk, pm=pm):
                    ins = None
                    bv = blk[:, :].rearrange("p (k c) -> p k c", c=128)
                    for kc in range(8):
                        ins = e.matmul(pm[:, 0:256], bv[:, kc, :], MNT[:, kc, :], start=(kc == 0), stop=(kc == 7))
                    return ins
                T.op("pe", f, R=[bres, "G1a", "G1b"], W=[pres])
                wdone(("kvk", l, h))
                T.op("act", lambda e, pm=pm, h=h: e.activation(out=KMT[:, h, :], in_=pm[:, 0:256], func=AF.Identity, scale=128 ** -0.5),
                     R=[pres], W=["KMT"])
            vblk = [ws.get_safe(("kvv", l, j)) for j in range(4)]
            for mc in range(2):
                pm, pres = next_mbank()

                def f(e, pm=pm, mc=mc):
                    ins = None
                    for kc in range(8):
                        bv = vblk[kc // 2][0][:, :].rearrange("p (k c) -> p k c", c=512)
                        ins = e.matmul(pm[:, :], MNT[:, kc, mc * 128:(mc + 1) * 128], bv[:, kc % 2, :], start=(kc == 0), stop=(kc == 7))
                    return ins
                T.op("pe", f, R=["G1a", "G1b"] + [b_[1] for b_ in vblk], W=[pres])
                T.op("act", lambda e, pm=pm, mc=mc: e.copy(out=VM[:, mc, :, 0:128], in_=pm[:, :].rearrange("p (h e) -> p h e", e=128)),
                     R=[pres], W=["VM"])
            for j in range(4):
                wdone(("kvv", l, j))

        def phase_N(l, hf):
            t0 = hf * 8
            T.dma("sp", [(GX[:, :], gb_d[l])], W=["GX"])
            for tl in range(8):
                t = t0 + tl
                T.op("act", lambda e, t=t: e.activation(out=G2[:, (t % 2) * 1024:(t % 2) * 1024 + 1024], in_=X[:, t, :], func=AF.Square, accum_out=SS[:, t:t + 1]),
                     R=["X%d" % t], W=["SS"] + (["G2a"] if t % 2 == 0 else ["G2b", "G2c"]))
            rstd_cols(SS[:, t0:t0 + 8], RSTD[:, t0:t0 + 8], 8, ["SS"], ["RSTD"], 1.0 / D, EPS)
            if l == 0 and hf == 0:
                T.call(lambda: ws._issue_upto(RING - 1))
            for tl in range(8):
                t = t0 + tl
                norm_transpose(X[:, t, :], "X%d" % t, RSTD[:, t:t + 1], "RSTD", GX, "GX", tl % 2,
                               XNT[:, :, tl * 128:(tl + 1) * 128], "XNT%d" % (tl // 4))

        def phase_O(l, hf, fin=False):
            t0 = hf * 8
            SPXl, sxr = SPX2[:, l % 2, :], "SPX%d" % (l % 2)
            for ch in range(2):
                wob = [ws.get_safe(("wo", l, hf, ch, j)) for j in range(6)]
                wv_ = [b_[0][:, :].rearrange("p (k c) -> p k c", c=512) for b_ in wob]
                for kc in range(4):
                    T.op("dve", lambda e, kc=kc, wv_=wv_: e.tensor_scalar(out=wv_[kc // 2][:, kc % 2, :], in0=wv_[kc // 2][:, kc % 2, :],
                                                                scalar1=SPXl[:, 8 + kc:9 + kc], scalar2=None, op0=ALU.mult),
                         R=[wob[kc // 2][1], sxr], W=[wob[kc // 2][1]], cost=0.25)
                for tl in range(8):
                    t = t0 + tl
                    c0 = tl * 128
                    pm, pres = next_mm()

                    def f(e, pm=pm, c0=c0, wv_=wv_):
                        ins = None
                        for kc in range(12):
                            ins = e.matmul(pm[:, :], YT[:, kc, c0:c0 + 128], wv_[kc // 2][:, kc % 2, :],
                                           start=(kc == 0), stop=(kc == 11))
                        return ins
                    yres = ["YT%s%d" % (b_, tl // 4) for b_ in "abc"]
                    T.op("pe", f, R=yres + [b_[1] for b_ in wob], W=[pres], cost=2.8)
                    T.op("dve", lambda e, pm=pm, t=t, ch=ch: e.tensor_tensor(out=X[:, t, ch * 512:(ch + 1) * 512], in0=pm[:, :],
                                                                            in1=X[:, t, ch * 512:(ch + 1) * 512], op=ALU.add),
                         R=[pres, "X%d" % t], W=["X%d" % t], cost=0.7)
                    if fin and ch == 1:
                        final_tile(t, 8 + tl)
                for j in range(6):
                    wdone(("wo", l, hf, ch, j))

        okeys = []

        def final_tile(t, col):
            if final_norm:
                T.op("act", lambda e: e.activation(out=G2[:, (t % 2) * 1024:(t % 2) * 1024 + 1024], in_=X[:, t, :], func=AF.Square, accum_out=SS[:, t:t + 1]),
                     R=["X%d" % t], W=["SS"] + (["G2a"] if t % 2 == 0 else ["G2b", "G2c"]), cost=1.1)
                T.op("dve", lambda e: e.tensor_scalar(out=TMPS[:, col:col + 1], in0=SS[:, t:t + 1], scalar1=1.0 / D, scalar2=EPS,
                                                      op0=ALU.mult, op1=ALU.add), R=["SS"], W=["TMPS"], cost=0.1)
                T.op("pool", lambda e: e.tensor_tensor(out=RSTD[:, t:t + 1], in0=TMPS[:, col:col + 1], in1=NEGH[:, 0:1], op=ALU.pow),
                     R=["TMPS", "NEGH"], W=["RSTD"], cost=0.7)
                T.op("dve", lambda e: e.scalar_tensor_tensor(out=X[:, t, :], in0=X[:, t, :], scalar=RSTD[:, t:t + 1], in1=GX[:, :],
                                                             op0=ALU.mult, op1=ALU.mult), R=["X%d" % t, "RSTD", "GX"], W=["X%d" % t], cost=1.25)
            T.dma("sp", [(out_d[t * 128:(t + 1) * 128, :], X[:, t, :])], R=["X%d" % t], key="out%d" % t)
            okeys.append("out%d" % t)

        def phase_final(ts, load_g=True):
            n_ = len(ts)
            if final_norm:
                if load_g:
                    T.dma("sp", [(GX[:, :], gb_d[4])], W=["GX"])
                for t in ts:
                    T.op("act", lambda e, t=t: e.activation(out=G2[:, (t % 2) * 1024:(t % 2) * 1024 + 1024], in_=X[:, t, :], func=AF.Square, accum_out=SS[:, t:t + 1]),
                         R=["X%d" % t], W=["SS"] + (["G2a"] if t % 2 == 0 else ["G2b", "G2c"]))
                rstd_cols(SS[:, ts[0]:ts[0] + n_], RSTD[:, ts[0]:ts[0] + n_], n_, ["SS"], ["RSTD"], 1.0 / D, EPS)
                for t in ts:
                    T.op("dve", lambda e, t=t: e.scalar_tensor_tensor(out=X[:, t, :], in0=X[:, t, :], scalar=RSTD[:, t:t + 1], in1=GX[:, :],
                                                                     op0=ALU.mult, op1=ALU.mult), R=["X%d" % t, "RSTD", "GX"], W=["X%d" % t])
            for t in ts:
                T.dma("sp", [(out_d[t * 128:(t + 1) * 128, :], X[:, t, :])], R=["X%d" % t], key="out%d" % t)
                okeys.append("out%d" % t)

        layer_setup(0)
        T.begin()
        phase_M(0, preloaded=True)
        LM0 = T.end()
        T.begin()
        phase_N(0, 0)
        LN0 = T.end()
        T.emit([LM0, LN0])
        flush_done()
        ws._issue_upto(RING - 1)
        for t in range(8, 16):
            T.dma("sp", [(X[:, t, :], x_d[t * 128:(t + 1) * 128, :])], W=["X%d" % t])
        for l in range(n_layers):
            SPXl, sxr = SPX2[:, l % 2, :], "SPX%d" % (l % 2)
            CWv = CWvs[l]
            CBv = SPALL[:, l, 32:40]
            for hf in range(2):
                t0 = hf * 8
                first = (hf == 0)
                if l == 0 and hf == 0:
                    dump("xnt", XNT[:, :, :], ["XNT0", "XNT1"])

                zcb = [ws.get(("zc", l, hf, j)) for j in range(4)]
                qcb = [ws.get(("qc", l, hf, h)) for h in range(4)]
                YC = G0[:, :].rearrange("p (t c) -> p t c", c=512)
                SZC = G1[:, :].rearrange("p (t c) -> p t c", c=512)
                cb_ = [(MM[0], "MM0"), (NUM, "NUM")]
                ci_ = [0]

                def next_c():
                    ci_[0] = (ci_[0] + 1) % 2
                    return cb_[ci_[0]]
                listC = []
                for mt in range(2):
                    xres = "XNT%d" % mt
                    T.begin()
                    for tt in range(4):
                        c0 = mt * 512 + tt * 128
                        pm, pres = next_c()

                        def f(e, pm=pm, c0=c0):
                            ins = None
                            for kc in range(8):
                                bv = zcb[kc // 2][0][:, :].rearrange("p (k c) -> p k c", c=512)
                                ins = e.matmul(pm[:, :], XNT[:, kc, c0:c0 + 128], bv[:, kc % 2, :], start=(kc == 0), stop=(kc == 7))
                            return ins
                        T.op("pe", f, R=[xres] + [b[1] for b in zcb], W=[pres], cost=1.9)
                        th = FT[6 + tt % 2]
                        silu2_from_psum(pm, pres, 512, th, "F%d" % (6 + tt % 2), SZC[:, tt, :], "G1a" if tt < 2 else "G1b")
                    if mt == 1:
                        for j in range(4):
                            T.call(lambda j=j: ws.done(("zc", l, hf, j)))
                    Zl = T.end()
                    Pl, Vl = [], []
                    for h in range(4):
                        T.begin()
                        if h % 2 == 0:
                            PTB, QCB, ptr_, qcr_ = G2[:, 0:1024].rearrange("p (m c) -> p m c", c=512), G2[:, 1024:1536], "G2a", "G2b"
                        else:
                            PTB, QCB, ptr_, qcr_ = G3[:, 0:1024].rearrange("p (m c) -> p m c", c=512), G3[:, 1024:1536], "G3a", "G3b"
                        pm, pres = next_c()
                        T.op("pe", mm_feat(qcb[h][0], (mt * 512, mt * 512 + 512), pm[:, :]), R=[xres, qcb[h][1]], W=[pres], cost=1.9)
                        T.op("act", lambda e, pm=pm, QCB=QCB: e.copy(out=QCB, in_=pm[:, :]), R=[pres], W=[qcr_], cost=0.65)
                        for mc in range(2):
                            pm2, pres2 = next_c()
                            T.op("pe", lambda e, pm2=pm2, mc=mc, h=h, QCB=QCB: e.matmul(pm2[:, :], KMT[:, h, mc * 128:(mc + 1) * 128], QCB, start=True, stop=True),
                                 R=["KMT", qcr_], W=[pres2])
                            T.op("act", lambda e, pm2=pm2, mc=mc, PTB=PTB: e.activation(out=PTB[:, mc, :], in_=pm2[:, :], func=AF.Exp),
                                 R=[pres2], W=[ptr_], cost=0.65)
                        if mt == 1:
                            T.call(lambda h=h: ws.done(("qc", l, hf, h)))
                        Pl.append(T.end())
                        T.begin()
                        for tp in range(2):
                            bank, bres_ = (DCA, "DCA") if tp == 0 else (DCB, "DCB")

                            def f(e, bank=bank, tp=tp, h=h, PTB=PTB):
                                ins = None
                                for i in range(2):
                                    tt = tp * 2 + i
                                    for mc in range(2):
                                        ins = e.matmul(bank[:, i * 129:(i + 1) * 129], PTB[:, mc, tt * 128:(tt + 1) * 128],
                                                       VM[:, mc, h, :], start=(mc == 0), stop=(mc == 1))
                                return ins
                            T.op("pe", f, R=[ptr_, "VM"], W=[bres_], cost=0.5)
                            bv = bank[:, 0:258].rearrange("p (i e) -> p i e", e=129)
                            T.op("dve", lambda e, bv=bv, tp=tp: e.reciprocal(out=RDC[:, 2 * tp:2 * tp + 2], in_=bv[:, :, 128]), R=[bres_], W=["RDC%d" % tp], cost=0.2)
                            for i in range(2):
                                tt = tp * 2 + i
                                T.op("dve", lambda e, bv=bv, i=i, tt=tt, h=h, tp=tp: e.scalar_tensor_tensor(
                                    out=YC[:, tt, h * 128:(h + 1) * 128], in0=bv[:, i, 0:128], scalar=RDC[:, 2 * tp + i:2 * tp + i + 1],
                                    in1=SZC[:, tt, h * 128:(h + 1) * 128], op0=ALU.mult, op1=ALU.mult),
                                    R=[bres_, "RDC%d" % tp, "G1a", "G1b"], W=["G0a" if tt < 2 else "G0b"], cost=0.3)
                        Vl.append(T.end())
                    T.begin()
                    for tt in range(4):
                        c0 = mt * 512 + tt * 128

                        def tr(e, tt=tt):
                            ins = None
                            for h in range(4):
                                ins = e.transpose(PT[:, h * 128:(h + 1) * 128], YC[:, tt, h * 128:(h + 1) * 128], IDB[:, :])
                            return ins
                        T.op("pe", tr, R=["G0a", "G0b", "IDB"], W=["PT"])
                        T.op("act", lambda e, c0=c0: e.copy(out=YT[:, 8:12, c0:c0 + 128], in_=PT[:, 0:512].rearrange("p (h c) -> p h c", c=128)),
                             R=["PT"], W=["YTc%d" % mt])
                    TRl = T.end()
                    listC += Zl + Pl[0] + Pl[1] + Vl[0] + Pl[2] + Vl[1] + Pl[3] + Vl[2] + Vl[3] + TRl

                ub = [ws.get(("u", l, hf, g)) for g in range(4)]
                zbb = [ws.get(("zb", l, hf, g)) for g in range(4)]
                UW, S1, S2, THB, SZB, TMPF = FT[0], FT[1], FT[2], FT[3], FT[4], FT[5]
                Xl, Yl = [], []
                for mt in range(2):
                    xres = "XNT%d" % mt
                    for g in range(4):
                        T.begin()
                        win = 2 << g
                        PB, pbr = XNK[len(Xl) % 2][:, 0:512], "XNK%d" % (len(Xl) % 2)
                        pm, pres = MM[1], "MM1"
                        T.op("pe", mm_feat(ub[g][0], (mt * 512, mt * 512 + 512), pm[:, :]), R=[xres, ub[g][1]], W=[pres], cost=1.9)
                        T.op("dve", lambda e, g=g: e.tensor_copy(out=UW[:, 0:16], in_=HALO_U[:, g, :]), R=["HALO_U"], W=["F0"])
                        T.op("act", lambda e, pm=pm: e.copy(out=UW[:, 16:528], in_=pm[:, :]), R=[pres], W=["F0"], cost=0.65)
                        T.op(BADD, lambda e: e.tensor_tensor(out=S1[:, 1:528], in0=UW[:, 1:528], in1=UW[:, 0:527], op=ALU.add),
                             R=["F0"], W=["F1"], cost=0.72 if BADD == "dve" else 1.1)
                        cur, cres = S1, "F1"
                        oth, ores = S2, "F2"
                        lo, sh = 1, 2
                        for step in range(g):
                            nlo = lo + sh
                            T.op(BADD if step % 2 == 0 else BADD2, lambda e, cur=cur, oth=oth, nlo=nlo, sh=sh: e.tensor_tensor(
                                out=oth[:, nlo:528], in0=cur[:, nlo:528], in1=cur[:, nlo - sh:528 - sh], op=ALU.add),
                                R=[cres], W=[ores], cost=0.72)
                            cur, cres, oth, ores = oth, ores, cur, cres
                            lo, sh = nlo, sh * 2
                        T.op("dve", lambda e, cur=cur, win=win, PB=PB: e.scalar_tensor_tensor(
                            out=PB, in0=cur[:, 16:528], scalar=1.0 / win, in1=UW[:, 16:528], op0=ALU.mult, op1=ALU.subtract),
                            R=[cres, "F0"], W=[pbr], cost=0.7)
                        if first and mt == 0:
                            w1 = win - 1
                            T.op("dve", lambda e, cur=cur, w1=w1: e.tensor_tensor(out=TMPF[:, 0:w1], in0=cur[:, 16:16 + w1], in1=INVC[:, 0:w1], op=ALU.mult),
                                 R=[cres, "CST"], W=["F5"])
                            T.op("dve", lambda e, w1=w1, PB=PB: e.tensor_tensor(out=PB[:, 0:w1], in0=TMPF[:, 0:w1], in1=UW[:, 16:16 + w1], op=ALU.subtract),
                                 R=["F5", "F0"], W=[pbr])
                        T.op("dve", lambda e, g=g: e.tensor_copy(out=HALO_U[:, g, :], in_=UW[:, 512:528]), R=["F0"], W=["HALO_U"])
                        Xl.append(T.end())
                        T.begin()
                        pm2, pres2 = BBP, "BBP"
                        T.op("pe", lambda e, pm2=pm2, g=g, PB=PB: e.matmul(pm2[:, :], PW[:, g, :], PB, start=True, stop=True), R=["PW", pbr], W=[pres2])
                        T.op("pe", mm_feat(zbb[g][0], (mt * 512, mt * 512 + 512), MM[1][:, :]), R=[xres, zbb[g][1]], W=["MM1"], cost=1.9)
                        silu2_from_psum(MM[1], "MM1", 512, THB, "F3", SZB[:, 0:512], "F4")
                        T.op("dve", lambda e, pm2=pm2, g=g, mt=mt: e.scalar_tensor_tensor(
                            out=YT[:, 4 + g, mt * 512:(mt + 1) * 512], in0=pm2[:, :], scalar=SPXl[:, 12 + g:13 + g], in1=SZB[:, 0:512],
                            op0=ALU.mult, op1=ALU.mult), R=[pres2, sxr, "F4"], W=["YTb%d" % mt], cost=0.7)
                        if mt == 1:
                            T.call(lambda g=g: (ws.done(("u", l, hf, g)), ws.done(("zb", l, hf, g))))
                        Yl.append(T.end())
                listB = list(Xl[0])
                for k_ in range(8):
                    if k_ + 1 < 8:
                        listB += Xl[k_ + 1]
                    listB += Yl[k_]
                T.begin()
                def f(e):
                    ins = None
                    for tl in range(8):
                        for kc in range(8):
                            ins = e.matmul(MISC[:, tl * 8:(tl + 1) * 8], XNT[:, kc, tl * 128:(tl + 1) * 128], WG[:, kc, :],
                                           start=(kc == 0), stop=(kc == 7))
                    return ins
                T.op("pe", f, R=["XNT0", "XNT1", "WG"], W=["MISC"])
                T.op("dve", lambda e: e.tensor_tensor(out=GA[:, :, :], in0=MISC[:, 0:64].rearrange("p (t g) -> p t g", g=8),
                                                      in1=SPXl[:, 0:8].unsqueeze(1).to_broadcast([128, 8, 8]), op=ALU.add),
                     R=["MISC", sxr], W=["GA"])
                T.op("act", lambda e: e.activation(out=E1[:, :, :], in_=GA[:, :, 4:8], func=AF.Exp, scale=-1.0), R=["GA"], W=["E1"])
                T.op("act", lambda e: e.activation(out=L1[:, :].rearrange("p (t h) -> p t h", h=4), in_=E1[:, :, :], func=AF.Ln, bias=1.0),
                     R=["E1"], W=["L1"])
                T.op("pe", lambda e: e.matmul(MISC[:, 64:96], U_, L1[:, :], start=True, stop=True), R=["CST", "L1"], W=["MISC"])
                T.op("pe", lambda e: e.matmul(MISC[:, 96:128], LX_, L1[:, :], start=True, stop=True), R=["CST", "L1"], W=["MISC"])
                IGC = GA[:, :, 0:4]
                T.op("dve", lambda e: e.tensor_tensor(out=TB[:, 0:32].rearrange("p (t h) -> p t h", h=4), in0=IGC,
                                                      in1=MISC[:, 64:96].rearrange("p (t h) -> p t h", h=4), op=ALU.add),
                     R=["GA", "MISC"], W=["TB"])
                T.op("dve", lambda e: e.tensor_tensor(out=TB[:, 32:64].rearrange("p (t h) -> p t h", h=4), in0=IGC,
                                                      in1=MISC[:, 96:128].rearrange("p (t h) -> p t h", h=4), op=ALU.subtract),
                     R=["GA", "MISC"], W=["TB"])
                T.op("act", lambda e: e.activation(out=EXPB[:, :], in_=TB[:, 0:32], func=AF.Exp), R=["TB"], W=["EXPB"])
                T.op("act", lambda e: e.activation(out=WK[:, :], in_=TB[:, 32:64], func=AF.Exp), R=["TB"], W=["WK"])
                T.op("act", lambda e: e.activation(out=EBT[:, :], in_=MISC[:, 64:96], func=AF.Exp, scale=-1.0), R=["MISC"], W=["EBT"])
                T.op("act", lambda e: e.activation(out=IEB[:, :], in_=MISC[:, 64:96], func=AF.Exp), R=["MISC"], W=["IEB"])
                T.op("pe", lambda e: e.matmul(MISC[:, 128:160], SEL_, EBT[:, :], start=True, stop=True), R=["CST", "EBT"], W=["MISC"])
                T.op("act", lambda e: e.copy(out=DEC[:, :], in_=MISC[:, 128:160]), R=["MISC"], W=["DEC"])

                listG = T.end()
                T.emit([listC, listB, listG])
                if l == 0 and hf == 0:
                    dump("ytc", YT[:, 8:12, :], ["YTc0", "YTc1"])
                if l == 0 and hf == 0:
                    dump("ytb", YT[:, 4:8, :], ["YTb0", "YTb1"])

                QTm = [G0[:, 0:1024].rearrange("p (h c) -> p h c", c=512), G2[:, 0:1024].rearrange("p (h c) -> p h c", c=512)]
                KTm = [G0[:, 1024:2048].rearrange("p (h c) -> p h c", c=512), G2[:, 1024:2048].rearrange("p (h c) -> p h c", c=512)]
                QTr = ["G0a", "G2a"]
                KTr = [["G0b"], ["G2b", "G2c"]]
                SZAm = [G1[:, 1024:2048].rearrange("p (h c) -> p h c", c=512), G1[:, 0:1024].rearrange("p (h c) -> p h c", c=512)]
                SZAr = ["G1b", "G1a"]
                wts = {}

                def fetch_weights(hp, with_vo=True):
                    wts[hp] = ([ws.get(("q", l, hf, 2 * hp + hh)) for hh in range(2)],
                               [ws.get(("k", l, hf, 2 * hp + hh)) for hh in range(2)],
                               [ws.get(("za", l, hf, 2 * hp + hh)) for hh in range(2)],
                               [ws.get(("vo", l, hf, hp, j)) for j in range(4)] if with_vo else None)

                def build_dg(hp, bps=(0, 1, 2, 3)):
                    for bp in bps:
                        blk_i = 4 * (bp % 2) + 2 * hp + bp // 2
                        T.op("dve", lambda e, bp=bp, blk_i=blk_i: e.tensor_tensor(
                            out=DG[:, bp, :, :], in0=IDB[:, :].unsqueeze(1).to_broadcast([128, 4, 128]),
                            in1=CWv[:, blk_i, :].unsqueeze(2).to_broadcast([128, 4, 128]), op=ALU.mult),
                            R=["IDB", "SPALL"], W=["DG%d" % bp], cost=0.7)

                def prepass_block(hp, mt, bidx):
                    qb, kb, zab, vob = wts[hp]
                    xres = "XNT%d" % mt
                    mp = mt % 2
                    if bidx < 4:
                        hh, isk = bidx // 2, bidx % 2
                        h = 2 * hp + hh
                        blk_i = 4 * isk + h
                        wb = (kb if isk else qb)[hh]
                        RAWB, rres = XNK[bidx % 2], "XNK%d" % (bidx % 2)
                        TH, tres = FT[4 + bidx % 2], "F%d" % (4 + bidx % 2)
                        pm, pres = MM[1], "MM1"
                        T.op("pe", mm_feat(wb[0], (mt * 512, mt * 512 + 512), pm[:, :]), R=[xres, wb[1]], W=[pres], cost=1.9)
                        T.op("dve", lambda e: e.tensor_copy(out=RAWB[:, 0:3], in_=HALOB[:, blk_i, 0:3]), R=["HALOB"], W=[rres], cost=0.1)
                        T.op("act", lambda e: e.copy(out=RAWB[:, 3:515], in_=pm[:, :]), R=[pres], W=[rres], cost=0.65)
                        T.op("act", lambda e: e.copy(out=HALOB[:, blk_i, 0:3], in_=RAWB[:, 512:515]), R=[rres], W=["HALOB"], cost=0.2)
                        pc, pcres = DCB, "DCB"

                        def f(e):
                            ins = None
                            for j in range(4):
                                ins = e.matmul(pc[:, :], DG[:, bidx, j, :], RAWB[:, j:j + 512], start=(j == 0), stop=(j == 3))
                            return ins
                        T.op("pe", f, R=["DG%d" % bidx, rres], W=[pcres], cost=0.95)
                        dstv = (KTm if isk else QTm)[mp][:, hh, :]
                        dstres = KTr[mp] if isk else [QTr[mp]]
                        cbv = CBv
                        T.op("act", lambda e: e.activation(out=dstv, in_=pc[:, 0:512], func=AF.Silu, bias=cbv[:, blk_i:blk_i + 1]),
                             R=[pcres, "SPALL"], W=dstres, cost=0.65)
                    else:
                        hh = bidx - 4
                        pm, pres = MM[1], "MM1"
                        T.op("pe", mm_feat(zab[hh][0], (mt * 512, mt * 512 + 512), pm[:, :]), R=[xres, zab[hh][1]], W=[pres], cost=1.9)
                        T.op("act", lambda e: e.activation(out=SZAm[mp][:, hh, :], in_=pm[:, 0:512], func=AF.Silu),
                             R=[pres], W=[SZAr[mp]], cost=0.65)

                def front(g):
                    hp, i = g // 8, g % 8
                    qb, kb, zab, vob = wts[hp]
                    mt, tt = i // 4, i % 4
                    mp = mt % 2
                    p = g % 2
                    xres = "XNT%d" % mt
                    QT, KT = QTm[mp], KTm[mp]
                    qres, kres = QTr[mp], KTr[mp]
                    c0 = i * 128
                    cm = tt * 128
                    vb, vbr = VB[p], "VB%d" % p
                    T.op("act", lambda e: e.copy(out=CBA[p][:, :, :], in_=C32[:, 2 * hp:2 * hp + 2, :]), R=["C32"], W=["CBA%d" % p])
                    pm, pres = MM[0], "MM0"

                    def f(e):
                        ins = None
                        for kc in range(8):
                            bv = vob[kc // 2][0][:, :].rearrange("p (k c) -> p k c", c=512)
                            ins = e.matmul(pm[:, :], XNT[:, kc, c0:c0 + 128], bv[:, kc % 2, :], start=(kc == 0), stop=(kc == 7))
                        return ins
                    T.op("pe", f, R=[xres] + [b_[1] for b_ in vob], W=[pres], cost=1.9)
                    gi = [i * 4 + 2 * hp + hh for hh in range(2)]

                    def tr(e):
                        ins = None
                        for hh in range(2):
                            ins = e.transpose(PT[:, hh * 128:(hh + 1) * 128], KT[:, hh, cm:cm + 128], IDB[:, :])
                        return ins
                    T.op("pe", tr, R=kres + ["IDB"], W=["PT"])
                    T.op("act", lambda e: e.copy(out=vb[:, :, 0:128], in_=pm[:, 0:256].rearrange("p (h e) -> p h e", e=128)),
                         R=[pres], W=[vbr])
                    T.op("act", lambda e: e.activation(out=THO[p][:, :], in_=pm[:, 256:512], func=AF.Tanh, scale=0.5),
                         R=[pres], W=["THO%d" % p])
                    if KW_ACT:
                        for hh in range(2):
                            T.op("act", lambda e, hh=hh: e.activation(out=KW[:, hh, :], in_=PT[:, hh * 128:(hh + 1) * 128], func=AF.Copy,
                                                                     scale=WK[:, gi[hh]:gi[hh] + 1]),
                                 R=["PT", "WK"], W=["KW%d" % hh], cost=0.3)
                    else:
                        T.op("dve", lambda e: e.tensor_tensor(
                            out=KW[:, :, :], in0=PT[:, 0:256].rearrange("p (h c) -> p h c", c=128),
                            in1=WK[:, gi[0]:gi[0] + 2].unsqueeze(2).to_broadcast([128, 2, 128]), op=ALU.mult),
                            R=["PT", "WK"], W=["KW0", "KW1"], cost=0.42)

                    def f(e):
                        ins = None
                        for hh in range(2):
                            ins = e.matmul(DCA[:, hh * 129:(hh + 1) * 129], KW[:, hh, :], vb[:, hh, :], start=True, stop=True)
                        return ins
                    T.op("pe", f, R=["KW0", "KW1", vbr], W=["DCA"], cost=0.35)

                    def f(e):
                        ins = None
                        for hh in range(2):
                            ins = e.matmul(BBP[:, hh * 128:(hh + 1) * 128], KT[:, hh, cm:cm + 128], QT[:, hh, cm:cm + 128], start=True, stop=True)
                        return ins
                    T.op("pe", f, R=kres + [qres], W=["BBP"], cost=0.3)
                    for hh in range(2):
                        h = 2 * hp + hh
                        T.op("dve", lambda e, hh=hh, h=h: e.scalar_tensor_tensor(
                            out=C32[:, h, :], in0=C32[:, h, :], scalar=DEC[:, gi[hh]:gi[hh] + 1],
                            in1=DCA[:, hh * 129:(hh + 1) * 129], op0=ALU.mult, op1=ALU.add), R=["C32", "DEC", "DCA"], W=["C32"], cost=0.36)
                    for hh in range(2):
                        T.op("dve", lambda e, hh=hh, g_=gi[hh]: e.scalar_tensor_tensor(
                            out=STB[p][:, hh * 128:(hh + 1) * 128], in0=BBP[:, hh * 128:(hh + 1) * 128], scalar=EXPB[:, g_:g_ + 1],
                            in1=U_, op0=ALU.mult, op1=ALU.mult), R=["BBP", "EXPB", "CST"], W=["STB%d" % p], cost=0.27)

                def back_a(g):
                    hp, i = g // 8, g % 8
                    p = g % 2
                    vb, vbr = VB[p], "VB%d" % p
                    mp = (i // 4) % 2
                    QT, qres = QTm[mp], QTr[mp]
                    cm = (i % 4) * 128
                    gi0 = i * 4 + 2 * hp

                    def f(e):
                        ins = None
                        for hh in range(2):
                            o_ = NUM[:, hh * 129:(hh + 1) * 129]
                            e.matmul(o_, STB[p][:, hh * 128:(hh + 1) * 128], vb[:, hh, :], start=True, stop=False)
                            ins = e.matmul(o_, QT[:, hh, cm:cm + 128], CBA[p][:, hh, :], start=False, stop=True)
                        return ins
                    T.op("pe", f, R=["STB%d" % p, vbr, qres, "CBA%d" % p], W=["NUM"], cost=0.5)
                    NV = NUM[:, 0:258].rearrange("p (h e) -> p h e", e=129)
                    T.op("act", lambda e: e.activation(out=SM[:, 0:2], in_=NV[:, :, 128], func=AF.Abs), R=["NUM"], W=["SM"])
                    T.op("dve", lambda e: e.scalar_tensor_tensor(
                        out=TTt[:, :].rearrange("p (h e) -> p h e", e=128), in0=THO[p][:, :].rearrange("p (h e) -> p h e", e=128),
                        scalar=1.0, in1=NV[:, :, 0:128], op0=ALU.add, op1=ALU.mult), R=["THO%d" % p, "NUM"], W=["TTt0", "TTt1"])
                    T.op("dve", lambda e: e.tensor_tensor(out=SM[:, 2:4], in0=SM[:, 0:2], in1=IEB[:, gi0:gi0 + 2], op=ALU.max),
                         R=["SM", "IEB"], W=["SMb"], cost=0.1)
                    T.op("dve", lambda e: e.tensor_tensor(out=SM[:, 4:6], in0=SM[:, 2:4], in1=SM[:, 2:4], op=ALU.mult), R=["SMb"], W=["SMc"], cost=0.1)
                    for hh in range(2):
                        T.op("act", lambda e, hh=hh: e.activation(out=HGt[:, hh * 128:(hh + 1) * 128], in_=TTt[:, hh * 128:(hh + 1) * 128], func=AF.Square,
                                                                 scale=128 ** -0.5, accum_out=SM[:, 6 + hh:7 + hh]),
                             R=["TTt%d" % hh], W=["SMd%d" % hh, "HGt%d" % hh])
                    T.op("dve", lambda e: e.scalar_tensor_tensor(out=SM[:, 8:10], in0=SM[:, 4:6], scalar=4 * EPS, in1=SM[:, 6:8],
                                                                 op0=ALU.mult, op1=ALU.add), R=["SMc", "SMd0", "SMd1"], W=["SMe"], cost=0.1)
                    T.op("pool", lambda e: e.tensor_tensor(out=SM[:, 10:12], in0=SM[:, 8:10], in1=NEGH[:, 0:2], op=ALU.pow),
                         R=["SMe", "NEGH"], W=["SMf"], cost=0.7)
                    T.op("dve", lambda e: e.tensor_tensor(
                        out=YTK[p][:, :].rearrange("p (h e) -> p h e", e=128), in0=TTt[:, :].rearrange("p (h e) -> p h e", e=128),
                        in1=SM[:, 10:12].unsqueeze(2).to_broadcast([128, 2, 128]), op=ALU.mult),
                        R=["TTt0", "TTt1", "SMf"], W=["YTK%d" % p], cost=0.42)

                def back_b(g):
                    hp, i = g // 8, g % 8
                    mt, tt = i // 4, i % 4
                    mp = mt % 2
                    p = g % 2
                    c0 = i * 128
                    cm = tt * 128

                    def tr(e):
                        ins = None
                        for hh in range(2):
                            ins = e.transpose(PT[:, 256 + hh * 128:384 + hh * 128], YTK[p][:, hh * 128:(hh + 1) * 128], IDB[:, :])
                        return ins
                    T.op("pe", tr, R=["YTK%d" % p, "IDB"], W=["PT"])
                    T.op("dve", lambda e: e.tensor_tensor(
                        out=YT[:, 2 * hp:2 * hp + 2, c0:c0 + 128], in0=PT[:, 256:512].rearrange("p (h c) -> p h c", c=128),
                        in1=SZAm[mp][:, :, cm:cm + 128], op=ALU.mult), R=["PT", SZAr[mp]], W=["YTa%d" % mt])

                def rec(fn, *a):
                    T.begin()
                    fn(*a)
                    return T.end()

                fetch_weights(0)
                ws.throttle = True
                build_dg(0)
                for bidx in range(6):
                    prepass_block(0, 0, bidx)
                front(0)
                for g in range(16):
                    hp, i = g // 8, g % 8
                    lists = []
                    if g == 7:
                        fetch_weights(1)
                    if g + 1 < 16:
                        lists.append(rec(front, g + 1))
                    lists.append(rec(back_a, g))
                    if g >= 1:
                        lists.append(rec(back_b, g - 1))
                    if i < 3:
                        lists.append(rec(lambda: (prepass_block(hp, 1, 2 * i), prepass_block(hp, 1, 2 * i + 1),
                                                  (build_dg(1, (0, 1)) if (hp == 0 and i == 2) else None))))
                    elif hp == 0 and i == 3:
                        lists.append(rec(lambda: build_dg(1, (2, 3))))
                    elif hp == 0 and 4 <= i < 7:
                        if i == 4:
                            fetch_weights(1, with_vo=False)
                        k_ = i - 4
                        lists.append(rec(lambda: (prepass_block(1, 0, 2 * k_), prepass_block(1, 0, 2 * k_ + 1))))

                    T.emit(lists)
                    if i == 3:
                        for hh in range(2):
                            ws.done(("q", l, hf, 2 * hp + hh))
                            ws.done(("k", l, hf, 2 * hp + hh))
                        for hh in range(2):
                            ws.done(("za", l, hf, 2 * hp + hh))
                    if i == 7:
                        for j in range(4):
                            ws.done(("vo", l, hf, hp, j))
                    ws.pump(PUMP_N)
                T.emit([rec(back_b, 15)])
                ws.throttle = False
                ws._issue_upto(ws.limit)
                if l == 0 and hf == 0:
                    dump("yta", YT[:, 0:4, :], ["YTa0", "YTa1"])

                last = (hf == 1 and l + 1 == n_layers)
                if last and final_norm:
                    T.dma("sp", [(GX[:, :], gb_d[4])], W=["GX"])
                T.begin()
                phase_O(l, hf, fin=last)
                LO = T.end()
                T.begin()
                if hf == 0:
                    phase_N(l, 1)
                elif l + 1 < n_layers:
                    layer_setup(l + 1)
                    phase_N(l + 1, 0)
                else:
                    phase_final(list(range(0, 8)), load_g=False)
                LN = T.end()
                lists_ = [LO, LN]
                if hf == 1 and l + 1 < n_layers:
                    T.begin()
                    phase_M(l + 1)
                    lists_.append(T.end())
                T.emit(lists_)
                flush_done()
        T.finish(okeys + ["dbg_" + n for n in dbg_d])
    return nc


def host_constants():
    idx = np.arange(128)
    same = (idx[:, None] // 64) == (idx[None, :] // 64)
    ident = np.eye(128, dtype=np.float32)
    U = (idx[:, None] <= idx[None, :]).astype(np.float32)
    LX = (idx[:, None] > idx[None, :]).astype(np.float32)
    invc = np.broadcast_to(1.0 / np.arange(1, 17, dtype=np.float32), (128, 16))
    lnc8 = np.broadcast_to(np.array([LNC] * 4 + [0.0] * 4, dtype=np.float32), (128, 8))
    sel = np.zeros((128, 128), np.float32)
    sel[127, :] = 1.0
    return np.ascontiguousarray(np.concatenate([ident, U, LX, invc, lnc8, sel], axis=1).astype(np.float32))


def host_layout(norm_g, b_gates, conv_w, conv_b, mlstm_norm_g, pool_scale, mem_norm_g, final_norm_g):
    Ln = norm_g.shape[0]
    sp = np.zeros((128, Ln, 56), np.float32)
    for l in range(Ln):
        cw = conv_w[l].reshape(4, 8, 128)
        sp[:, l, 0:32] = cw.transpose(2, 1, 0).reshape(128, 32)
        sp[:, l, 32:40] = conv_b[l].reshape(8, 128).T
        sp[:, l, 40:44] = mlstm_norm_g[l].reshape(4, 128).T
        sp[:, l, 44:48] = pool_scale[l].reshape(4, 128).T
        sp[:, l, 48:56] = np.broadcast_to(b_gates[l], (128, 8))
    gb = np.stack([np.broadcast_to(v, (128, D)) for v in (norm_g[0], norm_g[1], mem_norm_g[0], mem_norm_g[1], final_norm_g,
                                                            conv_b[0], conv_b[1])])
    return sp, np.ascontiguousarray(gb.astype(np.float32))


_CACHE = {}


def kernel(x, mem, norm_g, w_in, b_gates, conv_w, conv_b, mlstm_norm_g, pool_w, pool_scale,
           mem_norm_g, w_mem_kv, w_out, final_norm_g):
    f = lambda a: np.ascontiguousarray(np.asarray(a, dtype=np.float32))
    x, mem, w_in, w_mem_kv, w_out, pool_w = f(x), f(mem), f(w_in), f(w_mem_kv), f(w_out), f(pool_w)
    sp, gb = host_layout(f(norm_g), f(b_gates), f(conv_w), f(conv_b), f(mlstm_norm_g), f(pool_scale), f(mem_norm_g), f(final_norm_g))
    cst = host_constants()
    if "nc" not in _CACHE:
        _CACHE["nc"] = build_program()
    nc = _CACHE["nc"]
    n = x.shape[0]
    in_maps = [{"x": x[b], "mem": mem[b], "w_in": w_in, "w_mem_kv": w_mem_kv, "w_out": w_out, "pool_w": pool_w,
                "smallp": sp, "gb": gb, "cst": cst} for b in range(n)]
    res = run_bass_kernel_spmd(nc, in_maps, core_ids=list(range(n)))
    return np.stack([np.asarray(r["out"], dtype=np.float32) for r in res.results], axis=0)
```

```python
import contextlib
import math
import numpy as np
import concourse.bass as bass
import concourse.mybir as mybir
from concourse.bass_utils import run_bass_kernel_spmd

F32 = mybir.dt.float32
BF16 = mybir.dt.bfloat16
AF = mybir.ActivationFunctionType
ALU = mybir.AluOpType

S = 2048
D = 1024
NIN = 4616
NMEM = 256
EPS = 1e-6
O_QK, O_V, O_O, O_ZA, O_G, O_U, O_ZB, O_QC, O_ZC = 0, 1024, 1536, 2048, 2560, 2568, 3080, 3592, 4104
LNC = math.log(128 ** -0.5)
RING = 16
import os
PSUM_RES = {"MM0", "MM1", "PT", "BBP", "DCA", "DCB", "NUM", "MISC"}
WS_LAG = int(os.environ.get("WS_LAG", "0"))
PUMP_N = int(os.environ.get("PUMP_N", "2"))
BADD = os.environ.get("BADD", "dve")
BADD2 = os.environ.get("BADD2", "dve")
KW_ACT = bool(int(os.environ.get("KW_ACT", "0")))
SEQ_EMIT = bool(int(os.environ.get("SEQ_EMIT", "0")))


class Tracker:
    def __init__(self, nc, es):
        self.nc = nc
        self.es = es
        self.eng = {"pe": nc.tensor, "act": nc.scalar, "dve": nc.vector, "pool": nc.gpsimd, "sp": nc.sync}
        self.sem = {}
        self.cnt = {}
        self.known = {k: {} for k in self.eng}
        for k in self.eng:
            self.sem[k] = es.enter_context(nc.semaphore("s_" + k))
            self.cnt[k] = 0
        self.lastw = {}
        self.readers = {}
        self.nwait = 0

    def _key(self, key):
        if key not in self.sem:
            self.sem[key] = self.es.enter_context(self.nc.semaphore("s_" + key.replace(":", "_")))
            self.cnt[key] = 0
        return self.sem[key]

    def _deps(self, ename, R, W):
        deps = {}

        def add(k, v, war=False):
            if war and k == ename and ename == "pe":
                return
            if v > deps.get(k, 0):
                deps[k] = v

        for r in R:
            if r in self.lastw:
                add(*self.lastw[r])
        for w in W:
            if w in self.lastw:
                add(*self.lastw[w])
            for k, v in self.readers.get(w, {}).items():
                add(k, v, war=True)
        return deps

    def _wait(self, ename, deps):
        e = self.eng[ename]
        for k, v in deps.items():
            if k == ename and ename == "pe":
                continue
            if self.known[ename].get(k, 0) >= v:
                continue
            e.wait_ge(self.sem[k], v)
            self.nwait += 1
            self.known[ename][k] = v

    def _commit(self, key, val, R, W):
        for r in R:
            self.readers.setdefault(r, {})[key] = val
        for w in W:
            self.lastw[w] = (key, val)
            self.readers[w] = {}

    def begin(self):
        self.rec = []

    def end(self):
        r, self.rec = self.rec, None
        return r

    DEF_COST = {"pe": 0.35, "act": 0.42, "dve": 0.33, "pool": 0.5, "sp": 0.1}
    SYNC = float(os.environ.get("M_SYNC", "0.15"))
    CSCALE = float(os.environ.get("M_CSCALE", "1.0"))

    def _m_norm(self, R, W):
        W2 = tuple(W) + tuple(r for r in R if r in PSUM_RES)
        R2 = tuple(r for r in R if r not in PSUM_RES)
        return R2, W2

    def _m_start(self, ename, R, W):
        if not hasattr(self, "m_free"):
            self.m_free, self.m_w, self.m_r, self.m_we = {}, {}, {}, {}
        t = self.m_free.get(ename, 0.0)
        for r in R:
            if r in self.m_w:
                t = max(t, self.m_w[r] + (self.SYNC if self.m_we.get(r) != ename else 0.05))
        for w in W:
            if w in self.m_w:
                t = max(t, self.m_w[w] + (self.SYNC if self.m_we.get(w) != ename else 0.0))
            if w in self.m_r:
                t = max(t, self.m_r[w] + self.SYNC)
        return t

    def _m_commit(self, ename, R, W, cost):
        R, W = self._m_norm(R, W)
        st = self._m_start(ename, R, W)
        en = st + cost
        self.m_free[ename] = en
        for r in R:
            self.m_r[r] = max(self.m_r.get(r, 0.0), en)
        for w in W:
            self.m_w[w] = en
            self.m_r[w] = 0.0
            self.m_we[w] = ename

    def emit(self, lists):
        lists = [l_ for l_ in lists if l_]
        if SEQ_EMIT:
            lists = [[x for l_ in lists for x in l_]]
        idx = [0] * len(lists)
        QNT = float(os.environ.get("M_QNT", "0.25"))
        rem = []
        for l_ in lists:
            acc, suf = 0.0, [0.0] * (len(l_) + 1)
            for j in range(len(l_) - 1, -1, -1):
                kind_, a_ = l_[j]
                acc += (a_[4] if kind_ == "op" else 0.0)
                suf[j] = acc
            rem.append(suf)
        while True:
            best, bk = None, None
            for k, l_ in enumerate(lists):
                if idx[k] < len(l_):
                    kind, a = l_[idx[k]]
                    if kind == "op":
                        R, W = self._m_norm(a[2], a[3])
                        st = self._m_start(a[0], R, W)
                    elif kind == "call":
                        st = -1.0
                    elif kind == "wait":
                        if not a():
                            continue
                        st = -2.0
                    else:
                        st = self._m_start(a[0], (), ())
                    if QNT > 0 and st >= 0:
                        key = (int(st / QNT), -rem[k][idx[k]], st)
                    else:
                        key = (st, (idx[k] + 0.5) / len(l_))
                    if best is None or key < best:
                        best, bk = key, k
            if bk is None:
                assert all(idx[k] >= len(l_) for k, l_ in enumerate(lists)), "emit: all remaining lists blocked"
                break
            kind, a = lists[bk][idx[bk]]
            idx[bk] += 1
            if kind == "op":
                self.op(*a)
            elif kind == "call":
                a()
            elif kind == "wait":
                pass
            else:
                self.dma(*a)

    def waitfor(self, pred):
        if getattr(self, "rec", None) is not None:
            self.rec.append(("wait", pred))
        else:
            assert pred()

    def call(self, fn):
        if getattr(self, "rec", None) is not None:
            self.rec.append(("call", fn))
        else:
            fn()

    def op(self, ename, fn, R=(), W=(), cost=None):
        if cost is None:
            cost = self.DEF_COST[ename]
        if ename != "pe":
            cost = cost * self.CSCALE
        if getattr(self, "rec", None) is not None:
            self.rec.append(("op", (ename, fn, tuple(R), tuple(W), cost)))
            return
        self._m_commit(ename, R, W, cost)
        W = tuple(W) + tuple(r for r in R if r in PSUM_RES)
        R = tuple(r for r in R if r not in PSUM_RES)
        self._wait(ename, self._deps(ename, R, W))
        ins = fn(self.eng[ename])
        self.cnt[ename] += 1
        ins.then_inc(self.sem[ename], 1)
        self._commit(ename, self.cnt[ename], R, W)

    def dma(self, qname, pairs, R=(), W=(), key=None):
        if getattr(self, "rec", None) is not None:
            self.rec.append(("dma", (qname, pairs, tuple(R), tuple(W), key)))
            return
        key = "dma:" + (key or (W[0] if W else R[0]))
        self._m_commit(key, R, W, 2.5)
        sem = self._key(key)
        self._wait(qname, self._deps(qname, R, W))
        for (o, i) in pairs:
            self.eng[qname].dma_start(out=o, in_=i).then_inc(sem, 16)
            self.cnt[key] += 16
        self._commit(key, self.cnt[key], R, W)

    def finish(self, keys):
        for k in keys:
            k = "dma:" + k
            if k in self.cnt and self.cnt[k] > 0:
                self.eng["sp"].wait_ge(self.sem[k], self.cnt[k])


class WStream:
    def __init__(self, T, slots):
        self.T = T
        self.slots = slots
        self.blocks = []
        self.index = {}
        self.issued = 0

    def plan(self, key, fn):
        self.index[key] = len(self.blocks)
        self.blocks.append(fn)

    def _issue_upto(self, i):
        while self.issued <= i and self.issued < len(self.blocks):
            j = self.issued
            s = j % len(self.slots)
            pairs = self.blocks[j](self.slots[s])
            self.T.dma("pool", pairs, W=["W%d" % s], key="W%d" % s)
            self.issued += 1

    def start(self):
        self._issue_upto(len(self.slots) - 1)

    def get(self, key):
        i = self.index[key]
        self._issue_upto(i)
        s = i % len(self.slots)
        return self.slots[s], "W%d" % s

    def peek(self, key):
        i = self.index[key]
        s_ = i % len(self.slots)
        return self.slots[s_], "W%d" % s_

    def can_issue(self, key):
        i = self.index[key]
        return i < self.issued or i < len(self.slots) or i <= getattr(self, "limit", -1)

    def get_safe(self, key):
        T = self.T
        if getattr(T, "rec", None) is None:
            return self.get(key)
        i = self.index[key]
        T.waitfor(lambda: self.can_issue(key))
        T.call(lambda: self._issue_upto(i))
        return self.peek(key)

    def done(self, key):
        i = self.index[key]
        if not hasattr(self, "done_set"):
            self.done_set, self.first_undone = set(), 0
        self.done_set.add(i)
        while self.first_undone in self.done_set:
            self.first_undone += 1
        self.limit = max(getattr(self, "limit", -1), self.first_undone + len(self.slots) - 1 - WS_LAG)
        if not getattr(self, "throttle", False):
            self._issue_upto(self.limit)

    def pump(self, n):
        lim = min(getattr(self, "limit", -1), self.issued + n - 1)
        self._issue_upto(lim)


def build_program(n_layers=2, final_norm=True, dbg=None):
    dbg = dbg or {}
    nc = bass.Bass("TRN2", target_bir_lowering=False)
    L = 2
    x_d = nc.dram_tensor("x", [S, D], F32, kind="ExternalInput").ap()
    mem_d = nc.dram_tensor("mem", [NMEM, D], F32, kind="ExternalInput").ap()
    w_in_d = nc.dram_tensor("w_in", [L, D, NIN], F32, kind="ExternalInput").ap()
    w_kv_d = nc.dram_tensor("w_mem_kv", [L, D, 1024], F32, kind="ExternalInput").ap()
    w_out_d = nc.dram_tensor("w_out", [L, 1536, D], F32, kind="ExternalInput").ap()
    pool_w_d = nc.dram_tensor("pool_w", [L, 4, 128, 128], F32, kind="ExternalInput").ap()
    smallp_d = nc.dram_tensor("smallp", [128, L, 56], F32, kind="ExternalInput").ap()
    gb_d = nc.dram_tensor("gb", [7, 128, D], F32, kind="ExternalInput").ap()
    cst_d = nc.dram_tensor("cst", [128, 536], F32, kind="ExternalInput").ap()
    out_d = nc.dram_tensor("out", [S, D], F32, kind="ExternalOutput").ap()
    dbg_d = {}
    for name, shape in dbg.items():
        dbg_d[name] = nc.dram_tensor("dbg_" + name, list(shape), F32, kind="ExternalOutput").ap()

    with contextlib.ExitStack() as es:
        T = Tracker(nc, es)

        def sb(name, shape, dt):
            return es.enter_context(nc.sbuf_tensor(name, list(shape), dt))

        def ps(name, shape, dt):
            return es.enter_context(nc.psum_tensor(name, list(shape), dt))

        X = sb("X", [128, 16, D], F32)
        XNT = sb("XNT", [128, 8, 1024], BF16)
        YT = sb("YT", [128, 12, 1024], BF16)
        slots = [sb("W%d" % i, [128, 1024], BF16) for i in range(RING)]
        FBIG = sb("FBIG", [128, 8 * 528], F32)
        FT = [FBIG[:, i * 528:(i + 1) * 528] for i in range(8)]
        G0 = sb("G0", [128, 2048], BF16)
        G1 = sb("G1", [128, 2048], BF16)
        G2 = sb("G2", [128, 2048], BF16)
        XNK = [sb("XNK%d" % i, [128, 1024], BF16) for i in range(2)]
        MEMB = FBIG[:, 0:1024]
        CST = sb("CST", [128, 536], F32)
        IDB = sb("IDB", [128, 128], BF16)
        ONESF = sb("ONESF", [128, 128], F32)
        NEGH = sb("NEGH", [128, 32], F32)
        GX = sb("GX", [128, D], F32)
        GM = GX
        DG = sb("DG", [128, 4, 4, 128], BF16)
        CBROW = sb("CBROW", [1, 1024], BF16)
        ONESROW = sb("ONESROW", [1, 512], BF16)
        HALOB = sb("HALOB", [128, 8, 4], BF16)
        SPALL = sb("SPALL", [128, L, 56], F32)
        SPX2 = sb("SPX", [128, 2, 24], F32)
        PW = sb("PW", [128, 4, 128], BF16)
        WG = sb("WG", [128, 8, 8], BF16)
        KMT = sb("KMT", [128, 4, 256], BF16)
        VM = sb("VM", [128, 2, 4, 129], BF16)
        SS = sb("SS", [128, 16], F32)
        RSTD = sb("RSTD", [128, 16], F32)
        TMPS = sb("TMPS", [128, 16], F32)
        MSS = sb("MSS", [128, 8], F32)
        HALO_U = sb("HALO_U", [128, 4, 16], F32)
        GA = sb("GA", [128, 8, 8], F32)
        E1 = sb("E1", [128, 8, 4], F32)
        L1 = sb("L1", [128, 32], F32)
        TB = sb("TB", [128, 64], F32)
        EXPB = sb("EXPB", [128, 32], F32)
        WK = sb("WK", [128, 32], F32)
        VB = [sb("VB%d" % i, [128, 2, 129], BF16) for i in range(2)]
        THO = [sb("THO%d" % i, [128, 256], F32) for i in range(2)]
        EBT = sb("EBT", [128, 32], F32)
        IEB = sb("IEB", [128, 32], F32)
        DEC = sb("DEC", [128, 32], F32)
        STB = [sb("STB%d" % i, [128, 256], BF16) for i in range(2)]
        KW = sb("KW", [128, 2, 128], BF16)
        C32 = sb("C32", [128, 4, 129], F32)
        CBA = [sb("CBA%d" % i, [128, 2, 129], BF16) for i in range(2)]
        TTt = sb("TTt", [128, 256], F32)
        HGt = sb("HGt", [128, 256], F32)
        G3 = sb("G3", [128, 1536], BF16)
        SM = sb("SM", [128, 16], F32)
        YTK = [sb("YTK%d" % i, [128, 256], BF16) for i in range(2)]
        RDC = sb("RDC", [128, 4], F32)
        MM = [ps("MM0", [128, 512], F32), ps("MM1", [128, 512], F32)]
        PT = ps("PT", [128, 1024], BF16)
        BBP = ps("BBP", [128, 512], F32)
        DCA = ps("DCA", [128, 512], F32)
        DCB = ps("DCB", [128, 512], F32)
        NUM = ps("NUM", [128, 512], F32)
        MISC = ps("MISC", [128, 512], F32)
        mmi = [0]

        def next_mm():
            mmi[0] ^= 1
            return MM[mmi[0]], "MM%d" % mmi[0]

        U_ = CST[:, 128:256]
        LX_ = CST[:, 256:384]
        INVC = CST[:, 384:400]
        LNC8 = CST[:, 400:408]
        SEL_ = CST[:, 408:536]

        ws = WStream(T, slots)

        def wv(wd, l):
            return wd[l].rearrange("(kc p) n -> p kc n", p=128)

        def feat_block(wd, l, c0):
            def f(slot):
                return [(slot[:, :].rearrange("p (k c) -> p k c", c=128), wv(wd, l)[:, :, c0:c0 + 128])]
            return f

        def mov_block(wd, l, j, c0):
            def f(slot):
                return [(slot[:, :].rearrange("p (k c) -> p k c", c=512), wv(wd, l)[:, 2 * j:2 * j + 2, c0:c0 + 512])]
            return f

        def vo_block(l, j, hp):
            def f(slot):
                v = slot[:, :].rearrange("p (k c) -> p k c", c=512)
                w = wv(w_in_d, l)
                return [(v[:, :, 0:256], w[:, 2 * j:2 * j + 2, O_V + hp * 256:O_V + hp * 256 + 256]),
                        (v[:, :, 256:512], w[:, 2 * j:2 * j + 2, O_O + hp * 256:O_O + hp * 256 + 256])]
            return f

        def wo_block(l, kc):
            def f(slot):
                return [(slot[:, :], w_out_d[l, kc * 128:(kc + 1) * 128, :])]
            return f

        for l in range(n_layers):
            for h in range(4):
                ws.plan(("kvk", l, h), feat_block(w_kv_d, l, h * 128))
            for j in range(4):
                ws.plan(("kvv", l, j), mov_block(w_kv_d, l, j, 512))
            for hf in range(2):
                for j in range(4):
                    ws.plan(("zc", l, hf, j), mov_block(w_in_d, l, j, O_ZC))
                for h in range(4):
                    ws.plan(("qc", l, hf, h), feat_block(w_in_d, l, O_QC + h * 128))
                for g in range(4):
                    ws.plan(("u", l, hf, g), feat_block(w_in_d, l, O_U + g * 128))
                    ws.plan(("zb", l, hf, g), feat_block(w_in_d, l, O_ZB + g * 128))
                for hp in range(2):
                    for hh in range(2):
                        h = 2 * hp + hh
                        ws.plan(("q", l, hf, h), feat_block(w_in_d, l, O_QK + h * 128))
                        ws.plan(("k", l, hf, h), feat_block(w_in_d, l, O_QK + 512 + h * 128))
                    for hh in range(2):
                        ws.plan(("za", l, hf, 2 * hp + hh), feat_block(w_in_d, l, O_ZA + (2 * hp + hh) * 128))
                    for j in range(4):
                        ws.plan(("vo", l, hf, hp, j), vo_block(l, j, hp))
                for ch in range(2):
                    for j in range(6):
                        ws.plan(("wo", l, hf, ch, j), mov_block(w_out_d, l, j, ch * 512))

        T.dma("sp", [(CST[:, :], cst_d[:, :])], W=["CST"])
        T.dma("sp", [(SPALL[:, :, :], smallp_d[:, :, :])], W=["SPALL"])
        T.dma("pool", [(IDB[:, :], cst_d[:, 0:128])], W=["IDB"])
        T.op("dve", lambda e: e.memset(ONESF[:, :], 1.0), W=["ONESF"])
        T.op("dve", lambda e: e.memset(NEGH[:, :], -0.5), W=["NEGH"])
        T.op("dve", lambda e: e.memset(VM[:, :, :, 128:129], 2.0), W=["VM"])
        for p_ in range(2):
            T.op("dve", lambda e, p_=p_: e.memset(VB[p_][:, :, 128:129], 1.0), W=["VB%d" % p_])
        T.op("dve", lambda e: e.memset(ONESROW[:, :], 1.0), W=["ONESROW"])
        ws._issue_upto(7)
        T.dma("sp", [(FBIG[:, 2 * 528:2 * 528 + 1024], gb_d[2])], W=["F2", "F3"], key="GXM")
        T.dma("sp", [(MEMB, mem_d[0:128, :])], W=["F0", "F1"], key="MEMB")
        for t in range(8):
            T.dma("sp", [(X[:, t, :], x_d[t * 128:(t + 1) * 128, :])], W=["X%d" % t])
        pending_done = []

        def wdone(key):
            T.call(lambda: ws.done(key))

        def flush_done():
            for k_ in pending_done:
                ws.done(k_)
            del pending_done[:]
        mbi = [0]

        def next_mbank():
            mbi[0] ^= 1
            return (BBP, "BBP") if mbi[0] else (NUM, "NUM")

        def dump(name, ap_sb, res):
            if name in dbg_d:
                T.dma("pool", [(dbg_d[name], ap_sb)], R=res, key="dbg_" + name)

        def rstd_cols(ss_ap, out_ap, n, res_in, res_out, scale, eps):
            T.op("dve", lambda e: e.tensor_scalar(out=TMPS[:, 0:n], in0=ss_ap, scalar1=scale, scalar2=eps,
                                                   op0=ALU.mult, op1=ALU.add), R=res_in, W=["TMPS"])
            T.op("pool", lambda e: e.tensor_tensor(out=out_ap, in0=TMPS[:, 0:n], in1=NEGH[:, 0:n], op=ALU.pow),
                 R=["TMPS", "NEGH"], W=res_out)

        def norm_transpose(src_ap, src_res, rstd_ap, rstd_res, gtile, gres, par, dst_ap, dst_res):
            xn = XNK[par]
            T.op("dve", lambda e: e.scalar_tensor_tensor(out=xn[:, :], in0=src_ap, scalar=rstd_ap, in1=gtile[:, :],
                                                          op0=ALU.mult, op1=ALU.mult),
                 R=(src_res if isinstance(src_res, list) else [src_res]) + [rstd_res, gres], W=["XNK%d" % par], cost=1.25)

            def tr(e):
                ins = None
                for kc in range(8):
                    ins = e.transpose(PT[:, kc * 128:(kc + 1) * 128], xn[:, kc * 128:(kc + 1) * 128], IDB[:, :])
                return ins
            T.op("pe", tr, R=["XNK%d" % par, "IDB"], W=["PT"], cost=1.2)
            T.op("act", lambda e: e.copy(out=dst_ap, in_=PT[:, :].rearrange("p (k c) -> p k c", c=128)),
                 R=["PT"], W=(["G1a", "G1b"] if dst_res == "G1a" else [dst_res]), cost=1.0)

        def mm_feat(blk, XN_cols, out_ap):
            def f(e):
                ins = None
                bv = blk[:, :].rearrange("p (k c) -> p k c", c=128)
                for kc in range(8):
                    ins = e.matmul(out_ap, bv[:, kc, :], XNT[:, kc, XN_cols[0]:XN_cols[1]], start=(kc == 0), stop=(kc == 7))
                return ins
            return f

        def silu2_from_psum(pm, pm_res, n, th_tile, th_res, out_ap, out_res, c_act=0.65, c_dve=0.72):
            T.op("act", lambda e: e.activation(out=th_tile[:, 0:n], in_=pm[:, 0:n], func=AF.Tanh, scale=0.5),
                 R=[pm_res], W=[th_res], cost=c_act)
            T.op("dve", lambda e: e.scalar_tensor_tensor(out=out_ap, in0=th_tile[:, 0:n], scalar=1.0, in1=pm[:, 0:n],
                                                          op0=ALU.add, op1=ALU.mult),
                 R=[th_res, pm_res], W=(out_res if isinstance(out_res, list) else [out_res]), cost=c_dve)

        CWvs = [SPALL[:, l_, 0:32].rearrange("p (b j) -> p b j", j=4) for l_ in range(L)]

        def layer_setup(l):
            SPXl, sxr = SPX2[:, l % 2, :], "SPX%d" % (l % 2)
            T.dma("pool", [(CBROW[0:1, :], gb_d[5 + l][0:1, :])], W=["CBROW"])
            T.dma("pool", [(PW[:, :, :], pool_w_d[l].rearrange("g c d -> c g d"))], W=["PW"])
            T.dma("pool", [(WG[:, :, :], wv(w_in_d, l)[:, :, O_G:O_G + 8])], W=["WG"])
            SP = SPALL[:, l, :]
            T.op("dve", lambda e: e.tensor_tensor(out=SPXl[:, 0:8], in0=SP[:, 48:56], in1=LNC8, op=ALU.add),
                 R=["SPALL", "CST"], W=[sxr])
            T.op("dve", lambda e: e.tensor_scalar(out=SPXl[:, 8:12], in0=SP[:, 40:44], scalar1=1.0, scalar2=None, op0=ALU.mult),
                 R=["SPALL"], W=[sxr])
            T.op("dve", lambda e: e.tensor_scalar(out=SPXl[:, 12:16], in0=SP[:, 44:48], scalar1=0.5, scalar2=None, op0=ALU.mult),
                 R=["SPALL"], W=[sxr])
            T.op("dve", lambda e: e.memset(C32[:, :, :], 0.0), W=["C32"])
            T.op("dve", lambda e: e.memset(HALO_U[:, :, :], 0.0), W=["HALO_U"])
            T.op("dve", lambda e: e.memset(HALOB[:, :, :], 0.0), W=["HALOB"])

        def phase_M(l, preloaded=False):
            MNT = G1[:, :].rearrange("p (k c) -> p k c", c=256)
            GXM = FBIG[:, 2 * 528:2 * 528 + 1024]
            MNF = FBIG[:, 4 * 528:4 * 528 + 1024]
            if not preloaded:
                T.dma("sp", [(GXM, gb_d[2 + l])], W=["F2", "F3"], key="GXM")
            for mc in range(2):
                if not (preloaded and mc == 0):
                    T.dma("sp", [(MEMB, mem_d[mc * 128:(mc + 1) * 128, :])], W=["F0", "F1"], key="MEMB")
                T.op("act", lambda e, mc=mc: e.activation(out=MNF, in_=MEMB, func=AF.Square, accum_out=MSS[:, mc:mc + 1]),
                     R=["F0", "F1"], W=["MSS", "F4", "F5"], cost=1.2)
                T.op("dve", lambda e, mc=mc: e.tensor_scalar(out=TMPS[:, 15:16], in0=MSS[:, mc:mc + 1], scalar1=1.0 / D, scalar2=EPS,
                                                             op0=ALU.mult, op1=ALU.add), R=["MSS"], W=["TMPS"], cost=0.1)
                T.op("pool", lambda e, mc=mc: e.tensor_tensor(out=MSS[:, 4 + mc:5 + mc], in0=TMPS[:, 15:16], in1=NEGH[:, 0:1], op=ALU.pow),
                     R=["TMPS", "NEGH"], W=["MSS"], cost=0.7)
                T.op("dve", lambda e, mc=mc: e.scalar_tensor_tensor(out=MNF, in0=MEMB, scalar=MSS[:, 4 + mc:5 + mc], in1=GXM,
                                                                    op0=ALU.mult, op1=ALU.mult),
                     R=["F0", "F1", "MSS", "F2", "F3"], W=["F4", "F5"], cost=1.25)
                for half_, (bk, bkr) in enumerate(((BBP, "BBP"), (NUM, "NUM"))):
                    def tr(e, half_=half_, bk=bk):
                        ins = None
                        for k in range(4):
                            kc = half_ * 4 + k
                            ins = e.transpose(bk[:, k * 128:(k + 1) * 128], MNF[:, kc * 128:(kc + 1) * 128], CST[:, 0:128])
                        return ins
                    T.op("pe", tr, R=["F4", "F5", "CST"], W=[bkr], cost=1.0)
                    T.op("act", lambda e, half_=half_, bk=bk, mc=mc: e.copy(out=MNT[:, half_ * 4:half_ * 4 + 4, mc * 128:(mc + 1) * 128],
                                                                          in_=bk[:, :].rearrange("p (k c) -> p k c", c=128)),
                         R=[bkr], W=["G1a", "G1b"], cost=0.7)
            for h in range(4):
                blk, bres = ws.get_safe(("kvk", l, h))
                pm, pres = next_mbank()

                def f(e, blk=blk, pm=pm):
                    ins = None
                    bv = blk[:, :].rearrange("p (k c) -> p k c", c=128)
                    for kc in range(8):
                        ins = e.matmul(pm[:, 0:256], bv[:, kc, :], MNT[:, kc, :], start=(kc == 0), stop=(kc == 7))
                    return ins
                T.op("pe", f, R=[bres, "G1a", "G1b"], W=[pres])
                wdone(("kvk", l, h))
                T.op("act", lambda e, pm=pm, h=h: e.activation(out=KMT[:, h, :], in_=pm[:, 0:256], func=AF.Identity, scale=128 ** -0.5),
                     R=[pres], W=["KMT"])
            vblk = [ws.get_safe(("kvv", l, j)) for j in range(4)]
            for mc in range(2):
                pm, pres = next_mbank()

                def f(e, pm=pm, mc=mc):
                    ins = None
                    for kc in range(8):
                        bv = vblk[kc // 2][0][:, :].rearrange("p (k c) -> p k c", c=512)
                        ins = e.matmul(pm[:, :], MNT[:, kc, mc * 128:(mc + 1) * 128], bv[:, kc % 2, :], start=(kc == 0), stop=(kc == 7))
                    return ins
                T.op("pe", f, R=["G1a", "G1b"] + [b_[1] for b_ in vblk], W=[pres])
                T.op("act", lambda e, pm=pm, mc=mc: e.copy(out=VM[:, mc, :, 0:128], in_=pm[:, :].rearrange("p (h e) -> p h e", e=128)),
                     R=[pres], W=["VM"])
            for j in range(4):
                wdone(("kvv", l, j))

        def phase_N(l, hf):
            t0 = hf * 8
            T.dma("sp", [(GX[:, :], gb_d[l])], W=["GX"])
            for tl in range(8):
                t = t0 + tl
                T.op("act", lambda e, t=t: e.activation(out=G2[:, (t % 2) * 1024:(t % 2) * 1024 + 1024], in_=X[:, t, :], func=AF.Square, accum_out=SS[:, t:t + 1]),
                     R=["X%d" % t], W=["SS"] + (["G2a"] if t % 2 == 0 else ["G2b", "G2c"]))
            rstd_cols(SS[:, t0:t0 + 8], RSTD[:, t0:t0 + 8], 8, ["SS"], ["RSTD"], 1.0 / D, EPS)
            for tl in range(8):
                t = t0 + tl
                norm_transpose(X[:, t, :], "X%d" % t, RSTD[:, t:t + 1], "RSTD", GX, "GX", tl % 2,
                               XNT[:, :, tl * 128:(tl + 1) * 128], "XNT%d" % (tl // 4))

        def phase_O(l, hf, fin=False):
            t0 = hf * 8
            SPXl, sxr = SPX2[:, l % 2, :], "SPX%d" % (l % 2)
            for ch in range(2):
                wob = [ws.get_safe(("wo", l, hf, ch, j)) for j in range(6)]
                wv_ = [b_[0][:, :].rearrange("p (k c) -> p k c", c=512) for b_ in wob]
                for kc in range(4):
                    T.op("dve", lambda e, kc=kc, wv_=wv_: e.tensor_scalar(out=wv_[kc // 2][:, kc % 2, :], in0=wv_[kc // 2][:, kc % 2, :],
                                                                scalar1=SPXl[:, 8 + kc:9 + kc], scalar2=None, op0=ALU.mult),
                         R=[wob[kc // 2][1], sxr], W=[wob[kc // 2][1]], cost=0.25)
                for tl in range(8):
                    t = t0 + tl
                    c0 = tl * 128
                    pm, pres = next_mm()

                    def f(e, pm=pm, c0=c0, wv_=wv_):
                        ins = None
                        for kc in range(12):
                            ins = e.matmul(pm[:, :], YT[:, kc, c0:c0 + 128], wv_[kc // 2][:, kc % 2, :],
                                           start=(kc == 0), stop=(kc == 11))
                        return ins
                    yres = ["YT%s%d" % (b_, tl // 4) for b_ in "abc"]
                    T.op("pe", f, R=yres + [b_[1] for b_ in wob], W=[pres], cost=2.8)
                    T.op("dve", lambda e, pm=pm, t=t, ch=ch: e.tensor_tensor(out=X[:, t, ch * 512:(ch + 1) * 512], in0=pm[:, :],
                                                                            in1=X[:, t, ch * 512:(ch + 1) * 512], op=ALU.add),
                         R=[pres, "X%d" % t], W=["X%d" % t], cost=0.7)
                    if fin and ch == 1:
                        final_tile(t, 8 + tl)
                for j in range(6):
                    wdone(("wo", l, hf, ch, j))

        okeys = []

        def final_tile(t, col):
            if final_norm:
                T.op("act", lambda e: e.activation(out=G2[:, (t % 2) * 1024:(t % 2) * 1024 + 1024], in_=X[:, t, :], func=AF.Square, accum_out=SS[:, t:t + 1]),
                     R=["X%d" % t], W=["SS"] + (["G2a"] if t % 2 == 0 else ["G2b", "G2c"]), cost=1.1)
                T.op("dve", lambda e: e.tensor_scalar(out=TMPS[:, col:col + 1], in0=SS[:, t:t + 1], scalar1=1.0 / D, scalar2=EPS,
                                                      op0=ALU.mult, op1=ALU.add), R=["SS"], W=["TMPS"], cost=0.1)
                T.op("pool", lambda e: e.tensor_tensor(out=RSTD[:, t:t + 1], in0=TMPS[:, col:col + 1], in1=NEGH[:, 0:1], op=ALU.pow),
                     R=["TMPS", "NEGH"], W=["RSTD"], cost=0.7)
                T.op("dve", lambda e: e.scalar_tensor_tensor(out=X[:, t, :], in0=X[:, t, :], scalar=RSTD[:, t:t + 1], in1=GX[:, :],
                                                             op0=ALU.mult, op1=ALU.mult), R=["X%d" % t, "RSTD", "GX"], W=["X%d" % t], cost=1.25)
            T.dma("sp", [(out_d[t * 128:(t + 1) * 128, :], X[:, t, :])], R=["X%d" % t], key="out%d" % t)
            okeys.append("out%d" % t)

        def phase_final(ts, load_g=True):
            n_ = len(ts)
            if final_norm:
                if load_g:
                    T.dma("sp", [(GX[:, :], gb_d[4])], W=["GX"])
                for t in ts:
                    T.op("act", lambda e, t=t: e.activation(out=G2[:, (t % 2) * 1024:(t % 2) * 1024 + 1024], in_=X[:, t, :], func=AF.Square, accum_out=SS[:, t:t + 1]),
                         R=["X%d" % t], W=["SS"] + (["G2a"] if t % 2 == 0 else ["G2b", "G2c"]))
                rstd_cols(SS[:, ts[0]:ts[0] + n_], RSTD[:, ts[0]:ts[0] + n_], n_, ["SS"], ["RSTD"], 1.0 / D, EPS)
                for t in ts:
                    T.op("dve", lambda e, t=t: e.scalar_tensor_tensor(out=X[:, t, :], in0=X[:, t, :], scalar=RSTD[:, t:t + 1], in1=GX[:, :],
                                                                     op0=ALU.mult, op1=ALU.mult), R=["X%d" % t, "RSTD", "GX"], W=["X%d" % t])
            for t in ts:
                T.dma("sp", [(out_d[t * 128:(t + 1) * 128, :], X[:, t, :])], R=["X%d" % t], key="out%d" % t)
                okeys.append("out%d" % t)

        layer_setup(0)
        T.begin()
        phase_M(0, preloaded=True)
        LM0 = T.end()
        T.begin()
        phase_N(0, 0)
        LN0 = T.end()
        T.emit([LM0, LN0])
        flush_done()
        ws._issue_upto(RING - 1)
        for t in range(8, 16):
            T.dma("sp", [(X[:, t, :], x_d[t * 128:(t + 1) * 128, :])], W=["X%d" % t])
        for l in range(n_layers):
            SPXl, sxr = SPX2[:, l % 2, :], "SPX%d" % (l % 2)
            CWv = CWvs[l]
            CBv = SPALL[:, l, 32:40]
            for hf in range(2):
                t0 = hf * 8
                first = (hf == 0)
                if l == 0 and hf == 0:
                    dump("xnt", XNT[:, :, :], ["XNT0", "XNT1"])

                zcb = [ws.get(("zc", l, hf, j)) for j in range(4)]
                qcb = [ws.get(("qc", l, hf, h)) for h in range(4)]
                YC = G0[:, :].rearrange("p (t c) -> p t c", c=512)
                SZC = G1[:, :].rearrange("p (t c) -> p t c", c=512)
                cb_ = [(MM[0], "MM0"), (NUM, "NUM")]
                ci_ = [0]

                def next_c():
                    ci_[0] = (ci_[0] + 1) % 2
                    return cb_[ci_[0]]
                listC = []
                for mt in range(2):
                    xres = "XNT%d" % mt
                    T.begin()
                    for tt in range(4):
                        c0 = mt * 512 + tt * 128
                        pm, pres = next_c()

                        def f(e, pm=pm, c0=c0):
                            ins = None
                            for kc in range(8):
                                bv = zcb[kc // 2][0][:, :].rearrange("p (k c) -> p k c", c=512)
                                ins = e.matmul(pm[:, :], XNT[:, kc, c0:c0 + 128], bv[:, kc % 2, :], start=(kc == 0), stop=(kc == 7))
                            return ins
                        T.op("pe", f, R=[xres] + [b[1] for b in zcb], W=[pres], cost=1.9)
                        th = FT[6 + tt % 2]
                        silu2_from_psum(pm, pres, 512, th, "F%d" % (6 + tt % 2), SZC[:, tt, :], "G1a" if tt < 2 else "G1b")
                    if mt == 1:
                        for j in range(4):
                            T.call(lambda j=j: ws.done(("zc", l, hf, j)))
                    Zl = T.end()
                    Pl, Vl = [], []
                    for h in range(4):
                        T.begin()
                        if h % 2 == 0:
                            PTB, QCB, ptr_, qcr_ = G2[:, 0:1024].rearrange("p (m c) -> p m c", c=512), G2[:, 1024:1536], "G2a", "G2b"
                        else:
                            PTB, QCB, ptr_, qcr_ = G3[:, 0:1024].rearrange("p (m c) -> p m c", c=512), G3[:, 1024:1536], "G3a", "G3b"
                        pm, pres = next_c()
                        T.op("pe", mm_feat(qcb[h][0], (mt * 512, mt * 512 + 512), pm[:, :]), R=[xres, qcb[h][1]], W=[pres], cost=1.9)
                        T.op("act", lambda e, pm=pm, QCB=QCB: e.copy(out=QCB, in_=pm[:, :]), R=[pres], W=[qcr_], cost=0.65)
                        for mc in range(2):
                            pm2, pres2 = next_c()
                            T.op("pe", lambda e, pm2=pm2, mc=mc, h=h, QCB=QCB: e.matmul(pm2[:, :], KMT[:, h, mc * 128:(mc + 1) * 128], QCB, start=True, stop=True),
                                 R=["KMT", qcr_], W=[pres2])
                            T.op("act", lambda e, pm2=pm2, mc=mc, PTB=PTB: e.activation(out=PTB[:, mc, :], in_=pm2[:, :], func=AF.Exp),
                                 R=[pres2], W=[ptr_], cost=0.65)
                        if mt == 1:
                            T.call(lambda h=h: ws.done(("qc", l, hf, h)))
                        Pl.append(T.end())
                        T.begin()
                        for tp in range(2):
                            bank, bres_ = (DCA, "DCA") if tp == 0 else (DCB, "DCB")

                            def f(e, bank=bank, tp=tp, h=h, PTB=PTB):
                                ins = None
                                for i in range(2):
                                    tt = tp * 2 + i
                                    for mc in range(2):
                                        ins = e.matmul(bank[:, i * 129:(i + 1) * 129], PTB[:, mc, tt * 128:(tt + 1) * 128],
                                                       VM[:, mc, h, :], start=(mc == 0), stop=(mc == 1))
                                return ins
                            T.op("pe", f, R=[ptr_, "VM"], W=[bres_], cost=0.5)
                            bv = bank[:, 0:258].rearrange("p (i e) -> p i e", e=129)
                            T.op("dve", lambda e, bv=bv, tp=tp: e.reciprocal(out=RDC[:, 2 * tp:2 * tp + 2], in_=bv[:, :, 128]), R=[bres_], W=["RDC%d" % tp], cost=0.2)
                            for i in range(2):
                                tt = tp * 2 + i
                                T.op("dve", lambda e, bv=bv, i=i, tt=tt, h=h, tp=tp: e.scalar_tensor_tensor(
                                    out=YC[:, tt, h * 128:(h + 1) * 128], in0=bv[:, i, 0:128], scalar=RDC[:, 2 * tp + i:2 * tp + i + 1],
                                    in1=SZC[:, tt, h * 128:(h + 1) * 128], op0=ALU.mult, op1=ALU.mult),
                                    R=[bres_, "RDC%d" % tp, "G1a", "G1b"], W=["G0a" if tt < 2 else "G0b"], cost=0.3)
                        Vl.append(T.end())
                    T.begin()
                    for tt in range(4):
                        c0 = mt * 512 + tt * 128

                        def tr(e, tt=tt):
                            ins = None
                            for h in range(4):
                                ins = e.transpose(PT[:, h * 128:(h + 1) * 128], YC[:, tt, h * 128:(h + 1) * 128], IDB[:, :])
                            return ins
                        T.op("pe", tr, R=["G0a", "G0b", "IDB"], W=["PT"])
                        T.op("act", lambda e, c0=c0: e.copy(out=YT[:, 8:12, c0:c0 + 128], in_=PT[:, 0:512].rearrange("p (h c) -> p h c", c=128)),
                             R=["PT"], W=["YTc%d" % mt])
                    TRl = T.end()
                    listC += Zl + Pl[0] + Pl[1] + Vl[0] + Pl[2] + Vl[1] + Pl[3] + Vl[2] + Vl[3] + TRl

                ub = [ws.get(("u", l, hf, g)) for g in range(4)]
                zbb = [ws.get(("zb", l, hf, g)) for g in range(4)]
                UW, S1, S2, THB, SZB, TMPF = FT[0], FT[1], FT[2], FT[3], FT[4], FT[5]
                Xl, Yl = [], []
                for mt in range(2):
                    xres = "XNT%d" % mt
                    for g in range(4):
                        T.begin()
                        win = 2 << g
                        PB, pbr = XNK[len(Xl) % 2][:, 0:512], "XNK%d" % (len(Xl) % 2)
                        pm, pres = MM[1], "MM1"
                        T.op("pe", mm_feat(ub[g][0], (mt * 512, mt * 512 + 512), pm[:, :]), R=[xres, ub[g][1]], W=[pres], cost=1.9)
                        T.op("dve", lambda e, g=g: e.tensor_copy(out=UW[:, 0:16], in_=HALO_U[:, g, :]), R=["HALO_U"], W=["F0"])
                        T.op("act", lambda e, pm=pm: e.copy(out=UW[:, 16:528], in_=pm[:, :]), R=[pres], W=["F0"], cost=0.65)
                        T.op(BADD, lambda e: e.tensor_tensor(out=S1[:, 1:528], in0=UW[:, 1:528], in1=UW[:, 0:527], op=ALU.add),
                             R=["F0"], W=["F1"], cost=0.72 if BADD == "dve" else 1.1)
                        cur, cres = S1, "F1"
                        oth, ores = S2, "F2"
                        lo, sh = 1, 2
                        for step in range(g):
                            nlo = lo + sh
                            T.op(BADD if step % 2 == 0 else BADD2, lambda e, cur=cur, oth=oth, nlo=nlo, sh=sh: e.tensor_tensor(
                                out=oth[:, nlo:528], in0=cur[:, nlo:528], in1=cur[:, nlo - sh:528 - sh], op=ALU.add),
                                R=[cres], W=[ores], cost=0.72)
                            cur, cres, oth, ores = oth, ores, cur, cres
                            lo, sh = nlo, sh * 2
                        T.op("dve", lambda e, cur=cur, win=win, PB=PB: e.scalar_tensor_tensor(
                            out=PB, in0=cur[:, 16:528], scalar=1.0 / win, in1=UW[:, 16:528], op0=ALU.mult, op1=ALU.subtract),
                            R=[cres, "F0"], W=[pbr], cost=0.7)
                        if first and mt == 0:
                            w1 = win - 1
                            T.op("dve", lambda e, cur=cur, w1=w1: e.tensor_tensor(out=TMPF[:, 0:w1], in0=cur[:, 16:16 + w1], in1=INVC[:, 0:w1], op=ALU.mult),
                                 R=[cres, "CST"], W=["F5"])
                            T.op("dve", lambda e, w1=w1, PB=PB: e.tensor_tensor(out=PB[:, 0:w1], in0=TMPF[:, 0:w1], in1=UW[:, 16:16 + w1], op=ALU.subtract),
                                 R=["F5", "F0"], W=[pbr])
                        T.op("dve", lambda e, g=g: e.tensor_copy(out=HALO_U[:, g, :], in_=UW[:, 512:528]), R=["F0"], W=["HALO_U"])
                        Xl.append(T.end())
                        T.begin()
                        pm2, pres2 = BBP, "BBP"
                        T.op("pe", lambda e, pm2=pm2, g=g, PB=PB: e.matmul(pm2[:, :], PW[:, g, :], PB, start=True, stop=True), R=["PW", pbr], W=[pres2])
                        T.op("pe", mm_feat(zbb[g][0], (mt * 512, mt * 512 + 512), MM[1][:, :]), R=[xres, zbb[g][1]], W=["MM1"], cost=1.9)
                        silu2_from_psum(MM[1], "MM1", 512, THB, "F3", SZB[:, 0:512], "F4")
                        T.op("dve", lambda e, pm2=pm2, g=g, mt=mt: e.scalar_tensor_tensor(
                            out=YT[:, 4 + g, mt * 512:(mt + 1) * 512], in0=pm2[:, :], scalar=SPXl[:, 12 + g:13 + g], in1=SZB[:, 0:512],
                            op0=ALU.mult, op1=ALU.mult), R=[pres2, sxr, "F4"], W=["YTb%d" % mt], cost=0.7)
                        if mt == 1:
                            T.call(lambda g=g: (ws.done(("u", l, hf, g)), ws.done(("zb", l, hf, g))))
                        Yl.append(T.end())
                listB = list(Xl[0])
                for k_ in range(8):
                    if k_ + 1 < 8:
                        listB += Xl[k_ + 1]
                    listB += Yl[k_]
                T.begin()
                def f(e):
                    ins = None
                    for tl in range(8):
                        for kc in range(8):
                            ins = e.matmul(MISC[:, tl * 8:(tl + 1) * 8], XNT[:, kc, tl * 128:(tl + 1) * 128], WG[:, kc, :],
                                           start=(kc == 0), stop=(kc == 7))
                    return ins
                T.op("pe", f, R=["XNT0", "XNT1", "WG"], W=["MISC"])
                T.op("dve", lambda e: e.tensor_tensor(out=GA[:, :, :], in0=MISC[:, 0:64].rearrange("p (t g) -> p t g", g=8),
                                                      in1=SPXl[:, 0:8].unsqueeze(1).to_broadcast([128, 8, 8]), op=ALU.add),
                     R=["MISC", sxr], W=["GA"])
                T.op("act", lambda e: e.activation(out=E1[:, :, :], in_=GA[:, :, 4:8], func=AF.Exp, scale=-1.0), R=["GA"], W=["E1"])
                T.op("act", lambda e: e.activation(out=L1[:, :].rearrange("p (t h) -> p t h", h=4), in_=E1[:, :, :], func=AF.Ln, bias=1.0),
                     R=["E1"], W=["L1"])
                T.op("pe", lambda e: e.matmul(MISC[:, 64:96], U_, L1[:, :], start=True, stop=True), R=["CST", "L1"], W=["MISC"])
                T.op("pe", lambda e: e.matmul(MISC[:, 96:128], LX_, L1[:, :], start=True, stop=True), R=["CST", "L1"], W=["MISC"])
                IGC = GA[:, :, 0:4]
                T.op("dve", lambda e: e.tensor_tensor(out=TB[:, 0:32].rearrange("p (t h) -> p t h", h=4), in0=IGC,
                                                      in1=MISC[:, 64:96].rearrange("p (t h) -> p t h", h=4), op=ALU.add),
                     R=["GA", "MISC"], W=["TB"])
                T.op("dve", lambda e: e.tensor_tensor(out=TB[:, 32:64].rearrange("p (t h) -> p t h", h=4), in0=IGC,
                                                      in1=MISC[:, 96:128].rearrange("p (t h) -> p t h", h=4), op=ALU.subtract),
                     R=["GA", "MISC"], W=["TB"])
                T.op("act", lambda e: e.activation(out=EXPB[:, :], in_=TB[:, 0:32], func=AF.Exp), R=["TB"], W=["EXPB"])
                T.op("act", lambda e: e.activation(out=WK[:, :], in_=TB[:, 32:64], func=AF.Exp), R=["TB"], W=["WK"])
                T.op("act", lambda e: e.activation(out=EBT[:, :], in_=MISC[:, 64:96], func=AF.Exp, scale=-1.0), R=["MISC"], W=["EBT"])
                T.op("act", lambda e: e.activation(out=IEB[:, :], in_=MISC[:, 64:96], func=AF.Exp), R=["MISC"], W=["IEB"])
                T.op("pe", lambda e: e.matmul(MISC[:, 128:160], SEL_, EBT[:, :], start=True, stop=True), R=["CST", "EBT"], W=["MISC"])
                T.op("act", lambda e: e.copy(out=DEC[:, :], in_=MISC[:, 128:160]), R=["MISC"], W=["DEC"])

                listG = T.end()
                T.emit([listC, listB, listG])
                if l == 0 and hf == 0:
                    dump("ytc", YT[:, 8:12, :], ["YTc0", "YTc1"])
                if l == 0 and hf == 0:
                    dump("ytb", YT[:, 4:8, :], ["YTb0", "YTb1"])

                QTm = [G0[:, 0:1024].rearrange("p (h c) -> p h c", c=512), G2[:, 0:1024].rearrange("p (h c) -> p h c", c=512)]
                KTm = [G0[:, 1024:2048].rearrange("p (h c) -> p h c", c=512), G2[:, 1024:2048].rearrange("p (h c) -> p h c", c=512)]
                QTr = ["G0a", "G2a"]
                KTr = [["G0b"], ["G2b", "G2c"]]
                SZAm = [G1[:, 1024:2048].rearrange("p (h c) -> p h c", c=512), G1[:, 0:1024].rearrange("p (h c) -> p h c", c=512)]
                SZAr = ["G1b", "G1a"]
                wts = {}

                def fetch_weights(hp, with_vo=True):
                    wts[hp] = ([ws.get(("q", l, hf, 2 * hp + hh)) for hh in range(2)],
                               [ws.get(("k", l, hf, 2 * hp + hh)) for hh in range(2)],
                               [ws.get(("za", l, hf, 2 * hp + hh)) for hh in range(2)],
                               [ws.get(("vo", l, hf, hp, j)) for j in range(4)] if with_vo else None)

                def build_dg(hp, bps=(0, 1, 2, 3)):
                    for bp in bps:
                        blk_i = 4 * (bp % 2) + 2 * hp + bp // 2
                        T.op("dve", lambda e, bp=bp, blk_i=blk_i: e.tensor_tensor(
                            out=DG[:, bp, :, :], in0=IDB[:, :].unsqueeze(1).to_broadcast([128, 4, 128]),
                            in1=CWv[:, blk_i, :].unsqueeze(2).to_broadcast([128, 4, 128]), op=ALU.mult),
                            R=["IDB", "SPALL"], W=["DG%d" % bp], cost=0.7)

                def prepass_block(hp, mt, bidx):
                    qb, kb, zab, vob = wts[hp]
                    xres = "XNT%d" % mt
                    mp = mt % 2
                    if bidx < 4:
                        hh, isk = bidx // 2, bidx % 2
                        h = 2 * hp + hh
                        blk_i = 4 * isk + h
                        wb = (kb if isk else qb)[hh]
                        RAWB, rres = XNK[bidx % 2], "XNK%d" % (bidx % 2)
                        TH, tres = FT[4 + bidx % 2], "F%d" % (4 + bidx % 2)
                        pm, pres = MM[1], "MM1"
                        T.op("pe", mm_feat(wb[0], (mt * 512, mt * 512 + 512), pm[:, :]), R=[xres, wb[1]], W=[pres], cost=1.9)
                        T.op("dve", lambda e: e.tensor_copy(out=RAWB[:, 0:3], in_=HALOB[:, blk_i, 0:3]), R=["HALOB"], W=[rres], cost=0.1)
                        T.op("act", lambda e: e.copy(out=RAWB[:, 3:515], in_=pm[:, :]), R=[pres], W=[rres], cost=0.65)
                        T.op("act", lambda e: e.copy(out=HALOB[:, blk_i, 0:3], in_=RAWB[:, 512:515]), R=[rres], W=["HALOB"], cost=0.2)
                        pc, pcres = DCB, "DCB"

                        def f(e):
                            ins = None
                            for j in range(4):
                                ins = e.matmul(pc[:, :], DG[:, bidx, j, :], RAWB[:, j:j + 512], start=(j == 0), stop=(j == 3))
                            return ins
                        T.op("pe", f, R=["DG%d" % bidx, rres], W=[pcres], cost=0.95)
                        dstv = (KTm if isk else QTm)[mp][:, hh, :]
                        dstres = KTr[mp] if isk else [QTr[mp]]
                        cbv = CBv
                        T.op("act", lambda e: e.activation(out=dstv, in_=pc[:, 0:512], func=AF.Silu, bias=cbv[:, blk_i:blk_i + 1]),
                             R=[pcres, "SPALL"], W=dstres, cost=0.65)
                    else:
                        hh = bidx - 4
                        pm, pres = MM[1], "MM1"
                        T.op("pe", mm_feat(zab[hh][0], (mt * 512, mt * 512 + 512), pm[:, :]), R=[xres, zab[hh][1]], W=[pres], cost=1.9)
                        T.op("act", lambda e: e.activation(out=SZAm[mp][:, hh, :], in_=pm[:, 0:512], func=AF.Silu),
                             R=[pres], W=[SZAr[mp]], cost=0.65)

                def front(g):
                    hp, i = g // 8, g % 8
                    qb, kb, zab, vob = wts[hp]
                    mt, tt = i // 4, i % 4
                    mp = mt % 2
                    p = g % 2
                    xres = "XNT%d" % mt
                    QT, KT = QTm[mp], KTm[mp]
                    qres, kres = QTr[mp], KTr[mp]
                    c0 = i * 128
                    cm = tt * 128
                    vb, vbr = VB[p], "VB%d" % p
                    T.op("act", lambda e: e.copy(out=CBA[p][:, :, :], in_=C32[:, 2 * hp:2 * hp + 2, :]), R=["C32"], W=["CBA%d" % p])
                    pm, pres = MM[0], "MM0"

                    def f(e):
                        ins = None
                        for kc in range(8):
                            bv = vob[kc // 2][0][:, :].rearrange("p (k c) -> p k c", c=512)
                            ins = e.matmul(pm[:, :], XNT[:, kc, c0:c0 + 128], bv[:, kc % 2, :], start=(kc == 0), stop=(kc == 7))
                        return ins
                    T.op("pe", f, R=[xres] + [b_[1] for b_ in vob], W=[pres], cost=1.9)
                    gi = [i * 4 + 2 * hp + hh for hh in range(2)]

                    def tr(e):
                        ins = None
                        for hh in range(2):
                            ins = e.transpose(PT[:, hh * 128:(hh + 1) * 128], KT[:, hh, cm:cm + 128], IDB[:, :])
                        return ins
                    T.op("pe", tr, R=kres + ["IDB"], W=["PT"])
                    T.op("act", lambda e: e.copy(out=vb[:, :, 0:128], in_=pm[:, 0:256].rearrange("p (h e) -> p h e", e=128)),
                         R=[pres], W=[vbr])
                    T.op("act", lambda e: e.activation(out=THO[p][:, :], in_=pm[:, 256:512], func=AF.Tanh, scale=0.5),
                         R=[pres], W=["THO%d" % p])
                    if KW_ACT:
                        for hh in range(2):
                            T.op("act", lambda e, hh=hh: e.activation(out=KW[:, hh, :], in_=PT[:, hh * 128:(hh + 1) * 128], func=AF.Copy,
                                                                     scale=WK[:, gi[hh]:gi[hh] + 1]),
                                 R=["PT", "WK"], W=["KW%d" % hh], cost=0.3)
                    else:
                        T.op("dve", lambda e: e.tensor_tensor(
                            out=KW[:, :, :], in0=PT[:, 0:256].rearrange("p (h c) -> p h c", c=128),
                            in1=WK[:, gi[0]:gi[0] + 2].unsqueeze(2).to_broadcast([128, 2, 128]), op=ALU.mult),
                            R=["PT", "WK"], W=["KW0", "KW1"], cost=0.42)

                    def f(e):
                        ins = None
                        for hh in range(2):
                            ins = e.matmul(DCA[:, hh * 129:(hh + 1) * 129], KW[:, hh, :], vb[:, hh, :], start=True, stop=True)
                        return ins
                    T.op("pe", f, R=["KW0", "KW1", vbr], W=["DCA"], cost=0.35)

                    def f(e):
                        ins = None
                        for hh in range(2):
                            ins = e.matmul(BBP[:, hh * 128:(hh + 1) * 128], KT[:, hh, cm:cm + 128], QT[:, hh, cm:cm + 128], start=True, stop=True)
                        return ins
                    T.op("pe", f, R=kres + [qres], W=["BBP"], cost=0.3)
                    for hh in range(2):
                        h = 2 * hp + hh
                        T.op("dve", lambda e, hh=hh, h=h: e.scalar_tensor_tensor(
                            out=C32[:, h, :], in0=C32[:, h, :], scalar=DEC[:, gi[hh]:gi[hh] + 1],
                            in1=DCA[:, hh * 129:(hh + 1) * 129], op0=ALU.mult, op1=ALU.add), R=["C32", "DEC", "DCA"], W=["C32"], cost=0.36)
                    for hh in range(2):
                        T.op("dve", lambda e, hh=hh, g_=gi[hh]: e.scalar_tensor_tensor(
                            out=STB[p][:, hh * 128:(hh + 1) * 128], in0=BBP[:, hh * 128:(hh + 1) * 128], scalar=EXPB[:, g_:g_ + 1],
                            in1=U_, op0=ALU.mult, op1=ALU.mult), R=["BBP", "EXPB", "CST"], W=["STB%d" % p], cost=0.27)

                def back_a(g):
                    hp, i = g // 8, g % 8
                    p = g % 2
                    vb, vbr = VB[p], "VB%d" % p
                    mp = (i // 4) % 2
                    QT, qres = QTm[mp], QTr[mp]
                    cm = (i % 4) * 128
                    gi0 = i * 4 + 2 * hp

                    def f(e):
                        ins = None
                        for hh in range(2):
                            o_ = NUM[:, hh * 129:(hh + 1) * 129]
                            e.matmul(o_, STB[p][:, hh * 128:(hh + 1) * 128], vb[:, hh, :], start=True, stop=False)
                            ins = e.matmul(o_, QT[:, hh, cm:cm + 128], CBA[p][:, hh, :], start=False, stop=True)
                        return ins
                    T.op("pe", f, R=["STB%d" % p, vbr, qres, "CBA%d" % p], W=["NUM"], cost=0.5)
                    NV = NUM[:, 0:258].rearrange("p (h e) -> p h e", e=129)
                    T.op("act", lambda e: e.activation(out=SM[:, 0:2], in_=NV[:, :, 128], func=AF.Abs), R=["NUM"], W=["SM"])
                    T.op("dve", lambda e: e.scalar_tensor_tensor(
                        out=TTt[:, :].rearrange("p (h e) -> p h e", e=128), in0=THO[p][:, :].rearrange("p (h e) -> p h e", e=128),
                        scalar=1.0, in1=NV[:, :, 0:128], op0=ALU.add, op1=ALU.mult), R=["THO%d" % p, "NUM"], W=["TTt0", "TTt1"])
                    T.op("dve", lambda e: e.tensor_tensor(out=SM[:, 2:4], in0=SM[:, 0:2], in1=IEB[:, gi0:gi0 + 2], op=ALU.max),
                         R=["SM", "IEB"], W=["SMb"], cost=0.1)
                    T.op("dve", lambda e: e.tensor_tensor(out=SM[:, 4:6], in0=SM[:, 2:4], in1=SM[:, 2:4], op=ALU.mult), R=["SMb"], W=["SMc"], cost=0.1)
                    for hh in range(2):
                        T.op("act", lambda e, hh=hh: e.activation(out=HGt[:, hh * 128:(hh + 1) * 128], in_=TTt[:, hh * 128:(hh + 1) * 128], func=AF.Square,
                                                                 scale=128 ** -0.5, accum_out=SM[:, 6 + hh:7 + hh]),
                             R=["TTt%d" % hh], W=["SMd%d" % hh, "HGt%d" % hh])
                    T.op("dve", lambda e: e.scalar_tensor_tensor(out=SM[:, 8:10], in0=SM[:, 4:6], scalar=4 * EPS, in1=SM[:, 6:8],
                                                                 op0=ALU.mult, op1=ALU.add), R=["SMc", "SMd0", "SMd1"], W=["SMe"], cost=0.1)
                    T.op("pool", lambda e: e.tensor_tensor(out=SM[:, 10:12], in0=SM[:, 8:10], in1=NEGH[:, 0:2], op=ALU.pow),
                         R=["SMe", "NEGH"], W=["SMf"], cost=0.7)
                    T.op("dve", lambda e: e.tensor_tensor(
                        out=YTK[p][:, :].rearrange("p (h e) -> p h e", e=128), in0=TTt[:, :].rearrange("p (h e) -> p h e", e=128),
                        in1=SM[:, 10:12].unsqueeze(2).to_broadcast([128, 2, 128]), op=ALU.mult),
                        R=["TTt0", "TTt1", "SMf"], W=["YTK%d" % p], cost=0.42)

                def back_b(g):
                    hp, i = g // 8, g % 8
                    mt, tt = i // 4, i % 4
                    mp = mt % 2
                    p = g % 2
                    c0 = i * 128
                    cm = tt * 128

                    def tr(e):
                        ins = None
                        for hh in range(2):
                            ins = e.transpose(PT[:, 256 + hh * 128:384 + hh * 128], YTK[p][:, hh * 128:(hh + 1) * 128], IDB[:, :])
                        return ins
                    T.op("pe", tr, R=["YTK%d" % p, "IDB"], W=["PT"])
                    T.op("dve", lambda e: e.tensor_tensor(
                        out=YT[:, 2 * hp:2 * hp + 2, c0:c0 + 128], in0=PT[:, 256:512].rearrange("p (h c) -> p h c", c=128),
                        in1=SZAm[mp][:, :, cm:cm + 128], op=ALU.mult), R=["PT", SZAr[mp]], W=["YTa%d" % mt])

                def rec(fn, *a):
                    T.begin()
                    fn(*a)
                    return T.end()

                fetch_weights(0)
                ws.throttle = True
                build_dg(0)
                for bidx in range(6):
                    prepass_block(0, 0, bidx)
                front(0)
                for g in range(16):
                    hp, i = g // 8, g % 8
                    lists = []
                    if g == 7:
                        fetch_weights(1)
                    if g + 1 < 16:
                        lists.append(rec(front, g + 1))
                    lists.append(rec(back_a, g))
                    if g >= 1:
                        lists.append(rec(back_b, g - 1))
                    if i < 3:
                        lists.append(rec(lambda: (prepass_block(hp, 1, 2 * i), prepass_block(hp, 1, 2 * i + 1),
                                                  (build_dg(1, (0, 1)) if (hp == 0 and i == 2) else None))))
                    elif hp == 0 and i == 3:
                        lists.append(rec(lambda: build_dg(1, (2, 3))))
                    elif hp == 0 and 4 <= i < 7:
                        if i == 4:
                            fetch_weights(1, with_vo=False)
                        k_ = i - 4
                        lists.append(rec(lambda: (prepass_block(1, 0, 2 * k_), prepass_block(1, 0, 2 * k_ + 1))))

                    T.emit(lists)
                    if i == 3:
                        for hh in range(2):
                            ws.done(("q", l, hf, 2 * hp + hh))
                            ws.done(("k", l, hf, 2 * hp + hh))
                        for hh in range(2):
                            ws.done(("za", l, hf, 2 * hp + hh))
                    if i == 7:
                        for j in range(4):
                            ws.done(("vo", l, hf, hp, j))
                    ws.pump(PUMP_N)
                T.emit([rec(back_b, 15)])
                ws.throttle = False
                ws._issue_upto(ws.limit)
                if l == 0 and hf == 0:
                    dump("yta", YT[:, 0:4, :], ["YTa0", "YTa1"])

                last = (hf == 1 and l + 1 == n_layers)
                if last and final_norm:
                    T.dma("sp", [(GX[:, :], gb_d[4])], W=["GX"])
                T.begin()
                phase_O(l, hf, fin=last)
                LO = T.end()
                T.begin()
                if hf == 0:
                    phase_N(l, 1)
                elif l + 1 < n_layers:
                    layer_setup(l + 1)
                    phase_N(l + 1, 0)
                else:
                    phase_final(list(range(0, 8)), load_g=False)
                LN = T.end()
                lists_ = [LO, LN]
                if hf == 1 and l + 1 < n_layers:
                    T.begin()
                    phase_M(l + 1)
                    lists_.append(T.end())
                T.emit(lists_)
                flush_done()
        T.finish(okeys + ["dbg_" + n for n in dbg_d])
    return nc


def host_constants():
    idx = np.arange(128)
    same = (idx[:, None] // 64) == (idx[None, :] // 64)
    ident = np.eye(128, dtype=np.float32)
    U = (idx[:, None] <= idx[None, :]).astype(np.float32)
    LX = (idx[:, None] > idx[None, :]).astype(np.float32)
    invc = np.broadcast_to(1.0 / np.arange(1, 17, dtype=np.float32), (128, 16))
    lnc8 = np.broadcast_to(np.array([LNC] * 4 + [0.0] * 4, dtype=np.float32), (128, 8))
    sel = np.zeros((128, 128), np.float32)
    sel[127, :] = 1.0
    return np.ascontiguousarray(np.concatenate([ident, U, LX, invc, lnc8, sel], axis=1).astype(np.float32))


def host_layout(norm_g, b_gates, conv_w, conv_b, mlstm_norm_g, pool_scale, mem_norm_g, final_norm_g):
    Ln = norm_g.shape[0]
    sp = np.zeros((128, Ln, 56), np.float32)
    for l in range(Ln):
        cw = conv_w[l].reshape(4, 8, 128)
        sp[:, l, 0:32] = cw.transpose(2, 1, 0).reshape(128, 32)
        sp[:, l, 32:40] = conv_b[l].reshape(8, 128).T
        sp[:, l, 40:44] = mlstm_norm_g[l].reshape(4, 128).T
        sp[:, l, 44:48] = pool_scale[l].reshape(4, 128).T
        sp[:, l, 48:56] = np.broadcast_to(b_gates[l], (128, 8))
    gb = np.stack([np.broadcast_to(v, (128, D)) for v in (norm_g[0], norm_g[1], mem_norm_g[0], mem_norm_g[1], final_norm_g,
                                                            conv_b[0], conv_b[1])])
    return sp, np.ascontiguousarray(gb.astype(np.float32))


_CACHE = {}


def kernel(x, mem, norm_g, w_in, b_gates, conv_w, conv_b, mlstm_norm_g, pool_w, pool_scale,
           mem_norm_g, w_mem_kv, w_out, final_norm_g):
    f = lambda a: np.ascontiguousarray(np.asarray(a, dtype=np.float32))
    x, mem, w_in, w_mem_kv, w_out, pool_w = f(x), f(mem), f(w_in), f(w_mem_kv), f(w_out), f(pool_w)
    sp, gb = host_layout(f(norm_g), f(b_gates), f(conv_w), f(conv_b), f(mlstm_norm_g), f(pool_scale), f(mem_norm_g), f(final_norm_g))
    cst = host_constants()
    if "nc" not in _CACHE:
        _CACHE["nc"] = build_program()
    nc = _CACHE["nc"]
    n = x.shape[0]
    in_maps = [{"x": x[b], "mem": mem[b], "w_in": w_in, "w_mem_kv": w_mem_kv, "w_out": w_out, "pool_w": pool_w,
                "smallp": sp, "gb": gb, "cst": cst} for b in range(n)]
    res = run_bass_kernel_spmd(nc, in_maps, core_ids=list(range(n)))
    return np.stack([np.asarray(r["out"], dtype=np.float32) for r in res.results], axis=0)
```
